# Optimizing a Trainium2 kernel written in Bass

```python
import math
import jax, jax.numpy as jnp
from jax import lax
import numpy as np

D_MODEL = 1024
BATCH = 8
SEQ = 8192
DEPTH = 2
DEC_BATCH = 16
DEC_SEQ = 4096
PAST_LEN = 128

ATTN_WIDTH = D_MODEL // 2
SSM_WIDTH = D_MODEL - ATTN_WIDTH
ATT_HEAD_DIM = 64
ATT_V_DIM = 2 * ATT_HEAD_DIM
ATT_HEADS = ATTN_WIDTH // ATT_V_DIM
QK_WIDTH = ATT_HEADS * 2 * ATT_HEAD_DIM
IN_WIDTH = 2 * QK_WIDTH + ATTN_WIDTH + SSM_WIDTH
SSM_GROUP = 16
SSM_GROUPS = SSM_WIDTH // SSM_GROUP
SSM_STATE = 64
FNET_GROUPS = 4
FNET_GROUP_WIDTH = D_MODEL // FNET_GROUPS
N_EXPERTS = 16
EC_CAPACITY_FACTOR = 2
D_FF = 2816
ROPE_THETA = 10000.0
LN_EPS = 1e-5
Q_BLOCK = 128
N_EVEN = (DEPTH + 1) // 2
N_ODD = DEPTH // 2
DEEPNORM_ALPHA = (2 * DEPTH) ** 0.25
DEEPNORM_BETA = (8 * DEPTH) ** -0.25

kernel_name = 'hybrid_diffattn_s5_fnet_ec_encoder'

F32 = jnp.float32


def layer_norm(x, g, b):
    xf = x.astype(F32)
    mu = jnp.mean(xf, -1, keepdims=True)
    var = jnp.mean(jnp.square(xf - mu), -1, keepdims=True)
    y = (xf - mu) * lax.rsqrt(var + LN_EPS)
    return (y * g.astype(F32) + b.astype(F32)).astype(x.dtype)


def rope_tables(seq_len, dim):
    inv = 1.0 / (ROPE_THETA ** (jnp.arange(0, dim, 2, dtype=F32) / dim))
    ang = jnp.arange(seq_len, dtype=F32)[:, None] * inv[None, :]
    ang = jnp.concatenate([ang, ang], -1)
    return jnp.cos(ang), jnp.sin(ang)


def apply_rope(x, cos, sin):
    half = x.shape[-1] // 2
    rot = jnp.concatenate([-x[..., half:], x[..., :half]], -1)
    c = cos[None, :, None, None, :].astype(x.dtype)
    s = sin[None, :, None, None, :].astype(x.dtype)
    return x * c + rot * s


def diff_attention(q, k, v, lam):
    B, S, H, _, dh = q.shape
    nblk = S // Q_BLOCK
    scale = dh ** -0.5
    qb = q.reshape(B, nblk, Q_BLOCK, H, 2, dh).transpose(1, 0, 2, 3, 4, 5)
    k1 = k[:, :, :, 0]
    k2 = k[:, :, :, 1]

    def block(qblk):
        s1 = jnp.einsum('bqhd,bkhd->bhqk', qblk[:, :, :, 0], k1).astype(F32) * scale
        s2 = jnp.einsum('bqhd,bkhd->bhqk', qblk[:, :, :, 1], k2).astype(F32) * scale
        p = jax.nn.softmax(s1, axis=-1) - lam * jax.nn.softmax(s2, axis=-1)
        return jnp.einsum('bhqk,bkhe->bqhe', p.astype(v.dtype), v)

    out = lax.map(block, qb)
    return out.transpose(1, 0, 2, 3, 4).reshape(B, S, H, v.shape[-1])


def s5_discretize(a_re, a_im, log_dt, b_re, b_im):
    dt = jnp.exp(log_dt)[:, None]
    mag = jnp.exp(a_re * dt)
    lr = mag * jnp.cos(a_im * dt)
    li = mag * jnp.sin(a_im * dt)
    nr = lr - 1.0
    den = a_re * a_re + a_im * a_im
    cr = (nr * a_re + li * a_im) / den
    ci = (li * a_re - nr * a_im) / den
    bbr = cr[..., None] * b_re - ci[..., None] * b_im
    bbi = cr[..., None] * b_im + ci[..., None] * b_re
    return lr, li, bbr, bbi


def _complex_linear_combine(e1, e2):
    a1r, a1i, b1r, b1i = e1
    a2r, a2i, b2r, b2i = e2
    return (a2r * a1r - a2i * a1i,
            a2r * a1i + a2i * a1r,
            a2r * b1r - a2i * b1i + b2r,
            a2r * b1i + a2i * b1r + b2i)


def s5_direction(u, a_re, a_im, log_dt, b_re, b_im, c_re, c_im, reverse):
    lr, li, bbr, bbi = s5_discretize(a_re, a_im, log_dt, b_re, b_im)
    bur = jnp.einsum('bsgh,gph->bsgp', u, bbr)
    bui = jnp.einsum('bsgh,gph->bsgp', u, bbi)
    ar = jnp.broadcast_to(lr, bur.shape)
    ai = jnp.broadcast_to(li, bur.shape)
    _, _, hr, hi = lax.associative_scan(_complex_linear_combine, (ar, ai, bur, bui), reverse=reverse, axis=1)
    return jnp.einsum('bsgp,ghp->bsgh', hr, c_re) - jnp.einsum('bsgp,ghp->bsgh', hi, c_im)


def s5_mixer(u, a_re, a_im, log_dt, b_re, b_im, c_re, c_im, d, glu_w, glu_b):
    B, S, _ = u.shape
    uf = u.astype(F32).reshape(B, S, SSM_GROUPS, SSM_GROUP)
    a_re = a_re.astype(F32); a_im = a_im.astype(F32); log_dt = log_dt.astype(F32)
    b_re = b_re.astype(F32); b_im = b_im.astype(F32); c_re = c_re.astype(F32); c_im = c_im.astype(F32)
    y = (s5_direction(uf, a_re[0], a_im[0], log_dt[0], b_re[0], b_im[0], c_re[0], c_im[0], False)
         + s5_direction(uf, a_re[1], a_im[1], log_dt[1], b_re[1], b_im[1], c_re[1], c_im[1], True)
         + d.astype(F32) * uf)
    y = jax.nn.gelu(y.reshape(B, S, SSM_WIDTH))
    gate = jax.nn.sigmoid(y @ glu_w.astype(F32) + glu_b.astype(F32))
    return (y * gate).astype(u.dtype)


def even_mixer(x, w_in, lam_q1, lam_k1, lam_q2, lam_k2, subln_g,
               s5_a_re, s5_a_im, s5_log_dt, s5_b_re, s5_b_im, s5_c_re, s5_c_im, s5_d,
               s5_glu_w, s5_glu_b, w_out, lambda_init, cos, sin):
    B, S, _ = x.shape
    h = x @ w_in
    q = h[..., :QK_WIDTH].reshape(B, S, ATT_HEADS, 2, ATT_HEAD_DIM)
    k = h[..., QK_WIDTH:2 * QK_WIDTH].reshape(B, S, ATT_HEADS, 2, ATT_HEAD_DIM)
    v = h[..., 2 * QK_WIDTH:2 * QK_WIDTH + ATTN_WIDTH].reshape(B, S, ATT_HEADS, ATT_V_DIM)
    u = h[..., 2 * QK_WIDTH + ATTN_WIDTH:]
    q = apply_rope(q, cos, sin)
    k = apply_rope(k, cos, sin)
    lam = (jnp.exp(jnp.sum(lam_q1.astype(F32) * lam_k1.astype(F32)))
           - jnp.exp(jnp.sum(lam_q2.astype(F32) * lam_k2.astype(F32))) + lambda_init)
    o = diff_attention(q, k, v, lam).astype(F32)
    o = o * lax.rsqrt(jnp.mean(o * o, -1, keepdims=True) + LN_EPS) * subln_g.astype(F32)
    attn = (o * (1.0 - lambda_init)).reshape(B, S, ATTN_WIDTH).astype(x.dtype)
    ssm = s5_mixer(u, s5_a_re, s5_a_im, s5_log_dt, s5_b_re, s5_b_im, s5_c_re, s5_c_im, s5_d, s5_glu_w, s5_glu_b)
    return jnp.concatenate([attn, ssm], -1) @ w_out


def fourier_mixer(x, w_out):
    B, S, _ = x.shape
    xg = x.astype(F32).reshape(B, S, FNET_GROUPS, FNET_GROUP_WIDTH)
    f = jnp.fft.fft2(xg, axes=(1, 3), norm='ortho').real
    return f.reshape(B, S, D_MODEL).astype(x.dtype) @ w_out


def expert_choice_ffn(x, w_router, w1, w3, w2):
    B, S, D = x.shape
    n = B * S
    cap = max(1, EC_CAPACITY_FACTOR * n // N_EXPERTS)
    xt = x.reshape(n, D)
    aff = jax.nn.softmax(xt.astype(F32) @ w_router.astype(F32), axis=-1)
    gate, idx = lax.top_k(aff.T, cap)
    xs = xt[idx]

    def expert(args):
        xe, a, c, o = args
        return (jax.nn.silu(xe @ a) * (xe @ c)) @ o

    ye = lax.map(expert, (xs, w1, w3, w2)) * gate[..., None].astype(x.dtype)
    y = jnp.zeros_like(xt).at[idx.reshape(-1)].add(ye.reshape(-1, D))
    return y.reshape(B, S, D)


def trunk(x, w_in, lam_q1, lam_k1, lam_q2, lam_k2, subln_g,
          s5_a_re, s5_a_im, s5_log_dt, s5_b_re, s5_b_im, s5_c_re, s5_c_im, s5_d, s5_glu_w, s5_glu_b,
          w_out_even, w_out_odd, ln_mix_g, ln_mix_b, w_router, w_ff1, w_ff3, w_ff2, ln_ffn_g, ln_ffn_b):
    cos, sin = rope_tables(x.shape[1], ATT_HEAD_DIM)
    for l in range(DEPTH):
        j = l // 2
        if l % 2 == 0:
            lambda_init = 0.8 - 0.6 * math.exp(-0.3 * l)
            m = even_mixer(x, w_in[j], lam_q1[j], lam_k1[j], lam_q2[j], lam_k2[j], subln_g[j],
                           s5_a_re[j], s5_a_im[j], s5_log_dt[j], s5_b_re[j], s5_b_im[j], s5_c_re[j], s5_c_im[j],
                           s5_d[j], s5_glu_w[j], s5_glu_b[j], w_out_even[j], lambda_init, cos, sin)
        else:
            m = fourier_mixer(x, w_out_odd[j])
        x = layer_norm(DEEPNORM_ALPHA * x + m, ln_mix_g[l], ln_mix_b[l])
        f = expert_choice_ffn(x, w_router[l], w_ff1[l], w_ff3[l], w_ff2[l])
        x = layer_norm(DEEPNORM_ALPHA * x + f, ln_ffn_g[l], ln_ffn_b[l])
    return x


def setup_inputs(seed: int = 0) -> dict:
    key = jax.random.key(seed)
    ks = jax.random.split(key, 32)
    nrm = lambda k, shape, s: jax.random.normal(k, shape, F32) * s
    G, P, H = SSM_GROUPS, SSM_STATE, SSM_GROUP
    a_im0 = jnp.pi * jnp.arange(P, dtype=F32)
    return {
        'x_prompt': nrm(ks[0], (BATCH, SEQ, D_MODEL), 1.0),
        'x_sample': nrm(ks[1], (DEC_BATCH, DEC_SEQ, D_MODEL), 1.0),
        'w_in': nrm(ks[2], (N_EVEN, D_MODEL, IN_WIDTH), D_MODEL ** -0.5),
        'lam_q1': nrm(ks[3], (N_EVEN, ATT_HEAD_DIM), 0.1),
        'lam_k1': nrm(ks[4], (N_EVEN, ATT_HEAD_DIM), 0.1),
        'lam_q2': nrm(ks[5], (N_EVEN, ATT_HEAD_DIM), 0.1),
        'lam_k2': nrm(ks[6], (N_EVEN, ATT_HEAD_DIM), 0.1),
        'subln_g': 1.0 + nrm(ks[7], (N_EVEN, ATT_V_DIM), 0.02),
        's5_a_re': -0.5 + nrm(ks[8], (N_EVEN, 2, G, P), 0.01),
        's5_a_im': a_im0 + nrm(ks[9], (N_EVEN, 2, G, P), 0.01),
        's5_log_dt': jax.random.uniform(ks[10], (N_EVEN, 2, G), F32, math.log(1e-3), math.log(1e-1)),
        's5_b_re': nrm(ks[11], (N_EVEN, 2, G, P, H), (2 * H) ** -0.5),
        's5_b_im': nrm(ks[12], (N_EVEN, 2, G, P, H), (2 * H) ** -0.5),
        's5_c_re': nrm(ks[13], (N_EVEN, 2, G, H, P), P ** -0.5),
        's5_c_im': nrm(ks[14], (N_EVEN, 2, G, H, P), P ** -0.5),
        's5_d': nrm(ks[15], (N_EVEN, G, H), 1.0),
        's5_glu_w': nrm(ks[16], (N_EVEN, SSM_WIDTH, SSM_WIDTH), SSM_WIDTH ** -0.5),
        's5_glu_b': nrm(ks[17], (N_EVEN, SSM_WIDTH), 0.02),
        'w_out_even': nrm(ks[18], (N_EVEN, D_MODEL, D_MODEL), D_MODEL ** -0.5 * DEEPNORM_BETA),
        'w_out_odd': nrm(ks[19], (N_ODD, D_MODEL, D_MODEL), D_MODEL ** -0.5 * DEEPNORM_BETA),
        'ln_mix_g': 1.0 + nrm(ks[20], (DEPTH, D_MODEL), 0.02),
        'ln_mix_b': nrm(ks[21], (DEPTH, D_MODEL), 0.02),
        'w_router': nrm(ks[22], (DEPTH, D_MODEL, N_EXPERTS), D_MODEL ** -0.5),
        'w_ff1': nrm(ks[23], (DEPTH, N_EXPERTS, D_MODEL, D_FF), D_MODEL ** -0.5),
        'w_ff3': nrm(ks[24], (DEPTH, N_EXPERTS, D_MODEL, D_FF), D_MODEL ** -0.5),
        'w_ff2': nrm(ks[25], (DEPTH, N_EXPERTS, D_FF, D_MODEL), D_FF ** -0.5 * DEEPNORM_BETA),
        'ln_ffn_g': 1.0 + nrm(ks[26], (DEPTH, D_MODEL), 0.02),
        'ln_ffn_b': nrm(ks[27], (DEPTH, D_MODEL), 0.02),
    }


def reference(x_prompt, x_sample, w_in, lam_q1, lam_k1, lam_q2, lam_k2, subln_g,
              s5_a_re, s5_a_im, s5_log_dt, s5_b_re, s5_b_im, s5_c_re, s5_c_im, s5_d, s5_glu_w, s5_glu_b,
              w_out_even, w_out_odd, ln_mix_g, ln_mix_b, w_router, w_ff1, w_ff3, w_ff2, ln_ffn_g, ln_ffn_b):
    y_prompt = trunk(x_prompt, w_in, lam_q1, lam_k1, lam_q2, lam_k2, subln_g,
                     s5_a_re, s5_a_im, s5_log_dt, s5_b_re, s5_b_im, s5_c_re, s5_c_im, s5_d, s5_glu_w, s5_glu_b,
                     w_out_even, w_out_odd, ln_mix_g, ln_mix_b, w_router, w_ff1, w_ff3, w_ff2, ln_ffn_g, ln_ffn_b)
    y_sample = trunk(x_sample, w_in, lam_q1, lam_k1, lam_q2, lam_k2, subln_g,
                     s5_a_re, s5_a_im, s5_log_dt, s5_b_re, s5_b_im, s5_c_re, s5_c_im, s5_d, s5_glu_w, s5_glu_b,
                     w_out_even, w_out_odd, ln_mix_g, ln_mix_b, w_router, w_ff1, w_ff3, w_ff2, ln_ffn_g, ln_ffn_b)
    return (y_prompt, y_sample)
```

```python
import math
import numpy as np
import ml_dtypes
import concourse.bass as bass
import concourse.mybir as mybir
from concourse.bass_utils import run_bass_kernel_spmd

F32 = mybir.dt.float32
BF16 = mybir.dt.bfloat16
I32 = mybir.dt.int32
ALU = mybir.AluOpType
AF = mybir.ActivationFunctionType
AX = mybir.AxisListType

D = 1024
DK = 8
NE = 16
DFF = 2816
FH = 1408
FK = 11
LN_EPS = 1e-5
ALPHA = 4.0 ** 0.25
NEG = -30000.0


class Buf:
    __slots__ = ("name", "w", "r")

    def __init__(self, name):
        self.name = name
        self.w = None
        self.r = []


class Eng:
    def __init__(self, k, name, e, sem):
        self.k = k
        self.name = name
        self.e = e
        self.sem = sem
        self.count = 0
        self.seen = {}


class K:
    def __init__(self, nc, stack):
        self.nc = nc
        self.stack = stack
        self.root = stack
        self.engs = {}
        for name, e in (("pe", nc.tensor), ("act", nc.scalar), ("dve", nc.vector),
                        ("pool", nc.gpsimd), ("sp", nc.sync)):
            sem = stack.enter_context(nc.semaphore("s_" + name))
            self.engs[name] = Eng(self, name, e, sem)
        self.ndma = 24
        self.dsems = [stack.enter_context(nc.semaphore("d%d" % i)) for i in range(self.ndma)]
        self.dcount = [0] * self.ndma
        self.dnext = 0
        self.pending = []
        self.same_engine_sync = False

    def _wait(self, eng, ev):
        if ev is None:
            return
        sem, val, src = ev
        if src == eng.name and src == "pe":
            return
        key = id(sem)
        if eng.seen.get(key, 0) >= val:
            return
        eng.e.wait_ge(sem, val)
        eng.seen[key] = val

    def deps(self, eng, reads, writes):
        for b in reads:
            self._wait(eng, b.w)
        for b in writes:
            self._wait(eng, b.w)
            for ev in b.r:
                self._wait(eng, ev)

    def _record(self, ev, reads, writes):
        for b in reads:
            b.r.append(ev)
            if len(b.r) > 12:
                b.r = b.r[-12:]
        for b in writes:
            b.w = ev
            b.r = []

    def op(self, en, reads, writes, fn):
        eng = self.engs[en]
        self.deps(eng, reads, writes)
        ins = fn(eng.e)
        eng.count += 1
        ins.then_inc(eng.sem, 1)
        self._record((eng.sem, eng.count, en), reads, writes)
        return ins

    def dma(self, qn, reads, writes, fn):
        eng = self.engs[qn]
        self.deps(eng, reads, writes)
        i = self.dnext
        self.dnext = (self.dnext + 1) % self.ndma
        sem = self.dsems[i]
        if self.dcount[i] > 0:
            self._wait(eng, (sem, self.dcount[i], "dma"))
        ins = fn(eng.e)
        self.dcount[i] += 16
        ins.then_inc(sem, 16)
        ev = (sem, self.dcount[i], "dma")
        self._record(ev, reads, writes)
        self.pending.append(ev)
        if len(self.pending) > 4 * self.ndma:
            self.pending = self.pending[-self.ndma:]
        return ins

    def barrier(self):
        evs = [(e.sem, e.count, e.name) for e in self.engs.values() if e.count > 0]
        evs += [(self.dsems[i], self.dcount[i], "dma") for i in range(self.ndma) if self.dcount[i] > 0]
        for eng in self.engs.values():
            for ev in evs:
                if ev[2] == eng.name:
                    continue
                self._wait(eng, ev)
        self.pending = []

    _uid = 0

    def rotate(self):
        self.barrier()
        for eng in self.engs.values():
            if eng.count > 0:
                eng.sem = self.root.enter_context(self.nc.semaphore("s_%s_%d" % (eng.name, K._uid)))
                K._uid += 1
                eng.count = 0

    def sb(self, name, shape, dt):
        K._uid += 1
        return self.stack.enter_context(self.nc.sbuf_tensor("%s_%d" % (name, K._uid), shape, dt))

    def ps(self, name, shape, dt=F32):
        K._uid += 1
        return self.stack.enter_context(self.nc.psum_tensor("%s_%d" % (name, K._uid), shape, dt))


class Cfg:
    def __init__(self, nseq, sl, rs):
        self.NSEQ = nseq
        self.SL = sl
        self.RS = rs
        self.NT = nseq * sl
        self.NTILE = self.NT // 128
        self.CAP = max(1, 2 * self.NT // NE)
        self.NCH = sl // 128
        self.QT = min(512, sl)
        self.NQT = sl // self.QT
        self.TS = min(512, self.CAP)
        self.N2E = sl // 128
        self.N2 = rs // 128


def const_tables(cfg):
    SL, RS, NSEQ, NT = cfg.SL, cfg.RS, cfg.NSEQ, cfg.NT
    t = {}
    inv = (1.0 / (np.float32(10000.0) ** (np.arange(0, 64, 2, dtype=np.float32) / np.float32(64)))).astype(np.float32)
    pos = (np.arange(SL) % RS).astype(np.float32)
    ang = (pos[:, None] * inv[None, :]).astype(np.float32)
    ang = np.concatenate([ang, ang], -1)
    cosT = np.cos(ang).astype(np.float32).T
    sinT = np.sin(ang).astype(np.float32).T
    t["ropec"] = np.ascontiguousarray(np.concatenate([cosT, cosT], 0))
    t["ropes"] = np.ascontiguousarray(np.concatenate([sinT, sinT], 0))
    nkc, nqt = cfg.NCH, cfg.NQT
    mb = np.zeros((nkc, nqt), np.float32)
    for kc in range(nkc):
        for qt in range(nqt):
            if (kc * 128) // RS != (qt * cfg.QT) // RS:
                mb[kc, qt] = NEG
    t["maskb"] = np.ascontiguousarray(np.broadcast_to(mb.reshape(1, -1), (128, nkc * nqt))).astype(np.float32)
    cf = np.ones(nkc, np.float32)
    cb = np.ones(nkc, np.float32)
    for c in range(nkc):
        if ((c + 1) * 128) % RS == 0:
            cf[c] = 0.0
        if (c * 128) % RS == 0:
            cb[c] = 0.0
    t["cmask"] = np.ascontiguousarray(np.broadcast_to(np.concatenate([cf, cb]).reshape(1, -1), (128, 2 * nkc))).astype(np.float32)
    i = np.arange(128)
    t["tri"] = (i[:, None] <= i[None, :]).astype(np.float32)
    t["trib"] = (i[:, None] >= i[None, :]).astype(np.float32)
    t["ustrict"] = (i[:, None] < i[None, :]).astype(np.float32)
    t["ident"] = np.eye(128, dtype=np.float32)
    gm = np.zeros((128, 4), np.float32)
    for r in range(128):
        gm[r, (r % 64) // 16] = 1.0
    t["gmask"] = gm
    tok0 = (np.arange(cfg.NTILE)[None, :] * 128 + np.arange(128)[:, None]).astype(np.float32)
    t["tok0"] = tok0
    kper = 128 // cfg.N2E if cfg.N2E <= 128 else 1
    tok1 = np.zeros((128, cfg.NTILE), np.float32)
    ntile_seq = cfg.NCH
    for s in range(NSEQ):
        for q in range(ntile_seq):
            for p in range(128):
                k1 = q * kper + p // cfg.N2E
                jj = p % cfg.N2E
                tok1[p, s * ntile_seq + q] = s * SL + k1 + 128 * jj
    t["tok1"] = tok1
    cfg.KPER = kper
    N2, N2E = cfg.N2, cfg.N2E
    fidx = np.zeros((128, NSEQ * N2E), np.int32)
    twr = np.zeros((128, N2E), np.float32)
    twi = np.zeros((128, N2E), np.float32)
    for j in range(N2E):
        sh, t2 = j // N2, j % N2
        for s in range(NSEQ):
            fidx[:, s * N2E + j] = s * SL + sh * RS + N2 * np.arange(128) + t2
        a = 2.0 * np.pi * t2 * np.arange(128) / RS
        twr[:, j] = np.cos(a)
        twi[:, j] = -np.sin(a)
    t["fidx"] = fidx
    t["twr"] = twr
    t["twi"] = twi
    a1 = 2.0 * np.pi * np.outer(np.arange(128), np.arange(128)) / 128.0
    t["c1"] = (np.cos(a1) / np.sqrt(128.0)).astype(np.float32)
    t["s1"] = (np.sin(a1) / np.sqrt(128.0)).astype(np.float32)
    c2 = np.zeros((N2E, N2E), np.float64)
    s2 = np.zeros((N2E, N2E), np.float64)
    for j in range(N2E):
        for jp in range(N2E):
            if j // N2 == jp // N2:
                a = 2.0 * np.pi * (j % N2) * (jp % N2) / N2
                c2[j, jp] = np.cos(a) / np.sqrt(N2)
                s2[j, jp] = np.sin(a) / np.sqrt(N2)
    c2p = np.zeros((N2E, kper, 128), np.float32)
    s2p = np.zeros((N2E, kper, 128), np.float32)
    for v in range(kper):
        c2p[:, v, v * N2E:(v + 1) * N2E] = c2
        s2p[:, v, v * N2E:(v + 1) * N2E] = s2
    t["c2p"] = c2p.reshape(N2E, kper * 128)
    t["s2p"] = s2p.reshape(N2E, kper * 128)
    ac = 2.0 * np.pi * np.outer(np.arange(256), np.arange(256)) / 256.0
    cc = (np.cos(ac) / 16.0).astype(np.float32)
    sc = (-np.sin(ac) / 16.0).astype(np.float32)
    t["cc2"] = np.ascontiguousarray(cc.reshape(2, 128, 2, 128).transpose(1, 0, 2, 3)).reshape(128, 512)
    t["sc2"] = np.ascontiguousarray(sc.reshape(2, 128, 2, 128).transpose(1, 0, 2, 3)).reshape(128, 512)
    return t


from contextlib import ExitStack


PHASE_GROUPS = (("A1",), ("A2",), ("A3",), ("A4", "B0"), ("C0",), ("D0",), ("F", "B1"), ("C1",), ("D1",))


class Prog:
    def __init__(self, cfg, dbg=()):
        self.cfg = cfg
        self.dbg = set(dbg)
        self.nc = bass.Bass("TRN2", target_bir_lowering=False)
        self.in_names = []
        self.out_names = []

    def din(self, name, shape, dt=F32):
        self.in_names.append(name)
        return self.nc.dram_tensor(name, list(shape), dt, kind="ExternalInput")

    def dscr(self, name, shape, dt):
        if name in self.dbg:
            self.out_names.append(name)
            return self.nc.dram_tensor(name, list(shape), dt, kind="ExternalOutput")
        return self.nc.dram_tensor(name, list(shape), dt)

    def dout(self, name, shape, dt=F32):
        self.out_names.append(name)
        return self.nc.dram_tensor(name, list(shape), dt, kind="ExternalOutput")


def bcast_rows(ap2d, nparts):
    return ap2d.partition_broadcast(nparts)


def build(cfg, dbg=(), stop_after=None, skip=()):
    P = Prog(cfg, dbg)
    P.skip = set(skip)
    nc = P.nc
    NT, SL, NSEQ, NCH = cfg.NT, cfg.SL, cfg.NSEQ, cfg.NCH
    x_in = P.din("x", [NT, D])
    w_in = P.din("w_in", [D, 2048])
    lamv = P.din("lamv", [4, 64])
    subln_g = P.din("subln_g", [128, 1])
    s5_a_re = P.din("s5_a_re", [2, 32, 64])
    s5_a_im = P.din("s5_a_im", [2, 32, 64])
    s5_log_dt = P.din("s5_log_dt", [2, 32, 1])
    s5_b_re = P.din("s5_b_re", [2, 32, 64, 16])
    s5_b_im = P.din("s5_b_im", [2, 32, 64, 16])
    s5_c_re = P.din("s5_c_re", [2, 512, 64])
    s5_c_im = P.din("s5_c_im", [2, 512, 64])
    s5_d = P.din("s5_d", [128, 4])
    s5_glu_w = P.din("s5_glu_w", [512, 512])
    s5_glu_b = P.din("s5_glu_b", [128, 4])
    w_out_even = P.din("w_out_even", [D, D])
    w_out_odd = P.din("w_out_odd", [D, D])
    ln_mix_g = P.din("ln_mix_g", [2, D])
    ln_mix_b = P.din("ln_mix_b", [2, D])
    w_router = P.din("w_router", [2, D, NE])
    if stop_after in ("A1", "A2", "A3", "A4", "B0"):
        w_ff1 = w_ff3 = w_ff2 = None
    else:
        w_ff1 = P.din("w_ff1", [2, NE, D, DFF])
        w_ff3 = P.din("w_ff3", [2, NE, D, DFF])
        w_ff2 = P.din("w_ff2", [2, NE, DFF, D])
    ln_ffn_g = P.din("ln_ffn_g", [2, D])
    ln_ffn_b = P.din("ln_ffn_b", [2, D])
    T = {}
    tabs = const_tables(cfg)
    for name, arr in tabs.items():
        T[name] = P.din("t_" + name, arr.shape, I32 if arr.dtype == np.int32 else F32)
    P.tabs = tabs
    y_out = P.dout("y", [NT, D])
    QKT = P.dscr("QKT", [8, 128, NT], BF16)
    VV = P.dscr("VV", [NT, 512], BF16)
    UT = P.dscr("UT", [4, 128, NT], BF16)
    ATT = P.dscr("ATT", [4, 128, NT], BF16)
    SSM = P.dscr("SSM", [4, 128, NT], BF16)
    YB = P.dscr("YB", [4, 128, NT], F32)
    XLN = P.dscr("XLN", [NT, D], F32)
    XBF = P.dscr("XBF", [NT + 128, D], BF16)
    YY = P.dscr("YY", [NT, D], F32)
    X1 = P.dscr("X1", [NT, D], F32)
    IDX = [P.dscr("IDX%d" % i, [cfg.CAP + 128, 2], F32) for i in range(NE)]
    AFS = P.dscr("AFS", [128, cfg.N2E, 2, D], BF16)
    AFFD = P.dscr("AFFD", [128, cfg.NTILE * NE], F32)

    with ExitStack() as root:
        k = K(nc, root)
        e_ = k.engs

        ident_f = k.sb("ident_f", [128, 128], F32)
        ident_b = k.sb("ident_b", [128, 128], BF16)
        ones_b = k.sb("ones_b", [128, 128], BF16)
        ones_f = k.sb("ones_f", [128, 128], F32)
        B_const = Buf("const")
        k.dma("sp", [], [B_const], lambda e: e.dma_start(out=ident_f[:, :], in_=T["ident"][:, :]))
        k.op("dve", [B_const], [B_const], lambda e: e.tensor_copy(out=ident_b[:, :], in_=ident_f[:, :]))
        k.op("dve", [], [B_const], lambda e: e.memset(ones_b[:, :], 1.0))
        k.op("dve", [], [B_const], lambda e: e.memset(ones_f[:, :], 1.0))

        bnd_reg = nc.gpsimd.alloc_register("bnd")
        nc.gpsimd.reg_mov(bnd_reg, cfg.CAP - 1)
        L = dict(locals())
        for group in PHASE_GROUPS:
            with ExitStack() as gs:
                k.stack = gs
                if group[0] in ("A4", "F"):
                    L["aff_sb"] = k.sb("aff_sb", [128, cfg.NTILE, NE], F32)
                    L["Baff"] = Buf("aff")
                for name in group:
                    fn = globals().get("phase_" + name)
                    if name in P.skip or fn is None:
                        continue
                    fn(P, k, L)
                    k.rotate()
                    if stop_after == name:
                        k.barrier()
                        k.stack = root
                        return P
            k.stack = root
        k.barrier()
    return P


class Ring:
    def __init__(self, k, name, shape, dt, n, psum=False):
        self.t = []
        self.b = []
        for i in range(n):
            nm = "%s%d" % (name, i)
            self.t.append(k.ps(nm, shape, dt) if psum else k.sb(nm, shape, dt))
            self.b.append(Buf(nm))
        self.i = 0
        self.n = n

    def next(self):
        i = self.i
        self.i = (self.i + 1) % self.n
        return self.t[i], self.b[i]


def phase_A1(P, k, L):
    cfg = P.cfg
    x_in, w_in, QKT, VV, UT, T = L["x_in"], L["w_in"], L["QKT"], L["VV"], L["UT"], L["T"]
    ident_b, root = L["ident_b"], L["root"]
    NT, SL = cfg.NT, cfg.SL
    prev = k.stack
    with ExitStack() as st:
        k.stack = st
        wall = k.sb("wall", [128, DK, 3072], BF16)
        Bw = Buf("wall")
        w_v = w_in.ap().rearrange("(dk p) f -> p dk f", p=128)
        k.dma("pool", [], [Bw], lambda e: e.dma_start(out=wall[:, :, 0:1024], in_=w_v[:, :, 0:1024]))
        k.dma("pool", [], [Bw], lambda e: e.dma_start(out=wall[:, :, 2048:3072], in_=w_v[:, :, 1024:2048]))
        for dk in range(DK):
            dst = wall[:, dk, 1024:2048].rearrange("p (b h i) -> p b h i", h=2, i=32)
            src = w_v[:, dk, 0:1024].rearrange("p (b h i) -> p b h i", h=2, i=32)
            k.dma("pool", [], [Bw], lambda e, d=dst, s=src: e.dma_start(out=d[:, :, 0, :], in_=s[:, :, 1, :]))
            k.dma("pool", [], [Bw], lambda e, d=dst, s=src: e.dma_start(out=d[:, :, 1, :], in_=s[:, :, 0, :]))
        for dk in range(DK):
            v = wall[:, dk, 1024:2048].rearrange("p (b h i) -> p b h i", h=2, i=32)[:, :, 0, :]
            k.op("dve", [Bw], [Bw], lambda e, v=v: e.tensor_scalar(out=v, in0=v, scalar1=-1.0, scalar2=None, op0=ALU.mult))

        xb = Ring(k, "a1_xb", [128, 4, D], BF16, 2)
        xT = Ring(k, "a1_xT", [128, DK, 512], BF16, 2)
        cs = Ring(k, "a1_cs", [128, 2, 512], F32, 2)
        t12 = Ring(k, "a1_t12", [128, 2, 512], F32, 2)
        qko = Ring(k, "a1_qko", [128, 512], BF16, 3)
        vo = Ring(k, "a1_vo", [128, 4, 512], BF16, 2)
        uo = Ring(k, "a1_uo", [128, 512], BF16, 3)
        pst = Ring(k, "a1_pst", [128, 512], BF16, 2, psum=True)
        psA = Ring(k, "a1_psA", [128, 512], F32, 2, psum=True)
        psB = Ring(k, "a1_psB", [128, 512], F32, 2, psum=True)
        psC = Ring(k, "a1_psC", [128, 512], F32, 2, psum=True)

        ntile = NT // 512
        for tt in range(ntile):
            t0 = tt * 512
            p0 = t0 % SL
            xbt, xbb = xb.next()
            k.dma("pool", [], [xbb], lambda e: e.dma_start(
                out=xbt[:, :, :], in_=x_in[t0:t0 + 512, :].rearrange("(j p) d -> p j d", p=128)))
            cst, csb = cs.next()
            k.dma("sp", [], [csb], lambda e: e.dma_start(out=cst[:, 0, :], in_=T["ropec"][:, p0:p0 + 512]))
            k.dma("sp", [], [csb], lambda e: e.dma_start(out=cst[:, 1, :], in_=T["ropes"][:, p0:p0 + 512]))
            xTt, xTb = xT.next()
            for dk in range(DK):
                pt, ptb = pst.next()

                def tr(e, pt=pt, dk=dk):
                    for j in range(4):
                        ins = e.transpose(out=pt[:, j * 128:(j + 1) * 128], in_=xbt[:, j, dk * 128:(dk + 1) * 128],
                                          identity=ident_b[:, :])
                    return ins
                k.op("pe", [xbb], [ptb], tr)
                if dk % 2 == 0:
                    k.op("act", [ptb], [xTb], lambda e, pt=pt, dk=dk: e.copy(out=xTt[:, dk, :], in_=pt[:, :]))
                else:
                    k.op("dve", [ptb], [xTb], lambda e, pt=pt, dk=dk: e.tensor_copy(out=xTt[:, dk, :], in_=pt[:, :]))
            for fc in range(8):
                pa, pab = psA.next()
                pb, pbb = psB.next()

                def mmA(e, pa=pa, fc=fc):
                    for dk in range(DK):
                        ins = e.matmul(pa[:, :], wall[:, dk, fc * 128:(fc + 1) * 128], xTt[:, dk, :],
                                       start=(dk == 0), stop=(dk == DK - 1))
                    return ins

                def mmB(e, pb=pb, fc=fc):
                    for dk in range(DK):
                        ins = e.matmul(pb[:, :], wall[:, dk, 1024 + fc * 128:1024 + (fc + 1) * 128], xTt[:, dk, :],
                                       start=(dk == 0), stop=(dk == DK - 1))
                    return ins
                k.op("pe", [Bw, xTb], [pab], mmA)
                k.op("pe", [Bw, xTb], [pbb], mmB)
                tt_, ttb = t12.next()
                k.op("dve", [pab, csb], [ttb], lambda e, pa=pa, tt_=tt_: e.tensor_tensor(
                    out=tt_[:, 0, :], in0=pa[:, :], in1=cst[:, 0, :], op=ALU.mult))
                k.op("dve", [pbb, csb], [ttb], lambda e, pb=pb, tt_=tt_: e.tensor_tensor(
                    out=tt_[:, 1, :], in0=pb[:, :], in1=cst[:, 1, :], op=ALU.mult))
                qo, qob = qko.next()
                k.op("pool", [ttb], [qob], lambda e, qo=qo, tt_=tt_: e.tensor_tensor(
                    out=qo[:, :], in0=tt_[:, 0, :], in1=tt_[:, 1, :], op=ALU.add))
                k.dma("sp", [qob], [], lambda e, qo=qo, fc=fc: e.dma_start(out=QKT[fc, :, t0:t0 + 512], in_=qo[:, :]))
            vt, vb = vo.next()
            for j in range(4):
                pc, pcb = psC.next()

                def mmV(e, pc=pc, j=j):
                    for dk in range(DK):
                        ins = e.matmul(pc[:, :], xTt[:, dk, j * 128:(j + 1) * 128], wall[:, dk, 2048:2560],
                                       start=(dk == 0), stop=(dk == DK - 1))
                    return ins
                k.op("pe", [Bw, xTb], [pcb], mmV)
                k.op("act", [pcb], [vb], lambda e, pc=pc, j=j: e.copy(out=vt[:, j, :], in_=pc[:, :]))
            k.dma("sp", [vb], [], lambda e: e.dma_start(
                out=VV[t0:t0 + 512, :].rearrange("(j p) f -> p j f", p=128), in_=vt[:, :, :]))
            for c in range(4):
                pc, pcb = psC.next()

                def mmU(e, pc=pc, c=c):
                    for dk in range(DK):
                        ins = e.matmul(pc[:, :], wall[:, dk, 2560 + c * 128:2560 + (c + 1) * 128], xTt[:, dk, :],
                                       start=(dk == 0), stop=(dk == DK - 1))
                    return ins
                k.op("pe", [Bw, xTb], [pcb], mmU)
                ut, ub = uo.next()
                k.op("act", [pcb], [ub], lambda e, pc=pc, ut=ut: e.copy(out=ut[:, :], in_=pc[:, :]))
                k.dma("sp", [ub], [], lambda e, ut=ut, c=c: e.dma_start(out=UT[c, :, t0:t0 + 512], in_=ut[:, :]))
        k.barrier()
    k.stack = prev


def core_inputs(cfg, P, x_flat, inp):
    m = {}
    m["x"] = np.ascontiguousarray(x_flat, dtype=np.float32)
    m["w_in"] = inp["w_in"][0]
    m["lamv"] = np.stack([inp["lam_q1"][0], inp["lam_k1"][0], inp["lam_q2"][0], inp["lam_k2"][0]], 0)
    m["subln_g"] = inp["subln_g"][0].reshape(128, 1)
    m["s5_a_re"] = inp["s5_a_re"][0]
    m["s5_a_im"] = inp["s5_a_im"][0]
    m["s5_log_dt"] = inp["s5_log_dt"][0].reshape(2, 32, 1)
    m["s5_b_re"] = inp["s5_b_re"][0]
    m["s5_b_im"] = inp["s5_b_im"][0]
    m["s5_c_re"] = inp["s5_c_re"][0].reshape(2, 512, 64)
    m["s5_c_im"] = inp["s5_c_im"][0].reshape(2, 512, 64)
    m["s5_d"] = inp["s5_d"][0].reshape(4, 128).T
    m["s5_glu_w"] = inp["s5_glu_w"][0]
    m["s5_glu_b"] = inp["s5_glu_b"][0].reshape(4, 128).T
    m["w_out_even"] = inp["w_out_even"][0]
    m["w_out_odd"] = inp["w_out_odd"][0]
    for n in ("ln_mix_g", "ln_mix_b", "w_router", "w_ff1", "w_ff3", "w_ff2", "ln_ffn_g", "ln_ffn_b"):
        m[n] = inp[n]
    for n, arr in P.tabs.items():
        m["t_" + n] = arr
    out = {}
    for n in P.in_names:
        out[n] = np.ascontiguousarray(m[n])
    return out


def phase_A2(P, k, L):
    cfg = P.cfg
    QKT, VV, ATT, T = L["QKT"], L["VV"], L["ATT"], L["T"]
    lamv, subln_g = L["lamv"], L["subln_g"]
    ones_b, root = L["ones_b"], L["root"]
    SL, NSEQ, NCH, QT, NQT = cfg.SL, cfg.NSEQ, cfg.NCH, cfg.QT, cfg.NQT
    lambda_init = 0.8 - 0.6 * math.exp(0.0)
    prev = k.stack
    with ExitStack() as st:
        k.stack = st
        lv = k.sb("a2_lv", [128, 4, 64], F32)
        Bs = Buf("a2_scal")
        k.dma("sp", [], [Bs], lambda e: e.dma_start(
            out=lv[:, :, :].rearrange("p a b -> p (a b)"),
            in_=lamv.ap().rearrange("a b -> (a b)").partition_broadcast(128)))
        pr = k.sb("a2_pr", [128, 2, 64], F32)
        sm = k.sb("a2_sm", [128, 4], F32)
        k.op("dve", [Bs], [Bs], lambda e: e.tensor_tensor(out=pr[:, 0, :], in0=lv[:, 0, :], in1=lv[:, 1, :], op=ALU.mult))
        k.op("dve", [Bs], [Bs], lambda e: e.tensor_tensor(out=pr[:, 1, :], in0=lv[:, 2, :], in1=lv[:, 3, :], op=ALU.mult))
        k.op("dve", [Bs], [Bs], lambda e: e.reduce_sum(out=sm[:, 0:1], in_=pr[:, 0, :], axis=AX.X))
        k.op("dve", [Bs], [Bs], lambda e: e.reduce_sum(out=sm[:, 1:2], in_=pr[:, 1, :], axis=AX.X))
        k.op("act", [Bs], [Bs], lambda e: e.activation(out=sm[:, 2:4], in_=sm[:, 0:2], func=AF.Exp))
        nlam = k.sb("a2_nlam", [128, 1], F32)
        k.op("dve", [Bs], [Bs], lambda e: e.tensor_tensor(out=nlam[:, :], in0=sm[:, 3:4], in1=sm[:, 2:3], op=ALU.subtract))
        k.op("dve", [Bs], [Bs], lambda e: e.tensor_scalar(out=nlam[:, :], in0=nlam[:, :], scalar1=-lambda_init, scalar2=None, op0=ALU.add))
        gsc = k.sb("a2_gsc", [128, 1], F32)
        k.dma("sp", [], [Bs], lambda e: e.dma_start(out=gsc[:, :], in_=subln_g[:, :]))
        k.op("dve", [Bs], [Bs], lambda e: e.tensor_scalar(out=gsc[:, :], in0=gsc[:, :], scalar1=1.0 - lambda_init, scalar2=None, op0=ALU.mult))
        mb = k.sb("a2_mb", [128, NCH * NQT], F32)
        k.dma("sp", [], [Bs], lambda e: e.dma_start(out=mb[:, :], in_=T["maskb"][:, :]))
        epsb = k.sb("a2_eps", [128, 1], F32)
        k.op("dve", [], [Bs], lambda e: e.memset(epsb[:, :], LN_EPS))

        qT = Ring(k, "a2_qT", [128, SL], BF16, 2)
        kT = Ring(k, "a2_kT", [128, SL], BF16, 2)
        vh = Ring(k, "a2_vh", [128, NCH, 128], BF16, 2)
        ps_s = Ring(k, "a2_pss", [128, 2, 512], F32, 2, psum=True)
        ps_o1 = k.ps("a2_o1", [128, 512]); Bo1 = Buf("o1")
        ps_o2 = k.ps("a2_o2", [128, 512]); Bo2 = Buf("o2")
        ps_z1 = k.ps("a2_z1", [128, 512]); Bz1 = Buf("z1")
        ps_z2 = k.ps("a2_z2", [128, 512]); Bz2 = Buf("z2")
        et = Ring(k, "a2_e", [128, 2, 512], BF16, 3)
        r12 = k.sb("a2_r12", [128, 2, 512], F32); Br = Buf("r12")
        ab = k.sb("a2_ab", [128, 2, 512], F32); Bab = Buf("ab")
        osb = k.sb("a2_o", [128, 512], F32); Bosb = Buf("osb")
        sq = k.sb("a2_sq", [128, 512], BF16); Bsq = Buf("sq")
        rstd = k.sb("a2_rstd", [128, 512], F32); Brs = Buf("rstd")
        ao = Ring(k, "a2_ao", [128, 512], BF16, 2)

        for s in range(NSEQ):
            for h in range(4):
                base = s * SL
                qt_, qb = qT.next()
                kt_, kb = kT.next()
                vt_, vb = vh.next()
                k.dma("sp", [], [qb], lambda e: e.dma_start(out=qt_[:, :], in_=QKT[h, :, base:base + SL]))
                k.dma("sp", [], [kb], lambda e: e.dma_start(out=kt_[:, :], in_=QKT[4 + h, :, base:base + SL]))
                nsp = 4 if NCH >= 16 else 1
                cpp = NCH // nsp
                for sp_ in range(nsp):
                    k.dma("sp", [], [vb], lambda e, sp_=sp_: e.dma_start(
                        out=vt_[:, sp_ * cpp:(sp_ + 1) * cpp, :],
                        in_=VV[base + sp_ * cpp * 128:base + (sp_ + 1) * cpp * 128, h * 128:(h + 1) * 128].rearrange("(c p) f -> p c f", p=128)))
                for qt in range(NQT):
                    q0 = qt * QT
                    pend = None
                    for kc in range(NCH + 1):
                        if kc < NCH:
                            pss, psb = ps_s.next()

                            def mmS(e, pss=pss, kc=kc):
                                e.matmul(pss[:, 0, 0:QT], kt_[0:64, kc * 128:(kc + 1) * 128], qt_[0:64, q0:q0 + QT],
                                         start=True, stop=True)
                                return e.matmul(pss[:, 1, 0:QT], kt_[64:128, kc * 128:(kc + 1) * 128],
                                                qt_[64:128, q0:q0 + QT], start=True, stop=True)
                            k.op("pe", [qb, kb], [psb], mmS)
                            ee, eb = et.next()
                            mcol = mb[:, kc * NQT + qt:kc * NQT + qt + 1]
                            k.op("act", [psb, Bs], [eb], lambda e, pss=pss, ee=ee, mcol=mcol: e.activation(
                                out=ee[:, :, 0:QT], in_=pss[:, :, 0:QT], func=AF.Exp, bias=mcol, scale=0.125))
                            cur = (ee, eb, kc)
                        else:
                            cur = None
                        if pend is not None:
                            pe_, peb, pkc = pend

                            def mmPV(e, pe_=pe_, pkc=pkc):
                                st_, sp_ = (pkc == 0), (pkc == NCH - 1)
                                e.matmul(ps_o1[:, 0:QT], vt_[:, pkc, :], pe_[:, 0, 0:QT], start=st_, stop=sp_)
                                e.matmul(ps_z1[:, 0:QT], ones_b[:, :], pe_[:, 0, 0:QT], start=st_, stop=sp_)
                                e.matmul(ps_o2[:, 0:QT], vt_[:, pkc, :], pe_[:, 1, 0:QT], start=st_, stop=sp_)
                                return e.matmul(ps_z2[:, 0:QT], ones_b[:, :], pe_[:, 1, 0:QT], start=st_, stop=sp_)
                            k.op("pe", [vb, peb], [Bo1, Bo2, Bz1, Bz2], mmPV)
                        pend = cur
                    k.op("dve", [Bz1], [Br], lambda e: e.reciprocal(out=r12[:, 0, 0:QT], in_=ps_z1[:, 0:QT]))
                    k.op("dve", [Bz2], [Br], lambda e: e.reciprocal(out=r12[:, 1, 0:QT], in_=ps_z2[:, 0:QT]))
                    k.op("dve", [Bo1, Br], [Bab], lambda e: e.tensor_tensor(out=ab[:, 0, 0:QT], in0=ps_o1[:, 0:QT], in1=r12[:, 0, 0:QT], op=ALU.mult))
                    k.op("dve", [Bo2, Br], [Bab], lambda e: e.tensor_tensor(out=ab[:, 1, 0:QT], in0=ps_o2[:, 0:QT], in1=r12[:, 1, 0:QT], op=ALU.mult))
                    k.op("dve", [Bab, Bs], [Bosb], lambda e: e.scalar_tensor_tensor(
                        out=osb[:, 0:QT], in0=ab[:, 1, 0:QT], scalar=nlam[:, 0:1], in1=ab[:, 0, 0:QT], op0=ALU.mult, op1=ALU.add))
                    k.op("pool", [Bosb], [Bsq], lambda e: e.tensor_tensor(out=sq[:, 0:QT], in0=osb[:, 0:QT], in1=osb[:, 0:QT], op=ALU.mult))
                    k.op("pe", [Bsq], [Bz1], lambda e: e.matmul(ps_z1[:, 0:QT], ones_b[:, :], sq[:, 0:QT], start=True, stop=True))
                    k.op("act", [Bz1, Bs], [Brs], lambda e: e.activation(out=rstd[:, 0:QT], in_=ps_z1[:, 0:QT], func=AF.Sqrt,
                                                                         bias=epsb[:, 0:1], scale=1.0 / 128.0))
                    k.op("dve", [Brs], [Brs], lambda e: e.reciprocal(out=rstd[:, 0:QT], in_=rstd[:, 0:QT]))
                    aot, aob = ao.next()
                    k.op("dve", [Bosb, Brs, Bs], [aob], lambda e: e.scalar_tensor_tensor(
                        out=aot[:, 0:QT], in0=osb[:, 0:QT], scalar=gsc[:, 0:1], in1=rstd[:, 0:QT], op0=ALU.mult, op1=ALU.mult))
                    import os
                    dm = os.environ.get("A2DBG", "")
                    if dm == "a":
                        k.op("dve", [Bab], [aob], lambda e: e.tensor_copy(out=aot[:, 0:QT], in_=ab[:, 0, 0:QT]))
                    elif dm == "b":
                        k.op("dve", [Bab], [aob], lambda e: e.tensor_copy(out=aot[:, 0:QT], in_=ab[:, 1, 0:QT]))
                    elif dm == "o":
                        k.op("dve", [Bosb], [aob], lambda e: e.tensor_copy(out=aot[:, 0:QT], in_=osb[:, 0:QT]))
                    elif dm == "n":
                        k.op("dve", [Bosb, Bs], [aob], lambda e: e.tensor_scalar(out=aot[:, 0:QT], in0=osb[:, 0:QT], scalar1=0.0, scalar2=nlam[:, 0:1], op0=ALU.mult, op1=ALU.add))
                    elif dm == "s":
                        k.op("dve", [Bosb, Bs], [aob], lambda e: e.tensor_scalar(out=aot[:, 0:QT], in0=osb[:, 0:QT], scalar1=0.0, scalar2=sm[:, int(os.environ.get("SMI", "0")):int(os.environ.get("SMI", "0")) + 1], op0=ALU.mult, op1=ALU.add))
                    elif dm == "r":
                        k.op("dve", [Brs], [aob], lambda e: e.tensor_copy(out=aot[:, 0:QT], in_=rstd[:, 0:QT]))
                    k.dma("sp", [aob], [], lambda e: e.dma_start(out=ATT[h, :, base + q0:base + q0 + QT], in_=aot[:, 0:QT]))
        k.barrier()
    k.stack = prev


def _cmul_bcast(k, eng, Bt, outr, outi, ar, ai, sr, si, tmp, shape):
    t1, t2, t3, t4 = tmp
    k.op(eng, [Bt], [Bt], lambda e: e.tensor_tensor(out=t1, in0=ar, in1=sr, op=ALU.mult))
    k.op(eng, [Bt], [Bt], lambda e: e.tensor_tensor(out=t2, in0=ai, in1=si, op=ALU.mult))
    k.op(eng, [Bt], [Bt], lambda e: e.tensor_tensor(out=t3, in0=ar, in1=si, op=ALU.mult))
    k.op(eng, [Bt], [Bt], lambda e: e.tensor_tensor(out=t4, in0=ai, in1=sr, op=ALU.mult))
    k.op(eng, [Bt], [Bt], lambda e: e.tensor_tensor(out=outr, in0=t1, in1=t2, op=ALU.subtract))
    k.op(eng, [Bt], [Bt], lambda e: e.tensor_tensor(out=outi, in0=t3, in1=t4, op=ALU.add))


def phase_A3(P, k, L):
    cfg = P.cfg
    UT, SSM, YB, T = L["UT"], L["SSM"], L["YB"], L["T"]
    ident_f, ones_b, root = L["ident_f"], L["ones_b"], L["root"]
    SL, NSEQ, NCH = cfg.SL, cfg.NSEQ, cfg.NCH
    prev = k.stack
    with ExitStack() as st:
        k.stack = st
        Bt = Buf("a3_setup")
        pst = k.ps("a3_pst", [128, 128], F32); Bpst = Buf("a3_pst")
        Ppos = [[k.sb("a3_pp%d%d" % (d, r), [128, 16, 128], F32) for r in range(2)] for d in range(2)]
        Wneg = [[k.sb("a3_wn%d%d" % (d, r), [128, 16, 128], F32) for r in range(2)] for d in range(2)]
        Bblk = [[k.sb("a3_bb%d%d" % (d, c), [128, 512], BF16) for c in range(4)] for d in range(2)]
        Cpad = [k.sb("a3_cp%d" % d, [128, 16, 2, 128], BF16) for d in range(2)]
        dskip = k.sb("a3_dsk", [128, 4], F32)
        glub = k.sb("a3_glub", [128, 4], F32)
        gluw = k.sb("a3_gluw", [128, 4, 512], BF16)
        tri = [k.sb("a3_tri%d" % d, [128, 128], BF16) for d in range(2)]
        cm = k.sb("a3_cm", [128, 2 * NCH], F32)
        k.dma("sp", [], [Bt], lambda e: e.dma_start(out=dskip[:, :], in_=L["s5_d"][:, :]))
        k.dma("sp", [], [Bt], lambda e: e.dma_start(out=glub[:, :], in_=L["s5_glu_b"][:, :]))
        k.dma("pool", [], [Bt], lambda e: e.dma_start(out=gluw[:, :, :], in_=L["s5_glu_w"].ap().rearrange("(c p) o -> p c o", p=128)))
        k.dma("pool", [], [Bt], lambda e: e.dma_start(out=tri[0][:, :], in_=T["tri"][:, :]))
        k.dma("pool", [], [Bt], lambda e: e.dma_start(out=tri[1][:, :], in_=T["trib"][:, :]))
        k.dma("sp", [], [Bt], lambda e: e.dma_start(out=cm[:, :], in_=T["cmask"][:, :]))
        with ExitStack() as st2:
            k.stack = st2
            tmpA = k.sb("a3_tmpA", [128, 4, 16, 128], F32)
            Pneg = [[k.sb("a3_pn%d%d" % (d, r), [128, 16, 128], F32) for r in range(2)] for d in range(2)]
            for d in range(2):
                are = k.sb("a3_are%d" % d, [16, 128], F32)
                aim = k.sb("a3_aim%d" % d, [16, 128], F32)
                ldt = k.sb("a3_ldt%d" % d, [16, 2], F32)
                k.dma("sp", [], [Bt], lambda e: e.dma_start(out=are[:, :], in_=L["s5_a_re"][d].rearrange("(c g) p -> c (g p)", g=2)))
                k.dma("sp", [], [Bt], lambda e: e.dma_start(out=aim[:, :], in_=L["s5_a_im"][d].rearrange("(c g) p -> c (g p)", g=2)))
                k.dma("sp", [], [Bt], lambda e: e.dma_start(out=ldt[:, :], in_=L["s5_log_dt"][d].rearrange("(c g) o -> c (g o)", g=2)))
                dt = k.sb("a3_dt%d" % d, [16, 2], F32)
                k.op("act", [Bt], [Bt], lambda e: e.activation(out=dt[:, :], in_=ldt[:, :], func=AF.Exp))
                wk = k.sb("a3_wk%d" % d, [16, 12, 128], F32)
                dtb = dt[:, :].unsqueeze(2).to_broadcast([16, 2, 64])

                def v3(i):
                    return wk[:, i, :].rearrange("c (g p) -> c g p", g=2)
                k.op("dve", [Bt], [Bt], lambda e: e.tensor_tensor(out=v3(0), in0=are[:, :].rearrange("c (g p) -> c g p", g=2), in1=dtb, op=ALU.mult))
                k.op("dve", [Bt], [Bt], lambda e: e.tensor_tensor(out=v3(1), in0=aim[:, :].rearrange("c (g p) -> c g p", g=2), in1=dtb, op=ALU.mult))
                k.op("dve", [Bt], [Bt], lambda e: e.tensor_scalar(out=wk[:, 1, :], in0=wk[:, 1, :], scalar1=1.0 / 16.0, scalar2=None, op0=ALU.mult))
                hp = k.sb("a3_hp%d" % d, [16, 1], F32)
                k.op("dve", [], [Bt], lambda e: e.memset(hp[:, :], math.pi / 2.0))
                k.op("act", [Bt], [Bt], lambda e: e.activation(out=wk[:, 2, :], in_=wk[:, 0, :], func=AF.Exp))
                k.op("act", [Bt], [Bt], lambda e: e.activation(out=wk[:, 3, :], in_=wk[:, 1, :], func=AF.Sin))
                k.op("act", [Bt], [Bt], lambda e: e.activation(out=wk[:, 4, :], in_=wk[:, 1, :], func=AF.Sin, bias=hp[:, 0:1]))
                for _ in range(4):
                    k.op("dve", [Bt], [Bt], lambda e: e.tensor_tensor(out=wk[:, 5, :], in0=wk[:, 4, :], in1=wk[:, 4, :], op=ALU.mult))
                    k.op("dve", [Bt], [Bt], lambda e: e.tensor_tensor(out=wk[:, 6, :], in0=wk[:, 3, :], in1=wk[:, 3, :], op=ALU.mult))
                    k.op("dve", [Bt], [Bt], lambda e: e.tensor_tensor(out=wk[:, 7, :], in0=wk[:, 3, :], in1=wk[:, 4, :], op=ALU.mult))
                    k.op("dve", [Bt], [Bt], lambda e: e.tensor_tensor(out=wk[:, 4, :], in0=wk[:, 5, :], in1=wk[:, 6, :], op=ALU.subtract))
                    k.op("dve", [Bt], [Bt], lambda e: e.tensor_scalar(out=wk[:, 3, :], in0=wk[:, 7, :], scalar1=2.0, scalar2=None, op0=ALU.mult))
                k.op("dve", [Bt], [Bt], lambda e: e.tensor_tensor(out=wk[:, 5, :], in0=wk[:, 2, :], in1=wk[:, 4, :], op=ALU.mult))
                k.op("dve", [Bt], [Bt], lambda e: e.tensor_tensor(out=wk[:, 6, :], in0=wk[:, 2, :], in1=wk[:, 3, :], op=ALU.mult))
                k.op("dve", [Bt], [Bt], lambda e: e.tensor_scalar(out=wk[:, 7, :], in0=wk[:, 5, :], scalar1=-1.0, scalar2=None, op0=ALU.add))
                k.op("dve", [Bt], [Bt], lambda e: e.tensor_tensor(out=wk[:, 8, :], in0=are[:, :], in1=are[:, :], op=ALU.mult))
                k.op("dve", [Bt], [Bt], lambda e: e.tensor_tensor(out=wk[:, 9, :], in0=aim[:, :], in1=aim[:, :], op=ALU.mult))
                k.op("dve", [Bt], [Bt], lambda e: e.tensor_tensor(out=wk[:, 8, :], in0=wk[:, 8, :], in1=wk[:, 9, :], op=ALU.add))
                k.op("dve", [Bt], [Bt], lambda e: e.reciprocal(out=wk[:, 8, :], in_=wk[:, 8, :]))
                k.op("dve", [Bt], [Bt], lambda e: e.tensor_tensor(out=wk[:, 9, :], in0=wk[:, 7, :], in1=are[:, :], op=ALU.mult))
                k.op("dve", [Bt], [Bt], lambda e: e.tensor_tensor(out=wk[:, 10, :], in0=wk[:, 6, :], in1=aim[:, :], op=ALU.mult))
                k.op("dve", [Bt], [Bt], lambda e: e.tensor_tensor(out=wk[:, 9, :], in0=wk[:, 9, :], in1=wk[:, 10, :], op=ALU.add))
                k.op("dve", [Bt], [Bt], lambda e: e.tensor_tensor(out=wk[:, 9, :], in0=wk[:, 9, :], in1=wk[:, 8, :], op=ALU.mult))
                k.op("dve", [Bt], [Bt], lambda e: e.tensor_tensor(out=wk[:, 10, :], in0=wk[:, 6, :], in1=are[:, :], op=ALU.mult))
                k.op("dve", [Bt], [Bt], lambda e: e.tensor_tensor(out=wk[:, 11, :], in0=wk[:, 7, :], in1=aim[:, :], op=ALU.mult))
                k.op("dve", [Bt], [Bt], lambda e: e.tensor_tensor(out=wk[:, 10, :], in0=wk[:, 10, :], in1=wk[:, 11, :], op=ALU.subtract))
                k.op("dve", [Bt], [Bt], lambda e: e.tensor_tensor(out=wk[:, 10, :], in0=wk[:, 10, :], in1=wk[:, 8, :], op=ALU.mult))
                k.op("dve", [Bt], [Bt], lambda e: e.tensor_tensor(out=wk[:, 0, :], in0=wk[:, 2, :], in1=wk[:, 2, :], op=ALU.mult))
                k.op("dve", [Bt], [Bt], lambda e: e.reciprocal(out=wk[:, 0, :], in_=wk[:, 0, :]))
                k.op("dve", [Bt], [Bt], lambda e: e.tensor_tensor(out=wk[:, 7, :], in0=wk[:, 5, :], in1=wk[:, 0, :], op=ALU.mult))
                k.op("dve", [Bt], [Bt], lambda e: e.tensor_tensor(out=wk[:, 11, :], in0=wk[:, 6, :], in1=wk[:, 0, :], op=ALU.mult))
                k.op("dve", [Bt], [Bt], lambda e: e.tensor_scalar(out=wk[:, 11, :], in0=wk[:, 11, :], scalar1=-1.0, scalar2=None, op0=ALU.mult))
                lamT = k.sb("a3_lamT%d" % d, [128, 6, 16], F32)
                for oi, wi in enumerate((5, 6, 7, 11, 9, 10)):
                    k.op("pe", [Bt], [Bpst], lambda e, wi=wi: e.transpose(out=pst[:, 0:16], in_=wk[:, wi, :], identity=ident_f[0:16, 0:16]))
                    k.op("act", [Bpst], [Bt], lambda e, oi=oi: e.copy(out=lamT[:, oi, :], in_=pst[:, 0:16]))
                for tabs_, ri0 in ((Ppos[d], 0), (Pneg[d], 2)):
                    pr_, pi_ = tabs_
                    first = 0 if d == 0 else 127
                    k.op("dve", [Bt], [Bt], lambda e: e.tensor_copy(out=pr_[:, :, first], in_=lamT[:, ri0, :]))
                    k.op("dve", [Bt], [Bt], lambda e: e.tensor_copy(out=pi_[:, :, first], in_=lamT[:, ri0 + 1, :]))
                    for m in range(7):
                        n = 1 << m
                        if d == 0:
                            src = slice(0, n); dst = slice(n, 2 * n); sc = n - 1
                        else:
                            src = slice(128 - n, 128); dst = slice(128 - 2 * n, 128 - n); sc = 128 - n
                        sr = pr_[:, :, sc:sc + 1].to_broadcast([128, 16, n])
                        si = pi_[:, :, sc:sc + 1].to_broadcast([128, 16, n])
                        tmp = [tmpA[:, i, :, 0:n] for i in range(4)]
                        _cmul_bcast(k, "dve", Bt, pr_[:, :, dst], pi_[:, :, dst], pr_[:, :, src], pi_[:, :, src], sr, si, tmp, None)
                for r in range(2):
                    for ct in range(16):
                        k.op("pe", [Bt], [Bpst], lambda e, r=r, ct=ct: e.transpose(out=pst[:, :], in_=Pneg[d][r][:, ct, :], identity=ident_f[:, :]))
                        k.op("act", [Bpst], [Bt], lambda e, r=r, ct=ct: e.copy(out=Wneg[d][r][:, ct, :], in_=pst[:, :]))
                bre = k.sb("a3_bre%d" % d, [128, 16, 16], F32)
                bim = k.sb("a3_bim%d" % d, [128, 16, 16], F32)
                for gi in range(2):
                    k.dma("sp", [], [Bt], lambda e, gi=gi: e.dma_start(
                        out=bre[gi * 64:(gi + 1) * 64, :, :], in_=L["s5_b_re"][d].rearrange("(c g) p h -> g p c h", g=2)[gi]))
                    k.dma("sp", [], [Bt], lambda e, gi=gi: e.dma_start(
                        out=bim[gi * 64:(gi + 1) * 64, :, :], in_=L["s5_b_im"][d].rearrange("(c g) p h -> g p c h", g=2)[gi]))
                bbr = k.sb("a3_bbr%d" % d, [128, 16, 16], F32)
                bbi = k.sb("a3_bbi%d" % d, [128, 16, 16], F32)
                crb = lamT[:, 4, :].unsqueeze(2).to_broadcast([128, 16, 16])
                cib = lamT[:, 5, :].unsqueeze(2).to_broadcast([128, 16, 16])
                tmp = [tmpA[:, i, :, 0:16] for i in range(4)]
                _cmul_bcast(k, "dve", Bt, bbr[:, :, :], bbi[:, :, :], bre[:, :, :], bim[:, :, :], crb, cib, tmp, None)
                in2 = k.sb("a3_in2%d" % d, [128, 128], F32)
                for c in range(4):
                    k.op("dve", [Bt], [Bt], lambda e, c=c: e.memset(Bblk[d][c][:, :], 0.0))
                    for r, bb in enumerate((bbr, bbi)):
                        for ctl in range(2):
                            k.op("dve", [Bt, Bpst], [Bt], lambda e: e.memset(in2[:, :], 0.0))
                            for gi in range(2):
                                for hf in range(2):
                                    gl = 2 * ctl + gi
                                    ct = 4 * c + 2 * hf + ctl
                                    col = hf * 64 + gl * 16
                                    k.op("dve", [Bt], [Bt], lambda e, gi=gi, ct=ct, col=col, bb=bb: e.tensor_copy(
                                        out=in2[gi * 64:(gi + 1) * 64, col:col + 16], in_=bb[gi * 64:(gi + 1) * 64, ct, :]))
                            k.op("pe", [Bt], [Bpst], lambda e: e.transpose(out=pst[:, :], in_=in2[:, :], identity=ident_f[:, :]))
                            k.op("act", [Bpst], [Bt], lambda e, c=c, r=r, ctl=ctl: e.copy(
                                out=Bblk[d][c][:, (r * 2 + ctl) * 128:(r * 2 + ctl + 1) * 128], in_=pst[:, :]))
                k.op("dve", [Bt], [Bt], lambda e: e.memset(Cpad[d][:, :, :, :], 0.0))
                csb = k.sb("a3_csb%d" % d, [128, 2, 64], F32)
                for r, cten in enumerate((L["s5_c_re"], L["s5_c_im"])):
                    for yt in range(4):
                        for dup in range(2):
                            k.dma("sp", [Bpst], [Bt], lambda e, dup=dup, yt=yt, cten=cten: e.dma_start(
                                out=csb[:, dup, :], in_=cten[d, yt * 128:(yt + 1) * 128, :]))
                        if r == 1:
                            k.op("dve", [Bt], [Bt], lambda e: e.tensor_scalar(out=csb[:, :, :], in0=csb[:, :, :], scalar1=-1.0, scalar2=None, op0=ALU.mult))
                        k.op("pe", [Bt], [Bpst], lambda e: e.transpose(out=pst[:, :], in_=csb[:, :, :].rearrange("a b c -> a (b c)"), identity=ident_f[:, :]))
                        for ctp in range(4):
                            for gi in range(2):
                                k.op("act", [Bpst], [Bt], lambda e, ctp=ctp, gi=gi, yt=yt, r=r: e.copy(
                                    out=Cpad[d][gi * 64:(gi + 1) * 64, 4 * yt + ctp, r, ctp * 32 + gi * 16:ctp * 32 + gi * 16 + 16],
                                    in_=pst[gi * 64:(gi + 1) * 64, (2 * ctp + gi) * 16:(2 * ctp + gi) * 16 + 16]))
            k.barrier()
        k.stack = st
        u4r = Ring(k, "a3_u4", [128, 4, 128], BF16, 3)
        ybr = Ring(k, "a3_yb", [128, 4, 128], F32, 2)
        ps_bu = Ring(k, "a3_psbu", [128, 512], F32, 2, psum=True)
        ps_g = Ring(k, "a3_psg", [128, 2, 128], F32, 2, psum=True)
        ps_y = Ring(k, "a3_psy", [128, 128], F32, 2, psum=True)
        ps_gl = Ring(k, "a3_psgl", [128, 128], F32, 1, psum=True)
        tq = Ring(k, "a3_tq", [128, 4, 256], F32, 2)
        bp = Ring(k, "a3_bp", [128, 2, 16, 128], BF16, 2)
        t4 = Ring(k, "a3_t4", [128, 4, 128], F32, 2)
        hf_ = Ring(k, "a3_hf", [128, 2, 128], F32, 2)
        hb = Ring(k, "a3_hb", [128, 2, 128], BF16, 10)
        ybs = Ring(k, "a3_ybs", [128, 128], F32, 3)
        carry = [k.sb("a3_carry%d" % d, [128, 16, 2], F32) for d in range(2)]
        Bcar = [[Buf("car%d_%d" % (d, ct)) for ct in range(16)] for d in range(2)]
        ysum = k.sb("a3_ysum", [128, 4, 128], F32); Bys = Buf("ysum")
        gtm = k.sb("a3_gtm", [128, 4, 128], F32); Bgt = Buf("gtm")
        ygf = k.sb("a3_ygf", [128, 4, 128], F32); Bygf = Buf("ygf")
        ygb = k.sb("a3_ygb", [128, 4, 128], BF16); Bygb = Buf("ygb")
        gate = Ring(k, "a3_gate", [128, 128], F32, 2)
        sso = Ring(k, "a3_sso", [128, 4, 128], BF16, 2)

        for s in range(NSEQ):
            for d in (1, 0):
                for ct in range(16):
                    k.op("pool", [], [Bcar[d][ct]], lambda e, ct=ct: e.memset(carry[d][:, ct, :], 0.0))
                order = range(NCH - 1, -1, -1) if d == 1 else range(NCH)
                jl = 0 if d == 1 else 127
                for c in order:
                    tb = s * SL + c * 128
                    u4, u4b = u4r.next()
                    k.dma("sp", [], [u4b], lambda e: e.dma_start(out=u4[:, :, :], in_=UT[:, :, tb:tb + 128].rearrange("c p t -> p c t")))
                    if d == 0:
                        ybt, ybb = ybr.next()
                        k.dma("sp", [], [ybb], lambda e: e.dma_start(out=ybt[:, :, :], in_=YB[:, :, tb:tb + 128].rearrange("c p t -> p c t")))
                    bpt, bpb = bp.next()
                    for uc in range(4):
                        for hf in range(2):
                            pbu, pbub = ps_bu.next()
                            k.op("pe", [u4b], [pbub], lambda e, pbu=pbu, uc=uc, hf=hf: e.matmul(
                                pbu[:, :], u4[hf * 64:(hf + 1) * 64, uc, :], Bblk[d][uc][hf * 64:(hf + 1) * 64, :], start=True, stop=True))
                            ct0 = 4 * uc + 2 * hf
                            tqt, tqb = tq.next()
                            wr = Wneg[d][0][:, ct0:ct0 + 2, :].rearrange("p a b -> p (a b)")
                            wi = Wneg[d][1][:, ct0:ct0 + 2, :].rearrange("p a b -> p (a b)")
                            k.op("dve", [pbub], [tqb], lambda e, pbu=pbu, tqt=tqt, wr=wr: e.tensor_tensor(out=tqt[:, 0, :], in0=pbu[:, 0:256], in1=wr, op=ALU.mult))
                            k.op("dve", [pbub], [tqb], lambda e, pbu=pbu, tqt=tqt, wi=wi: e.tensor_tensor(out=tqt[:, 1, :], in0=pbu[:, 256:512], in1=wi, op=ALU.mult))
                            k.op("dve", [pbub], [tqb], lambda e, pbu=pbu, tqt=tqt, wi=wi: e.tensor_tensor(out=tqt[:, 2, :], in0=pbu[:, 0:256], in1=wi, op=ALU.mult))
                            k.op("dve", [pbub], [tqb], lambda e, pbu=pbu, tqt=tqt, wr=wr: e.tensor_tensor(out=tqt[:, 3, :], in0=pbu[:, 256:512], in1=wr, op=ALU.mult))
                            k.op("pool", [tqb], [bpb], lambda e, tqt=tqt, ct0=ct0: e.tensor_tensor(
                                out=bpt[:, 0, ct0:ct0 + 2, :].rearrange("p a b -> p (a b)"), in0=tqt[:, 0, :], in1=tqt[:, 1, :], op=ALU.subtract))
                            k.op("pool", [tqb], [bpb], lambda e, tqt=tqt, ct0=ct0: e.tensor_tensor(
                                out=bpt[:, 1, ct0:ct0 + 2, :].rearrange("p a b -> p (a b)"), in0=tqt[:, 2, :], in1=tqt[:, 3, :], op=ALU.add))
                    hbs = []
                    for ct in range(16):
                        pg, pgb = ps_g.next()

                        def mmG(e, pg=pg, ct=ct):
                            e.matmul(pg[:, 0, :], bpt[:, 0, ct, :], tri[d][:, :], start=True, stop=True)
                            return e.matmul(pg[:, 1, :], bpt[:, 1, ct, :], tri[d][:, :], start=True, stop=True)
                        k.op("pe", [bpb], [pgb], mmG)
                        t4t, t4b = t4.next()
                        cr_ = carry[d][:, ct, 0:1]
                        ci_ = carry[d][:, ct, 1:2]
                        Wr = Ppos[d][0][:, ct, :]
                        Wi = Ppos[d][1][:, ct, :]
                        cb_ = Bcar[d][ct]
                        k.op("dve", [pgb, cb_], [t4b], lambda e, pg=pg, t4t=t4t, cr_=cr_, Wr=Wr: e.scalar_tensor_tensor(out=t4t[:, 0, :], in0=pg[:, 0, :], scalar=cr_, in1=Wr, op0=ALU.add, op1=ALU.mult))
                        k.op("dve", [pgb, cb_], [t4b], lambda e, pg=pg, t4t=t4t, ci_=ci_, Wi=Wi: e.scalar_tensor_tensor(out=t4t[:, 1, :], in0=pg[:, 1, :], scalar=ci_, in1=Wi, op0=ALU.add, op1=ALU.mult))
                        k.op("dve", [pgb, cb_], [t4b], lambda e, pg=pg, t4t=t4t, cr_=cr_, Wi=Wi: e.scalar_tensor_tensor(out=t4t[:, 2, :], in0=pg[:, 0, :], scalar=cr_, in1=Wi, op0=ALU.add, op1=ALU.mult))
                        k.op("dve", [pgb, cb_], [t4b], lambda e, pg=pg, t4t=t4t, ci_=ci_, Wr=Wr: e.scalar_tensor_tensor(out=t4t[:, 3, :], in0=pg[:, 1, :], scalar=ci_, in1=Wr, op0=ALU.add, op1=ALU.mult))
                        hft, hfb = hf_.next()
                        k.op("pool", [t4b], [hfb], lambda e, hft=hft, t4t=t4t: e.tensor_tensor(out=hft[:, 0, :], in0=t4t[:, 0, :], in1=t4t[:, 1, :], op=ALU.subtract))
                        k.op("pool", [t4b], [hfb], lambda e, hft=hft, t4t=t4t: e.tensor_tensor(out=hft[:, 1, :], in0=t4t[:, 2, :], in1=t4t[:, 3, :], op=ALU.add))
                        cmc = cm[:, (NCH if d == 1 else 0) + c:(NCH if d == 1 else 0) + c + 1]
                        k.op("pool", [hfb, Bt], [cb_], lambda e, hft=hft, ct=ct, cmc=cmc: e.tensor_scalar(
                            out=carry[d][:, ct, :], in0=hft[:, :, jl], scalar1=cmc, scalar2=None, op0=ALU.mult))
                        hbt, hbb = hb.next()
                        k.op("act", [hfb], [hbb], lambda e, hbt=hbt, hft=hft: e.copy(out=hbt[:, :, :], in_=hft[:, :, :]))
                        hbs.append((hbt, hbb))
                        if ct % 4 == 3:
                            yt = ct // 4
                            py, pyb = ps_y.next()
                            grp = hbs[-4:]

                            def mmY(e, py=py, grp=grp, yt=yt):
                                n = 0
                                for ctp in range(4):
                                    for r in range(2):
                                        ins = e.matmul(py[:, :], Cpad[d][:, 4 * yt + ctp, r, :], grp[ctp][0][:, r, :], start=(n == 0), stop=(n == 7))
                                        n += 1
                                return ins
                            k.op("pe", [g_[1] for g_ in grp], [pyb], mmY)
                            if d == 1:
                                yo, yob = ybs.next()
                                k.op("act", [pyb], [yob], lambda e, py=py, yo=yo: e.copy(out=yo[:, :], in_=py[:, :]))
                                k.dma("sp", [yob], [], lambda e, yo=yo, yt=yt: e.dma_start(out=YB[yt, :, tb:tb + 128], in_=yo[:, :]))
                            else:
                                k.op("dve", [pyb, ybb], [Bys], lambda e, py=py, yt=yt: e.tensor_tensor(out=ysum[:, yt, :], in0=py[:, :], in1=ybt[:, yt, :], op=ALU.add))
                                k.op("dve", [Bys, u4b, Bt], [Bys], lambda e, yt=yt: e.scalar_tensor_tensor(
                                    out=ysum[:, yt, :], in0=u4[:, yt, :], scalar=dskip[:, yt:yt + 1], in1=ysum[:, yt, :], op0=ALU.mult, op1=ALU.add))
                    if d == 0:
                        k.op("pool", [Bys], [Bgt], lambda e: e.tensor_tensor(out=gtm[:, :, :], in0=ysum[:, :, :], in1=ysum[:, :, :], op=ALU.mult))
                        k.op("pool", [Bgt], [Bgt], lambda e: e.tensor_scalar(out=gtm[:, :, :], in0=gtm[:, :, :], scalar1=0.044715, scalar2=1.0, op0=ALU.mult, op1=ALU.add))
                        k.op("pool", [Bgt, Bys], [Bgt], lambda e: e.tensor_tensor(out=gtm[:, :, :], in0=gtm[:, :, :], in1=ysum[:, :, :], op=ALU.mult))
                        k.op("act", [Bgt], [Bgt], lambda e: e.activation(out=gtm[:, :, :], in_=gtm[:, :, :], func=AF.Sigmoid, scale=1.5957691216057308))
                        k.op("dve", [Bgt, Bys], [Bygf], lambda e: e.tensor_tensor(out=ygf[:, :, :], in0=gtm[:, :, :], in1=ysum[:, :, :], op=ALU.mult))
                        k.op("act", [Bygf], [Bygb], lambda e: e.copy(out=ygb[:, :, :], in_=ygf[:, :, :]))
                        sot, sob = sso.next()
                        for ot in range(4):
                            pgl, pglb = ps_gl.next()

                            def mmGL(e, pgl=pgl, ot=ot):
                                for it in range(4):
                                    ins = e.matmul(pgl[:, :], gluw[:, it, ot * 128:(ot + 1) * 128], ygb[:, it, :], start=(it == 0), stop=(it == 3))
                                return ins
                            k.op("pe", [Bygb, Bt], [pglb], mmGL)
                            gt_, gtb = gate.next()
                            k.op("act", [pglb, Bt], [gtb], lambda e, pgl=pgl, gt_=gt_, ot=ot: e.activation(
                                out=gt_[:, :], in_=pgl[:, :], func=AF.Sigmoid, bias=glub[:, ot:ot + 1]))
                            k.op("dve", [gtb, Bygf], [sob], lambda e, gt_=gt_, ot=ot: e.tensor_tensor(out=sot[:, ot, :], in0=ygf[:, ot, :], in1=gt_[:, :], op=ALU.mult))
                        k.dma("sp", [sob], [], lambda e: e.dma_start(out=SSM[:, :, tb:tb + 128].rearrange("c p t -> p c t"), in_=sot[:, :, :]))
                k.barrier()
    k.stack = prev


class LNR:
    def __init__(self, P, k, L, gam_ap, bet_ap, wr_ap, pfx, nrows=128):
        self.k = k
        self.L = L
        self.nrows = nrows
        self.Bc = Buf(pfx + "const")
        self.gb = k.sb(pfx + "gb", [128, 2, D], F32)
        k.dma("sp", [], [self.Bc], lambda e: e.dma_start(out=self.gb[:, 0, :], in_=gam_ap.partition_broadcast(128)))
        k.dma("sp", [], [self.Bc], lambda e: e.dma_start(out=self.gb[:, 1, :], in_=bet_ap.partition_broadcast(128)))
        self.eps = k.sb(pfx + "eps", [128, 1], F32)
        k.op("dve", [], [self.Bc], lambda e: e.memset(self.eps[:, :], LN_EPS))
        self.z = Ring(k, pfx + "z", [128, D], F32, 2)
        self.st = Ring(k, pfx + "st", [128, 2, 6], F32, 2)
        self.mv = Ring(k, pfx + "mv", [128, 4], F32, 2)
        self.xl = Ring(k, pfx + "xl", [128, D], F32, 2)
        self.router = wr_ap is not None
        if self.router:
            self.wr = k.sb(pfx + "wr", [128, DK, NE], F32)
            k.dma("sp", [], [self.Bc], lambda e: e.dma_start(out=self.wr[:, :, :], in_=wr_ap.rearrange("(dk p) e -> p dk e", p=128)))
            self.xlT = Ring(k, pfx + "xlT", [128, DK, 128], F32, 2)
            self.pst = Ring(k, pfx + "pst", [128, 4, 128], F32, 2, psum=True)
            self.psl = Ring(k, pfx + "psl", [128, NE], F32, 1, psum=True)
            self.sm = Ring(k, pfx + "sm", [128, 4], F32, 2)
            self.ex = Ring(k, pfx + "ex", [128, NE], F32, 2)

    def ln(self, xt, xb_, m_src, m_bufs, m_is_two_bank=True):
        k = self.k
        n = self.nrows
        zt, zb = self.z.next()
        for h in range(2):
            k.op("dve", [xb_] + m_bufs, [zb], lambda e, h=h: e.scalar_tensor_tensor(
                out=zt[0:n, h * 512:(h + 1) * 512], in0=xt[0:n, h * 512:(h + 1) * 512], scalar=ALPHA, in1=m_src(h), op0=ALU.mult, op1=ALU.add))
        stt, stb = self.st.next()
        for h in range(2):
            k.op("dve", [zb], [stb], lambda e, h=h: e.bn_stats(out=stt[0:n, h, :], in_=zt[0:n, h * 512:(h + 1) * 512]))
        mvt, mvb = self.mv.next()
        k.op("dve", [stb], [mvb], lambda e: e.bn_aggr(out=mvt[0:n, 0:2], in_=stt[0:n, :, :].rearrange("p a b -> p (a b)")))
        k.op("act", [mvb, self.Bc], [mvb], lambda e: e.activation(out=mvt[0:n, 2:3], in_=mvt[0:n, 1:2], func=AF.Sqrt, bias=self.eps[0:n, 0:1]))
        k.op("dve", [mvb], [mvb], lambda e: e.reciprocal(out=mvt[0:n, 2:3], in_=mvt[0:n, 2:3]))
        k.op("dve", [mvb], [mvb], lambda e: e.scalar_tensor_tensor(out=mvt[0:n, 3:4], in0=mvt[0:n, 0:1], scalar=-1.0, in1=mvt[0:n, 2:3], op0=ALU.mult, op1=ALU.mult))
        xlt, xlb = self.xl.next()
        k.op("act", [zb, mvb], [xlb], lambda e: e.activation(out=xlt[0:n, :], in_=zt[0:n, :], func=AF.Identity, scale=mvt[0:n, 2:3], bias=mvt[0:n, 3:4]))
        k.op("pool", [xlb, self.Bc], [xlb], lambda e: e.tensor_tensor(out=xlt[0:n, :], in0=xlt[0:n, :], in1=self.gb[0:n, 0, :], op=ALU.mult))
        k.op("pool", [xlb, self.Bc], [xlb], lambda e: e.tensor_tensor(out=xlt[0:n, :], in0=xlt[0:n, :], in1=self.gb[0:n, 1, :], op=ALU.add))
        return xlt, xlb

    def route(self, xlt, xlb, aff_dst, aff_buf):
        k = self.k
        ident_f = self.L["ident_f"]
        xTt, xTb = self.xlT.next()
        for g in range(2):
            pt, ptb = self.pst.next()

            def tr(e, pt=pt, g=g):
                for j in range(4):
                    dk = g * 4 + j
                    ins = e.transpose(out=pt[:, j, :], in_=xlt[:, dk * 128:(dk + 1) * 128], identity=ident_f[:, :])
                return ins
            k.op("pe", [xlb], [ptb], tr)
            k.op("act", [ptb], [xTb], lambda e, pt=pt, g=g: e.copy(out=xTt[:, g * 4:(g + 1) * 4, :], in_=pt[:, :, :]))
        pl, plb = self.psl.next()

        def mm(e):
            for dk in range(DK):
                ins = e.matmul(pl[:, :], xTt[:, dk, :], self.wr[:, dk, :], start=(dk == 0), stop=(dk == DK - 1))
            return ins
        k.op("pe", [xTb, self.Bc], [plb], mm)
        smt, smb = self.sm.next()
        ext, exb = self.ex.next()
        k.op("dve", [plb], [smb], lambda e: e.reduce_max(out=smt[:, 0:1], in_=pl[:, :], axis=AX.X))
        k.op("dve", [smb], [smb], lambda e: e.tensor_scalar(out=smt[:, 1:2], in0=smt[:, 0:1], scalar1=-1.0, scalar2=None, op0=ALU.mult))
        k.op("act", [plb, smb], [exb, smb], lambda e: e.activation(out=ext[:, :], in_=pl[:, :], func=AF.Exp, bias=smt[:, 1:2], accum_out=smt[:, 2:3]))
        k.op("dve", [smb], [smb], lambda e: e.reciprocal(out=smt[:, 3:4], in_=smt[:, 2:3]))
        k.op("dve", [exb, smb], [aff_buf], lambda e: e.tensor_scalar(out=aff_dst, in0=ext[:, :], scalar1=smt[:, 3:4], scalar2=None, op0=ALU.mult))


def phase_A4(P, k, L):
    cfg = P.cfg
    ATT, SSM, XLN, XBF, x_in, root = L["ATT"], L["SSM"], L["XLN"], L["XBF"], L["x_in"], L["root"]
    aff_sb, Baff = L["aff_sb"], L["Baff"]
    prev = k.stack
    with ExitStack() as st:
        k.stack = st
        wo = k.sb("a4_wo", [128, DK, D], BF16); Bwo = Buf("a4_wo")
        k.dma("pool", [], [Bwo], lambda e: e.dma_start(out=wo[:, :, :], in_=L["w_out_even"].ap().rearrange("(dk p) n -> p dk n", p=128)))
        lnr = LNR(P, k, L, L["ln_mix_g"][0:1, :], L["ln_mix_b"][0:1, :], L["w_router"][0], "a4_")
        cat = Ring(k, "a4_cat", [128, 8, 128], BF16, 3)
        xr = Ring(k, "a4_x", [128, D], F32, 3)
        psm = Ring(k, "a4_psm", [128, 2, 512], F32, 2, psum=True)
        for c in range(cfg.NTILE):
            tb = c * 128
            ct, cb = cat.next()
            k.dma("sp", [], [cb], lambda e: e.dma_start(out=ct[:, 0:4, :], in_=ATT[:, :, tb:tb + 128].rearrange("c p t -> p c t")))
            k.dma("sp", [], [cb], lambda e: e.dma_start(out=ct[:, 4:8, :], in_=SSM[:, :, tb:tb + 128].rearrange("c p t -> p c t")))
            xt, xb_ = xr.next()
            k.dma("sp", [], [xb_], lambda e: e.dma_start(out=xt[:, :], in_=x_in[tb:tb + 128, :]))
            pm, pmb = psm.next()

            def mm(e):
                for h in range(2):
                    for kc in range(8):
                        ins = e.matmul(pm[:, h, :], ct[:, kc, :], wo[:, kc, h * 512:(h + 1) * 512], start=(kc == 0), stop=(kc == 7))
                return ins
            k.op("pe", [cb, Bwo], [pmb], mm)
            xlt, xlb = lnr.ln(xt, xb_, lambda h: pm[:, h, :], [pmb])
            k.dma("sp", [xlb], [], lambda e: e.dma_start(out=XLN[tb:tb + 128, :], in_=xlt[:, :]))
            k.dma("pool", [xlb], [], lambda e: e.dma_start(out=XBF[tb:tb + 128, :], in_=xlt[:, :]))
            lnr.route(xlt, xlb, aff_sb[:, c, :], Baff)
        if "AFFD" in P.dbg:
            k.dma("sp", [Baff], [], lambda e: e.dma_start(out=L["AFFD"][:, :], in_=aff_sb[:, :, :].rearrange("p c e -> p (c e)")))
        k.barrier()
    k.stack = prev


def phase_B(P, k, L, layer):
    cfg = P.cfg
    T, IDX, root = L["T"], L["IDX"], L["root"]
    aff_sb, Baff = L["aff_sb"], L["Baff"]
    ones_f = L["ones_f"]
    NTL, CAP = cfg.NTILE, cfg.CAP
    prev = k.stack
    with ExitStack() as st:
        k.stack = st
        Bb = Buf("b_small")
        cmp = k.sb("b_cmp", [128, NTL, NE], F32); Bcmp = Buf("b_cmp")
        cs = k.sb("b_cs", [128, NTL, NE], F32); Bcs = Buf("b_cs")
        sl_i = k.sb("b_sli", [128, NTL, NE], I32); Bsl = Buf("b_sli")
        zer = k.sb("b_zer", [128, NTL], F32)
        tok = k.sb("b_tok", [128, NTL], F32)
        ust = k.sb("b_ust", [128, 128], F32)
        k.op("dve", [], [Bb], lambda e: e.memset(zer[:, :], 0.0))
        k.dma("sp", [], [Bb], lambda e: e.dma_start(out=tok[:, :], in_=T["tok%d" % layer][:, :]))
        k.dma("sp", [], [Bb], lambda e: e.dma_start(out=ust[:, :], in_=T["ustrict"][:, :]))
        sm = k.sb("b_sm", [128, 8, NE], F32)
        pst = k.ps("b_ps", [128, NE], F32); Bps = Buf("b_ps")
        k.op("dve", [], [Bb], lambda e: e.memset(sm[:, 0, :], 0.0))
        k.op("dve", [], [Bb], lambda e: e.memset(sm[:, 1, :], 1.0))

        def compare(thr_row):
            k.op("dve", [Baff, Bb], [Bcmp], lambda e: e.tensor_tensor(
                out=cmp[:, :, :], in0=aff_sb[:, :, :], in1=sm[:, thr_row, :].unsqueeze(1).to_broadcast([128, NTL, NE]), op=ALU.is_gt))
        for it in range(30):
            k.op("dve", [Bb], [Bb], lambda e: e.tensor_tensor(out=sm[:, 2, :], in0=sm[:, 0, :], in1=sm[:, 1, :], op=ALU.add))
            k.op("dve", [Bb], [Bb], lambda e: e.tensor_scalar(out=sm[:, 2, :], in0=sm[:, 2, :], scalar1=0.5, scalar2=None, op0=ALU.mult))
            compare(2)
            k.op("dve", [Bcmp], [Bb], lambda e: e.tensor_reduce(out=sm[:, 3, :], in_=cmp[:, :, :].rearrange("p c e -> p e c"), axis=AX.X, op=ALU.add))
            k.op("pe", [Bb], [Bps], lambda e: e.matmul(pst[:, :], ones_f[:, :], sm[:, 3, :], start=True, stop=True))
            k.op("dve", [Bps], [Bb], lambda e: e.tensor_scalar(out=sm[:, 4, :], in0=pst[:, :], scalar1=float(CAP) - 0.5, scalar2=None, op0=ALU.is_ge))
            k.op("dve", [Bb], [Bb], lambda e: e.tensor_tensor(out=sm[:, 5, :], in0=sm[:, 2, :], in1=sm[:, 0, :], op=ALU.subtract))
            k.op("dve", [Bb], [Bb], lambda e: e.tensor_tensor(out=sm[:, 5, :], in0=sm[:, 5, :], in1=sm[:, 4, :], op=ALU.mult))
            k.op("dve", [Bb], [Bb], lambda e: e.tensor_tensor(out=sm[:, 0, :], in0=sm[:, 0, :], in1=sm[:, 5, :], op=ALU.add))
            k.op("dve", [Bb], [Bb], lambda e: e.tensor_tensor(out=sm[:, 5, :], in0=sm[:, 1, :], in1=sm[:, 2, :], op=ALU.subtract))
            k.op("dve", [Bb], [Bb], lambda e: e.tensor_tensor(out=sm[:, 5, :], in0=sm[:, 5, :], in1=sm[:, 4, :], op=ALU.mult))
            k.op("dve", [Bb], [Bb], lambda e: e.tensor_tensor(out=sm[:, 1, :], in0=sm[:, 2, :], in1=sm[:, 5, :], op=ALU.add))
        compare(0)
        for ex in range(NE):
            k.op("dve", [Bcmp, Bb], [Bcs], lambda e, ex=ex: e.tensor_tensor_scan(
                out=cs[:, :, ex], data0=cmp[:, :, ex], data1=zer[:, :], initial=0.0, op0=ALU.add, op1=ALU.add))
        k.op("pe", [Bcs, Bb], [Bps], lambda e: e.matmul(pst[:, :], ust[:, :], cs[:, NTL - 1, :], start=True, stop=True))
        k.op("dve", [Bps], [Bb], lambda e: e.tensor_scalar(out=sm[:, 6, :], in0=pst[:, :], scalar1=-1.0, scalar2=None, op0=ALU.add))
        k.op("dve", [Bcs, Bb], [Bcs], lambda e: e.tensor_tensor(
            out=cs[:, :, :], in0=cs[:, :, :], in1=sm[:, 6, :].unsqueeze(1).to_broadcast([128, NTL, NE]), op=ALU.add))
        BIG = float(1 << 20)
        k.op("dve", [Bcmp], [Bcmp], lambda e: e.tensor_scalar(out=cmp[:, :, :], in0=cmp[:, :, :], scalar1=-BIG, scalar2=BIG, op0=ALU.mult, op1=ALU.add))
        k.op("dve", [Bcs, Bcmp], [Bcs], lambda e: e.tensor_tensor(out=cs[:, :, :], in0=cs[:, :, :], in1=cmp[:, :, :], op=ALU.add))
        k.op("dve", [Bcs], [Bsl], lambda e: e.tensor_copy(out=sl_i[:, :, :], in_=cs[:, :, :]))
        src = Ring(k, "b_src", [128, NTL, 2], F32, 2)
        for ex in range(NE):
            st_, sb_ = src.next()
            k.op("act", [Bb], [sb_], lambda e: e.copy(out=st_[:, :, 0], in_=tok[:, :]))
            k.op("act", [Baff], [sb_], lambda e, ex=ex: e.copy(out=st_[:, :, 1], in_=aff_sb[:, :, ex]))
            for c in range(NTL):
                k.dma("pool", [Bsl, sb_], [], lambda e, c=c, ex=ex: e.indirect_dma_start(
                    out=IDX[ex][:, :], out_offset=bass.IndirectOffsetOnAxis(ap=sl_i[:, c, ex:ex + 1], axis=0),
                    in_=st_[:, c, :], in_offset=None, bounds_check=L["bnd_reg"], oob_is_err=False))
        k.barrier()
    k.stack = prev


def phase_C(P, k, L, layer):
    cfg = P.cfg
    IDX, XBF, YY, root = L["IDX"], L["XBF"], L["YY"], L["root"]
    ident_b = L["ident_b"]
    w1d, w3d, w2d = L["w_ff1"], L["w_ff3"], L["w_ff2"]
    CAP, TS, NT = cfg.CAP, cfg.TS, cfg.NT
    NSUB = TS // 128
    prev = k.stack
    with ExitStack() as st:
        k.stack = st
        zt = k.sb("c_zt", [128, 4 * D], F32); Bz = Buf("c_zt")
        k.op("dve", [], [Bz], lambda e: e.memset(zt[:, :], 0.0))
        for r0 in range(0, NT, 512):
            k.dma("sp", [Bz], [], lambda e, r0=r0: e.dma_start(out=YY[r0:r0 + 512, :].rearrange("(p a) d -> p (a d)", a=4), in_=zt[:, :]))
        k.barrier()
        w1 = k.sb("c_w1", [128, DK, FH], BF16)
        w3 = k.sb("c_w3", [128, DK, FH], BF16)
        w2 = k.sb("c_w2", [128, FK, D], BF16)
        Bw13 = Buf("c_w13"); Bw2 = Buf("c_w2")
        idf = Ring(k, "c_idf", [128, CAP // 128, 2], F32, 2)
        idi = Ring(k, "c_idi", [128, CAP // 128], I32, 2)
        xg = Ring(k, "c_xg", [128, NSUB, D], BF16, 2)
        xT = Ring(k, "c_xT", [128, DK, TS], BF16, 2)
        gT = Ring(k, "c_gT", [128, FK, TS], BF16, 2)
        sl = Ring(k, "c_sl", [128, TS], F32, 2)
        osb = Ring(k, "c_osb", [128, D], F32, 3)
        pst = Ring(k, "c_pst", [128, TS], BF16, 2, psum=True)
        ph1 = Ring(k, "c_ph1", [128, TS], F32, 2, psum=True)
        ph3 = Ring(k, "c_ph3", [128, TS], F32, 2, psum=True)
        pso = Ring(k, "c_pso", [128, 2, 512], F32, 1, psum=True)
        for ex in range(NE):
            idft, idfb = idf.next()
            ncol = CAP // 128
            nsp = 4 if ncol >= 16 else 1
            cpp = ncol // nsp
            for sp_ in range(nsp):
                k.dma("sp", [], [idfb], lambda e, sp_=sp_: e.dma_start(
                    out=idft[:, sp_ * cpp:(sp_ + 1) * cpp, :],
                    in_=IDX[ex][sp_ * cpp * 128:(sp_ + 1) * cpp * 128, :].rearrange("(c p) t -> p c t", p=128)))
            idit, idib = idi.next()
            k.op("dve", [idfb], [idib], lambda e: e.tensor_copy(out=idit[:, :], in_=idft[:, :, 0]))
            for hfi in range(2):
                f0 = hfi * FH
                k.dma("pool", [], [Bw13], lambda e: e.dma_start(out=w1[:, :, :], in_=w1d[layer, ex, :, f0:f0 + FH].rearrange("(dk p) f -> p dk f", p=128)))
                k.dma("pool", [], [Bw13], lambda e: e.dma_start(out=w3[:, :, :], in_=w3d[layer, ex, :, f0:f0 + FH].rearrange("(dk p) f -> p dk f", p=128)))
                k.dma("pool", [], [Bw2], lambda e: e.dma_start(out=w2[:, :, :], in_=w2d[layer, ex, f0:f0 + FH, :].rearrange("(fk p) d -> p fk d", p=128)))
                for ti in range(CAP // TS):
                    xgt, xgb = xg.next()
                    for j in range(NSUB):
                        col = ti * NSUB + j
                        k.dma("pool", [idib], [xgb], lambda e, j=j, col=col: e.indirect_dma_start(
                            out=xgt[:, j, :], out_offset=None, in_=XBF[:, :],
                            in_offset=bass.IndirectOffsetOnAxis(ap=idit[:, col:col + 1], axis=0)))
                    xTt, xTb = xT.next()
                    for dk in range(DK):
                        pt, ptb = pst.next()

                        def tr(e, pt=pt, dk=dk):
                            for j in range(NSUB):
                                ins = e.transpose(out=pt[:, j * 128:(j + 1) * 128], in_=xgt[:, j, dk * 128:(dk + 1) * 128], identity=ident_b[:, :])
                            return ins
                        k.op("pe", [xgb], [ptb], tr)
                        if dk % 2 == 0:
                            k.op("act", [ptb], [xTb], lambda e, pt=pt, dk=dk: e.copy(out=xTt[:, dk, :], in_=pt[:, :]))
                        else:
                            k.op("dve", [ptb], [xTb], lambda e, pt=pt, dk=dk: e.tensor_copy(out=xTt[:, dk, :], in_=pt[:, :]))
                    gTt, gTb = gT.next()
                    for fk in range(FK):
                        p1, p1b = ph1.next()
                        p3, p3b = ph3.next()

                        def mm1(e, p1=p1, fk=fk):
                            for dk in range(DK):
                                ins = e.matmul(p1[:, :], w1[:, dk, fk * 128:(fk + 1) * 128], xTt[:, dk, :], start=(dk == 0), stop=(dk == DK - 1))
                            return ins

                        def mm3(e, p3=p3, fk=fk):
                            for dk in range(DK):
                                ins = e.matmul(p3[:, :], w3[:, dk, fk * 128:(fk + 1) * 128], xTt[:, dk, :], start=(dk == 0), stop=(dk == DK - 1))
                            return ins
                        k.op("pe", [Bw13, xTb], [p1b], mm1)
                        k.op("pe", [Bw13, xTb], [p3b], mm3)
                        slt, slb = sl.next()
                        k.op("act", [p1b], [slb], lambda e, p1=p1, slt=slt: e.activation(out=slt[:, :], in_=p1[:, :], func=AF.Silu))
                        k.op("dve", [slb, p3b], [gTb], lambda e, p3=p3, slt=slt, fk=fk: e.tensor_tensor(out=gTt[:, fk, :], in0=slt[:, :], in1=p3[:, :], op=ALU.mult))
                    for j in range(NSUB):
                        col = ti * NSUB + j
                        po, pob = pso.next()

                        def mmo(e, po=po, j=j):
                            for h in range(2):
                                for fk in range(FK):
                                    ins = e.matmul(po[:, h, :], gTt[:, fk, j * 128:(j + 1) * 128], w2[:, fk, h * 512:(h + 1) * 512], start=(fk == 0), stop=(fk == FK - 1))
                            return ins
                        k.op("pe", [Bw2, gTb], [pob], mmo)
                        ot, ob = osb.next()
                        k.op("act", [pob, idfb], [ob], lambda e, po=po, ot=ot, col=col: e.activation(
                            out=ot[:, :], in_=po[:, :, :].rearrange("p a b -> p (a b)"), func=AF.Copy, scale=idft[:, col, 1:2]))
                        k.dma("pool", [ob, idib], [], lambda e, ot=ot, col=col: e.indirect_dma_start(
                            out=YY[:, :], out_offset=bass.IndirectOffsetOnAxis(ap=idit[:, col:col + 1], axis=0),
                            in_=ot[:, :], in_offset=None, compute_op=ALU.add))
        k.barrier()
    k.stack = prev


def phase_D(P, k, L, layer, dst):
    cfg = P.cfg
    XLN, YY, root = L["XLN"], L["YY"], L["root"]
    prev = k.stack
    with ExitStack() as st:
        k.stack = st
        lnr = LNR(P, k, L, L["ln_ffn_g"][layer:layer + 1, :], L["ln_ffn_b"][layer:layer + 1, :], None, "d_")
        xr = Ring(k, "d_x", [128, D], F32, 3)
        yr = Ring(k, "d_y", [128, D], F32, 3)
        for c in range(cfg.NTILE):
            tb = c * 128
            xt, xb_ = xr.next()
            yt, yb_ = yr.next()
            k.dma("sp", [], [xb_], lambda e: e.dma_start(out=xt[:, :], in_=XLN[tb:tb + 128, :]))
            k.dma("sp", [], [yb_], lambda e: e.dma_start(out=yt[:, :], in_=YY[tb:tb + 128, :]))
            xlt, xlb = lnr.ln(xt, xb_, lambda h: yt[:, h * 512:(h + 1) * 512], [yb_])
            k.dma("sp", [xlb], [], lambda e: e.dma_start(out=dst[tb:tb + 128, :], in_=xlt[:, :]))
        k.barrier()
    k.stack = prev


def phase_B0(P, k, L):
    phase_B(P, k, L, 0)


def phase_C0(P, k, L):
    phase_C(P, k, L, 0)


def phase_D0(P, k, L):
    phase_D(P, k, L, 0, L["X1"])


def phase_F(P, k, L):
    cfg = P.cfg
    X1, XLN, XBF, AFS, T, root = L["X1"], L["XLN"], L["XBF"], L["AFS"], L["T"], L["root"]
    aff_sb, Baff, ident_b = L["aff_sb"], L["Baff"], L["ident_b"]
    SL, NSEQ, NCH, N2E, KPER = cfg.SL, cfg.NSEQ, cfg.NCH, cfg.N2E, cfg.KPER
    prev = k.stack
    with ExitStack() as st:
        k.stack = st
        Bt = Buf("f_setup")
        wcs = [k.sb("f_wc%d" % i, [128, DK, D], BF16) for i in range(2)]
        c1b = k.sb("f_c1", [128, 3, 128], BF16)
        c2b = k.sb("f_c2", [N2E, 2, KPER * 128], BF16)
        tw = k.sb("f_tw", [128, 2, N2E], F32)
        fidx = k.sb("f_idx", [128, NSEQ * N2E], I32)
        k.dma("pool", [], [Bt], lambda e: e.dma_start(out=c1b[:, 0, :], in_=T["c1"][:, :]))
        k.dma("pool", [], [Bt], lambda e: e.dma_start(out=c1b[:, 1, :], in_=T["s1"][:, :]))
        k.op("dve", [Bt], [Bt], lambda e: e.tensor_scalar(out=c1b[:, 2, :], in0=c1b[:, 1, :], scalar1=-1.0, scalar2=None, op0=ALU.mult))
        k.dma("pool", [], [Bt], lambda e: e.dma_start(out=c2b[:, 0, :], in_=T["c2p"][:, :]))
        k.dma("pool", [], [Bt], lambda e: e.dma_start(out=c2b[:, 1, :], in_=T["s2p"][:, :]))
        k.dma("sp", [], [Bt], lambda e: e.dma_start(out=tw[:, 0, :], in_=T["twr"][:, :]))
        k.dma("sp", [], [Bt], lambda e: e.dma_start(out=tw[:, 1, :], in_=T["twi"][:, :]))
        k.dma("sp", [], [Bt], lambda e: e.dma_start(out=fidx[:, :], in_=T["fidx"][:, :]))
        with ExitStack() as st2:
            k.stack = st2
            wob = k.sb("f_wob", [128, DK, D], BF16)
            ccb = k.sb("f_ccb", [128, 2, 512], BF16)
            k.dma("pool", [], [Bt], lambda e: e.dma_start(out=wob[:, :, :], in_=L["w_out_odd"].ap().rearrange("(dk p) n -> p dk n", p=128)))
            k.dma("pool", [], [Bt], lambda e: e.dma_start(out=ccb[:, 0, :], in_=T["cc2"][:, :]))
            k.dma("pool", [], [Bt], lambda e: e.dma_start(out=ccb[:, 1, :], in_=T["sc2"][:, :]))
            psw = Ring(k, "f_psw", [128, 2, 512], F32, 2, psum=True)
            for i in range(2):
                for mc in range(8):
                    g, mm = mc // 2, mc % 2
                    pw, pwb = psw.next()

                    def mmw(e, pw=pw, g=g, mm=mm, i=i):
                        for h in range(2):
                            for kk in range(2):
                                ins = e.matmul(pw[:, h, :], ccb[:, i, (kk * 2 + mm) * 128:(kk * 2 + mm + 1) * 128],
                                               wob[:, 2 * g + kk, h * 512:(h + 1) * 512], start=(kk == 0), stop=(kk == 1))
                        return ins
                    k.op("pe", [Bt], [pwb], mmw)
                    k.op("act", [pwb], [Bt], lambda e, pw=pw, i=i, mc=mc: e.copy(out=wcs[i][:, mc, :], in_=pw[:, :, :].rearrange("p a b -> p (a b)")))
            k.barrier()
        k.stack = st
        for s in range(NSEQ):
            base = s * SL
            with ExitStack() as sa:
                k.stack = sa
                xg = Ring(k, "f_xg", [128, D], F32, 2)
                xb = Ring(k, "f_xb", [128, D], BF16, 2)
                xT = Ring(k, "f_xT", [128, DK, 128], BF16, 2)
                ub = Ring(k, "f_ub", [128, 2, D], BF16, 2)
                tt = Ring(k, "f_tt", [128, 2, 512], F32, 2)
                apb = Ring(k, "f_apb", [128, 2, D], BF16, 2)
                pst = Ring(k, "f_pst", [128, 4, 128], BF16, 2, psum=True)
                psu = Ring(k, "f_psu", [128, 4, 512], F32, 1, psum=True)
                psa = Ring(k, "f_psa", [128, 2, 512], F32, 1, psum=True)
                for j in range(N2E):
                    xgt, xgb = xg.next()
                    col = s * N2E + j
                    k.dma("pool", [Bt], [xgb], lambda e: e.indirect_dma_start(
                        out=xgt[:, :], out_offset=None, in_=X1[:, :], in_offset=bass.IndirectOffsetOnAxis(ap=fidx[:, col:col + 1], axis=0)))
                    xbt, xbb = xb.next()
                    k.op("act", [xgb], [xbb], lambda e: e.copy(out=xbt[:, :], in_=xgt[:, :]))
                    xTt, xTb = xT.next()
                    for g in range(2):
                        pt, ptb = pst.next()

                        def tr(e, pt=pt, g=g):
                            for jj in range(4):
                                dk = g * 4 + jj
                                ins = e.transpose(out=pt[:, jj, :], in_=xbt[:, dk * 128:(dk + 1) * 128], identity=ident_b[:, :])
                            return ins
                        k.op("pe", [xbb], [ptb], tr)
                        k.op("dve", [ptb], [xTb], lambda e, pt=pt, g=g: e.tensor_copy(out=xTt[:, g * 4:(g + 1) * 4, :], in_=pt[:, :, :]))
                    pu, pub = psu.next()

                    def mmu(e):
                        for i in range(2):
                            for h in range(2):
                                for dk in range(DK):
                                    ins = e.matmul(pu[:, i * 2 + h, :], xTt[:, dk, :], wcs[i][:, dk, h * 512:(h + 1) * 512], start=(dk == 0), stop=(dk == DK - 1))
                        return ins
                    k.op("pe", [xTb, Bt], [pub], mmu)
                    ubt, ubb = ub.next()
                    k.op("act", [pub], [ubb], lambda e: e.copy(out=ubt[:, 0, :], in_=pu[:, 0:2, :].rearrange("p a b -> p (a b)")))
                    k.op("dve", [pub], [ubb], lambda e: e.tensor_copy(out=ubt[:, 1, :], in_=pu[:, 2:4, :].rearrange("p a b -> p (a b)")))
                    apt, apbb = apb.next()
                    for h in range(2):
                        pa, pab = psa.next()
                        hs = slice(h * 512, (h + 1) * 512)

                        def mma(e, pa=pa, hs=hs):
                            e.matmul(pa[:, 0, :], c1b[:, 0, :], ubt[:, 0, hs], start=True, stop=False)
                            e.matmul(pa[:, 0, :], c1b[:, 1, :], ubt[:, 1, hs], start=False, stop=True)
                            e.matmul(pa[:, 1, :], c1b[:, 0, :], ubt[:, 1, hs], start=True, stop=False)
                            return e.matmul(pa[:, 1, :], c1b[:, 2, :], ubt[:, 0, hs], start=False, stop=True)
                        k.op("pe", [ubb, Bt], [pab], mma)
                        ttt, ttb = tt.next()
                        k.op("act", [pab, Bt], [ttb], lambda e, pa=pa, ttt=ttt: e.activation(out=ttt[:, 0, :], in_=pa[:, 1, :], func=AF.Copy, scale=tw[:, 1, j:j + 1]))
                        k.op("act", [pab, Bt], [ttb], lambda e, pa=pa, ttt=ttt: e.activation(out=ttt[:, 1, :], in_=pa[:, 1, :], func=AF.Copy, scale=tw[:, 0, j:j + 1]))
                        k.op("dve", [pab, ttb, Bt], [apbb], lambda e, pa=pa, ttt=ttt, hs=hs: e.scalar_tensor_tensor(
                            out=apt[:, 0, hs], in0=pa[:, 0, :], scalar=tw[:, 0, j:j + 1], in1=ttt[:, 0, :], op0=ALU.mult, op1=ALU.subtract))
                        k.op("dve", [pab, ttb, Bt], [apbb], lambda e, pa=pa, ttt=ttt, hs=hs: e.scalar_tensor_tensor(
                            out=apt[:, 1, hs], in0=pa[:, 0, :], scalar=tw[:, 1, j:j + 1], in1=ttt[:, 1, :], op0=ALU.mult, op1=ALU.add))
                    k.dma("sp", [apbb], [], lambda e: e.dma_start(out=AFS[:, j, :, :], in_=apt[:, :, :]))
                k.barrier()
            with ExitStack() as sc:
                k.stack = sc
                lnr = LNR(P, k, L, L["ln_mix_g"][1:2, :], L["ln_mix_b"][1:2, :], L["w_router"][1], "fl_")
                a2 = Ring(k, "f_a2", [N2E, KPER, 2, D], BF16, 2 if KPER <= 4 else 1)
                xr = Ring(k, "f_xr", [128, D], F32, 2)
                psm = Ring(k, "f_psm", [128, 2, 512], F32, 2, psum=True)
                x1v = X1[base:base + SL, :].rearrange("(j k) d -> k j d", k=128)
                xlnv = XLN[base:base + SL, :].rearrange("(j k) d -> k j d", k=128)
                xbfv = XBF[base:base + SL, :].rearrange("(j k) d -> k j d", k=128)
                for q in range(NCH):
                    a2t, a2b = a2.next()
                    k.dma("sp", [], [a2b], lambda e: e.dma_start(out=a2t[:, :, :, :], in_=AFS[q * KPER:(q + 1) * KPER, :, :, :].rearrange("k j r c -> j k r c")))
                    xt, xb_ = xr.next()
                    for v in range(KPER):
                        k.dma("sp", [], [xb_], lambda e, v=v: e.dma_start(out=xt[v * N2E:(v + 1) * N2E, :], in_=x1v[q * KPER + v]))
                    pm, pmb = psm.next()

                    def mmm(e):
                        for h in range(2):
                            n = 0
                            for v in range(KPER):
                                for r in range(2):
                                    ins = e.matmul(pm[:, h, :], c2b[:, r, v * 128:(v + 1) * 128], a2t[:, v, r, h * 512:(h + 1) * 512],
                                                   start=(n == 0), stop=(n == 2 * KPER - 1))
                                    n += 1
                        return ins
                    k.op("pe", [a2b, Bt], [pmb], mmm)
                    xlt, xlb = lnr.ln(xt, xb_, lambda h: pm[:, h, :], [pmb])
                    for v in range(KPER):
                        k.dma("sp", [xlb], [], lambda e, v=v: e.dma_start(out=xlnv[q * KPER + v], in_=xlt[v * N2E:(v + 1) * N2E, :]))
                        k.dma("pool", [xlb], [], lambda e, v=v: e.dma_start(out=xbfv[q * KPER + v], in_=xlt[v * N2E:(v + 1) * N2E, :]))
                    lnr.route(xlt, xlb, aff_sb[:, s * NCH + q, :], Baff)
                k.barrier()
            k.stack = st
        if "AFFD" in P.dbg:
            k.dma("sp", [Baff], [], lambda e: e.dma_start(out=L["AFFD"][:, :], in_=aff_sb[:, :, :].rearrange("p c e -> p (c e)")))
        k.barrier()
    k.stack = prev


def phase_B1(P, k, L):
    phase_B(P, k, L, 1)


def phase_C1(P, k, L):
    phase_C(P, k, L, 1)


def phase_D1(P, k, L):
    phase_D(P, k, L, 1, L["y_out"])


_PROG = {}


def kernel(**inputs):
    xp = np.asarray(inputs["x_prompt"], dtype=np.float32)
    xs = np.asarray(inputs["x_sample"], dtype=np.float32)
    inp = {n: np.asarray(v) for n, v in inputs.items() if n not in ("x_prompt", "x_sample")}
    Bp, Sp, _ = xp.shape
    Bs, Ss, _ = xs.shape
    SL = max(Sp, Ss)
    assert Bp * Sp == Bs * Ss and SL % Sp == 0 and SL % Ss == 0
    nseq = Bp * Sp // SL
    cfg_p = Cfg(nseq, SL, Sp)
    cfg_s = Cfg(nseq, SL, Ss)
    key = (nseq, SL)
    if key not in _PROG:
        _PROG[key] = build(cfg_p)
    P = _PROG[key]
    maps = []
    for cfg, x in ((cfg_p, xp), (cfg_s, xs)):
        P.tabs = const_tables(cfg)
        maps.append(core_inputs(cfg, P, x.reshape(-1, D), inp))
    res = run_bass_kernel_spmd(P.nc, maps, core_ids=[0, 1])
    y_p = np.asarray(res.results[0]["y"], dtype=np.float32).reshape(Bp, Sp, D)
    y_s = np.asarray(res.results[1]["y"], dtype=np.float32).reshape(Bs, Ss, D)
    return (y_p, y_s)
```

```python
import math
import numpy as np
import ml_dtypes
import concourse.bass as bass
import concourse.mybir as mybir
from concourse.bass_utils import run_bass_kernel_spmd

F32 = mybir.dt.float32
BF16 = mybir.dt.bfloat16
I32 = mybir.dt.int32
ALU = mybir.AluOpType
AF = mybir.ActivationFunctionType
AX = mybir.AxisListType

D = 1024
DK = 8
NE = 16
DFF = 2816
FH = 1408
FK = 11
LN_EPS = 1e-5
ALPHA = 4.0 ** 0.25
NEG = -30000.0


class Buf:
    __slots__ = ("name", "w", "r")

    def __init__(self, name):
        self.name = name
        self.w = None
        self.r = []


class Eng:
    def __init__(self, k, name, e, sem):
        self.k = k
        self.name = name
        self.e = e
        self.sem = sem
        self.count = 0
        self.seen = {}


class K:
    def __init__(self, nc, stack):
        self.nc = nc
        self.stack = stack
        self.root = stack
        self.engs = {}
        for name, e in (("pe", nc.tensor), ("act", nc.scalar), ("dve", nc.vector),
                        ("pool", nc.gpsimd), ("sp", nc.sync)):
            sem = stack.enter_context(nc.semaphore("s_" + name))
            self.engs[name] = Eng(self, name, e, sem)
        self.ndma = 24
        self.dsems = [stack.enter_context(nc.semaphore("d%d" % i)) for i in range(self.ndma)]
        self.dcount = [0] * self.ndma
        self.dnext = 0
        self.pending = []
        self.same_engine_sync = False

    def _wait(self, eng, ev):
        if ev is None:
            return
        sem, val, src = ev
        if src == eng.name and src == "pe":
            return
        key = id(sem)
        if eng.seen.get(key, 0) >= val:
            return
        eng.e.wait_ge(sem, val)
        eng.seen[key] = val

    def deps(self, eng, reads, writes):
        for b in reads:
            self._wait(eng, b.w)
        for b in writes:
            self._wait(eng, b.w)
            for ev in b.r:
                self._wait(eng, ev)

    def _record(self, ev, reads, writes):
        for b in reads:
            b.r.append(ev)
            if len(b.r) > 12:
                b.r = b.r[-12:]
        for b in writes:
            b.w = ev
            b.r = []

    def op(self, en, reads, writes, fn):
        eng = self.engs[en]
        self.deps(eng, reads, writes)
        ins = fn(eng.e)
        eng.count += 1
        ins.then_inc(eng.sem, 1)
        self._record((eng.sem, eng.count, en), reads, writes)
        return ins

    def dma(self, qn, reads, writes, fn):
        eng = self.engs[qn]
        self.deps(eng, reads, writes)
        i = self.dnext
        self.dnext = (self.dnext + 1) % self.ndma
        sem = self.dsems[i]
        if self.dcount[i] > 0:
            self._wait(eng, (sem, self.dcount[i], "dma"))
        ins = fn(eng.e)
        self.dcount[i] += 16
        ins.then_inc(sem, 16)
        ev = (sem, self.dcount[i], "dma")
        self._record(ev, reads, writes)
        self.pending.append(ev)
        if len(self.pending) > 4 * self.ndma:
            self.pending = self.pending[-self.ndma:]
        return ins

    def barrier(self):
        evs = [(e.sem, e.count, e.name) for e in self.engs.values() if e.count > 0]
        evs += [(self.dsems[i], self.dcount[i], "dma") for i in range(self.ndma) if self.dcount[i] > 0]
        for eng in self.engs.values():
            for ev in evs:
                if ev[2] == eng.name:
                    continue
                self._wait(eng, ev)
        self.pending = []

    _uid = 0

    def rotate(self):
        self.barrier()
        for eng in self.engs.values():
            if eng.count > 0:
                eng.sem = self.root.enter_context(self.nc.semaphore("s_%s_%d" % (eng.name, K._uid)))
                K._uid += 1
                eng.count = 0

    def sb(self, name, shape, dt):
        K._uid += 1
        return self.stack.enter_context(self.nc.sbuf_tensor("%s_%d" % (name, K._uid), shape, dt))

    def ps(self, name, shape, dt=F32):
        K._uid += 1
        return self.stack.enter_context(self.nc.psum_tensor("%s_%d" % (name, K._uid), shape, dt))


class Cfg:
    def __init__(self, nseq, sl, rs):
        self.NSEQ = nseq
        self.SL = sl
        self.RS = rs
        self.NT = nseq * sl
        self.NTILE = self.NT // 128
        self.CAP = max(1, 2 * self.NT // NE)
        self.NCH = sl // 128
        self.QT = min(512, sl)
        self.NQT = sl // self.QT
        self.TS = min(512, self.CAP)
        self.N2E = sl // 128
        self.N2 = rs // 128


def const_tables(cfg):
    SL, RS, NSEQ, NT = cfg.SL, cfg.RS, cfg.NSEQ, cfg.NT
    t = {}
    inv = (1.0 / (np.float32(10000.0) ** (np.arange(0, 64, 2, dtype=np.float32) / np.float32(64)))).astype(np.float32)
    pos = (np.arange(SL) % RS).astype(np.float32)
    ang = (pos[:, None] * inv[None, :]).astype(np.float32)
    ang = np.concatenate([ang, ang], -1)
    cosT = np.cos(ang).astype(np.float32).T
    sinT = np.sin(ang).astype(np.float32).T
    t["ropec"] = np.ascontiguousarray(np.concatenate([cosT, cosT], 0))
    t["ropes"] = np.ascontiguousarray(np.concatenate([sinT, sinT], 0))
    nkc, nqt = cfg.NCH, cfg.NQT
    mb = np.zeros((nkc, nqt), np.float32)
    for kc in range(nkc):
        for qt in range(nqt):
            if (kc * 128) // RS != (qt * cfg.QT) // RS:
                mb[kc, qt] = NEG
    t["maskb"] = np.ascontiguousarray(np.broadcast_to(mb.reshape(1, -1), (128, nkc * nqt))).astype(np.float32)
    cf = np.ones(nkc, np.float32)
    cb = np.ones(nkc, np.float32)
    for c in range(nkc):
        if ((c + 1) * 128) % RS == 0:
            cf[c] = 0.0
        if (c * 128) % RS == 0:
            cb[c] = 0.0
    t["cmask"] = np.ascontiguousarray(np.broadcast_to(np.concatenate([cf, cb]).reshape(1, -1), (128, 2 * nkc))).astype(np.float32)
    i = np.arange(128)
    t["tri"] = (i[:, None] <= i[None, :]).astype(np.float32)
    t["trib"] = (i[:, None] >= i[None, :]).astype(np.float32)
    t["ustrict"] = (i[:, None] < i[None, :]).astype(np.float32)
    t["ident"] = np.eye(128, dtype=np.float32)
    gm = np.zeros((128, 4), np.float32)
    for r in range(128):
        gm[r, (r % 64) // 16] = 1.0
    t["gmask"] = gm
    tok0 = (np.arange(cfg.NTILE)[None, :] * 128 + np.arange(128)[:, None]).astype(np.float32)
    t["tok0"] = tok0
    kper = 128 // cfg.N2E if cfg.N2E <= 128 else 1
    tok1 = np.zeros((128, cfg.NTILE), np.float32)
    ntile_seq = cfg.NCH
    for s in range(NSEQ):
        for q in range(ntile_seq):
            for p in range(128):
                k1 = q * kper + p // cfg.N2E
                jj = p % cfg.N2E
                tok1[p, s * ntile_seq + q] = s * SL + k1 + 128 * jj
    t["tok1"] = tok1
    cfg.KPER = kper
    N2, N2E = cfg.N2, cfg.N2E
    fidx = np.zeros((128, NSEQ * N2E), np.int32)
    twr = np.zeros((128, N2E), np.float32)
    twi = np.zeros((128, N2E), np.float32)
    for j in range(N2E):
        sh, t2 = j // N2, j % N2
        for s in range(NSEQ):
            fidx[:, s * N2E + j] = s * SL + sh * RS + N2 * np.arange(128) + t2
        a = 2.0 * np.pi * t2 * np.arange(128) / RS
        twr[:, j] = np.cos(a)
        twi[:, j] = -np.sin(a)
    t["fidx"] = fidx
    t["twr"] = twr
    t["twi"] = twi
    a1 = 2.0 * np.pi * np.outer(np.arange(128), np.arange(128)) / 128.0
    t["c1"] = (np.cos(a1) / np.sqrt(128.0)).astype(np.float32)
    t["s1"] = (np.sin(a1) / np.sqrt(128.0)).astype(np.float32)
    c2 = np.zeros((N2E, N2E), np.float64)
    s2 = np.zeros((N2E, N2E), np.float64)
    for j in range(N2E):
        for jp in range(N2E):
            if j // N2 == jp // N2:
                a = 2.0 * np.pi * (j % N2) * (jp % N2) / N2
                c2[j, jp] = np.cos(a) / np.sqrt(N2)
                s2[j, jp] = np.sin(a) / np.sqrt(N2)
    c2p = np.zeros((N2E, kper, 128), np.float32)
    s2p = np.zeros((N2E, kper, 128), np.float32)
    for v in range(kper):
        c2p[:, v, v * N2E:(v + 1) * N2E] = c2
        s2p[:, v, v * N2E:(v + 1) * N2E] = s2
    t["c2p"] = c2p.reshape(N2E, kper * 128)
    t["s2p"] = s2p.reshape(N2E, kper * 128)
    ac = 2.0 * np.pi * np.outer(np.arange(256), np.arange(256)) / 256.0
    cc = (np.cos(ac) / 16.0).astype(np.float32)
    sc = (-np.sin(ac) / 16.0).astype(np.float32)
    t["cc2"] = np.ascontiguousarray(cc.reshape(2, 128, 2, 128).transpose(1, 0, 2, 3)).reshape(128, 512)
    t["sc2"] = np.ascontiguousarray(sc.reshape(2, 128, 2, 128).transpose(1, 0, 2, 3)).reshape(128, 512)
    return t


from contextlib import ExitStack


PHASE_GROUPS = (("A1",), ("A2",), ("A3",), ("A4", "B0"), ("C0",), ("D0",), ("F", "B1"), ("C1",), ("D1",))


class Prog:
    def __init__(self, cfg, dbg=()):
        self.cfg = cfg
        self.dbg = set(dbg)
        self.nc = bass.Bass("TRN2", target_bir_lowering=False)
        self.in_names = []
        self.out_names = []

    def din(self, name, shape, dt=F32):
        self.in_names.append(name)
        return self.nc.dram_tensor(name, list(shape), dt, kind="ExternalInput")

    def dscr(self, name, shape, dt):
        if name in self.dbg:
            self.out_names.append(name)
            return self.nc.dram_tensor(name, list(shape), dt, kind="ExternalOutput")
        return self.nc.dram_tensor(name, list(shape), dt)

    def dout(self, name, shape, dt=F32):
        self.out_names.append(name)
        return self.nc.dram_tensor(name, list(shape), dt, kind="ExternalOutput")


def bcast_rows(ap2d, nparts):
    return ap2d.partition_broadcast(nparts)


def build(cfg, dbg=(), stop_after=None, skip=()):
    P = Prog(cfg, dbg)
    P.skip = set(skip)
    nc = P.nc
    NT, SL, NSEQ, NCH = cfg.NT, cfg.SL, cfg.NSEQ, cfg.NCH
    x_in = P.din("x", [NT, D])
    w_in = P.din("w_in", [D, 2048])
    lamv = P.din("lamv", [4, 64])
    subln_g = P.din("subln_g", [128, 1])
    s5_a_re = P.din("s5_a_re", [2, 32, 64])
    s5_a_im = P.din("s5_a_im", [2, 32, 64])
    s5_log_dt = P.din("s5_log_dt", [2, 32, 1])
    s5_b_re = P.din("s5_b_re", [2, 32, 64, 16])
    s5_b_im = P.din("s5_b_im", [2, 32, 64, 16])
    s5_c_re = P.din("s5_c_re", [2, 512, 64])
    s5_c_im = P.din("s5_c_im", [2, 512, 64])
    s5_d = P.din("s5_d", [128, 4])
    s5_glu_w = P.din("s5_glu_w", [512, 512])
    s5_glu_b = P.din("s5_glu_b", [128, 4])
    w_out_even = P.din("w_out_even", [D, D])
    w_out_odd = P.din("w_out_odd", [D, D])
    ln_mix_g = P.din("ln_mix_g", [2, D])
    ln_mix_b = P.din("ln_mix_b", [2, D])
    w_router = P.din("w_router", [2, D, NE])
    if stop_after in ("A1", "A2", "A3", "A4", "B0"):
        w_ff1 = w_ff3 = w_ff2 = None
    else:
        w_ff1 = P.din("w_ff1", [2, NE, D, DFF])
        w_ff3 = P.din("w_ff3", [2, NE, D, DFF])
        w_ff2 = P.din("w_ff2", [2, NE, DFF, D])
    ln_ffn_g = P.din("ln_ffn_g", [2, D])
    ln_ffn_b = P.din("ln_ffn_b", [2, D])
    T = {}
    tabs = const_tables(cfg)
    for name, arr in tabs.items():
        T[name] = P.din("t_" + name, arr.shape, I32 if arr.dtype == np.int32 else F32)
    P.tabs = tabs
    y_out = P.dout("y", [NT, D])
    QKT = P.dscr("QKT", [8, 128, NT], BF16)
    VV = P.dscr("VV", [NT, 512], BF16)
    UT = P.dscr("UT", [4, 128, NT], BF16)
    ATT = P.dscr("ATT", [4, 128, NT], BF16)
    SSM = P.dscr("SSM", [4, 128, NT], BF16)
    YB = P.dscr("YB", [4, 128, NT], F32)
    XLN = P.dscr("XLN", [NT, D], F32)
    XBF = P.dscr("XBF", [NT + 128, D], BF16)
    YY = P.dscr("YY", [NT, D], F32)
    X1 = P.dscr("X1", [NT, D], F32)
    IDX = [P.dscr("IDX%d" % i, [cfg.CAP + 128, 2], F32) for i in range(NE)]
    AFS = P.dscr("AFS", [128, cfg.N2E, 2, D], BF16)
    AFFD = P.dscr("AFFD", [128, cfg.NTILE * NE], F32)

    with ExitStack() as root:
        k = K(nc, root)
        e_ = k.engs

        ident_f = k.sb("ident_f", [128, 128], F32)
        ident_b = k.sb("ident_b", [128, 128], BF16)
        ones_b = k.sb("ones_b", [128, 128], BF16)
        ones_f = k.sb("ones_f", [128, 128], F32)
        B_const = Buf("const")
        k.dma("sp", [], [B_const], lambda e: e.dma_start(out=ident_f[:, :], in_=T["ident"][:, :]))
        k.op("dve", [B_const], [B_const], lambda e: e.tensor_copy(out=ident_b[:, :], in_=ident_f[:, :]))
        k.op("dve", [], [B_const], lambda e: e.memset(ones_b[:, :], 1.0))
        k.op("dve", [], [B_const], lambda e: e.memset(ones_f[:, :], 1.0))

        bnd_reg = nc.gpsimd.alloc_register("bnd")
        nc.gpsimd.reg_mov(bnd_reg, cfg.CAP - 1)
        L = dict(locals())
        for group in PHASE_GROUPS:
            with ExitStack() as gs:
                k.stack = gs
                if group[0] in ("A4", "F"):
                    L["aff_sb"] = k.sb("aff_sb", [128, cfg.NTILE, NE], F32)
                    L["Baff"] = Buf("aff")
                for name in group:
                    fn = globals().get("phase_" + name)
                    if name in P.skip or fn is None:
                        continue
                    fn(P, k, L)
                    k.rotate()
                    if stop_after == name:
                        k.barrier()
                        k.stack = root
                        return P
            k.stack = root
        k.barrier()
    return P


class Ring:
    def __init__(self, k, name, shape, dt, n, psum=False):
        self.t = []
        self.b = []
        for i in range(n):
            nm = "%s%d" % (name, i)
            self.t.append(k.ps(nm, shape, dt) if psum else k.sb(nm, shape, dt))
            self.b.append(Buf(nm))
        self.i = 0
        self.n = n

    def next(self):
        i = self.i
        self.i = (self.i + 1) % self.n
        return self.t[i], self.b[i]


def phase_A1(P, k, L):
    cfg = P.cfg
    x_in, w_in, QKT, VV, UT, T = L["x_in"], L["w_in"], L["QKT"], L["VV"], L["UT"], L["T"]
    ident_b, root = L["ident_b"], L["root"]
    NT, SL = cfg.NT, cfg.SL
    prev = k.stack
    with ExitStack() as st:
        k.stack = st
        wall = k.sb("wall", [128, DK, 3072], BF16)
        Bw = Buf("wall")
        w_v = w_in.ap().rearrange("(dk p) f -> p dk f", p=128)
        k.dma("pool", [], [Bw], lambda e: e.dma_start(out=wall[:, :, 0:1024], in_=w_v[:, :, 0:1024]))
        k.dma("pool", [], [Bw], lambda e: e.dma_start(out=wall[:, :, 2048:3072], in_=w_v[:, :, 1024:2048]))
        for dk in range(DK):
            dst = wall[:, dk, 1024:2048].rearrange("p (b h i) -> p b h i", h=2, i=32)
            src = w_v[:, dk, 0:1024].rearrange("p (b h i) -> p b h i", h=2, i=32)
            k.dma("pool", [], [Bw], lambda e, d=dst, s=src: e.dma_start(out=d[:, :, 0, :], in_=s[:, :, 1, :]))
            k.dma("pool", [], [Bw], lambda e, d=dst, s=src: e.dma_start(out=d[:, :, 1, :], in_=s[:, :, 0, :]))
        for dk in range(DK):
            v = wall[:, dk, 1024:2048].rearrange("p (b h i) -> p b h i", h=2, i=32)[:, :, 0, :]
            k.op("dve", [Bw], [Bw], lambda e, v=v: e.tensor_scalar(out=v, in0=v, scalar1=-1.0, scalar2=None, op0=ALU.mult))

        xb = Ring(k, "a1_xb", [128, 4, D], BF16, 2)
        xT = Ring(k, "a1_xT", [128, DK, 512], BF16, 2)
        cs = Ring(k, "a1_cs", [128, 2, 512], F32, 2)
        t12 = Ring(k, "a1_t12", [128, 2, 512], F32, 2)
        qko = Ring(k, "a1_qko", [128, 512], BF16, 3)
        vo = Ring(k, "a1_vo", [128, 4, 512], BF16, 2)
        uo = Ring(k, "a1_uo", [128, 512], BF16, 3)
        pst = Ring(k, "a1_pst", [128, 512], BF16, 2, psum=True)
        psA = Ring(k, "a1_psA", [128, 512], F32, 2, psum=True)
        psB = Ring(k, "a1_psB", [128, 512], F32, 2, psum=True)
        psC = Ring(k, "a1_psC", [128, 512], F32, 2, psum=True)

        ntile = NT // 512
        for tt in range(ntile):
            t0 = tt * 512
            p0 = t0 % SL
            xbt, xbb = xb.next()
            k.dma("pool", [], [xbb], lambda e: e.dma_start(
                out=xbt[:, :, :], in_=x_in[t0:t0 + 512, :].rearrange("(j p) d -> p j d", p=128)))
            cst, csb = cs.next()
            k.dma("sp", [], [csb], lambda e: e.dma_start(out=cst[:, 0, :], in_=T["ropec"][:, p0:p0 + 512]))
            k.dma("sp", [], [csb], lambda e: e.dma_start(out=cst[:, 1, :], in_=T["ropes"][:, p0:p0 + 512]))
            xTt, xTb = xT.next()
            for dk in range(DK):
                pt, ptb = pst.next()

                def tr(e, pt=pt, dk=dk):
                    for j in range(4):
                        ins = e.transpose(out=pt[:, j * 128:(j + 1) * 128], in_=xbt[:, j, dk * 128:(dk + 1) * 128],
                                          identity=ident_b[:, :])
                    return ins
                k.op("pe", [xbb], [ptb], tr)
                if dk % 2 == 0:
                    k.op("act", [ptb], [xTb], lambda e, pt=pt, dk=dk: e.copy(out=xTt[:, dk, :], in_=pt[:, :]))
                else:
                    k.op("dve", [ptb], [xTb], lambda e, pt=pt, dk=dk: e.tensor_copy(out=xTt[:, dk, :], in_=pt[:, :]))
            for fc in range(8):
                pa, pab = psA.next()
                pb, pbb = psB.next()

                def mmA(e, pa=pa, fc=fc):
                    for dk in range(DK):
                        ins = e.matmul(pa[:, :], wall[:, dk, fc * 128:(fc + 1) * 128], xTt[:, dk, :],
                                       start=(dk == 0), stop=(dk == DK - 1))
                    return ins

                def mmB(e, pb=pb, fc=fc):
                    for dk in range(DK):
                        ins = e.matmul(pb[:, :], wall[:, dk, 1024 + fc * 128:1024 + (fc + 1) * 128], xTt[:, dk, :],
                                       start=(dk == 0), stop=(dk == DK - 1))
                    return ins
                k.op("pe", [Bw, xTb], [pab], mmA)
                k.op("pe", [Bw, xTb], [pbb], mmB)
                tt_, ttb = t12.next()
                k.op("dve", [pab, csb], [ttb], lambda e, pa=pa, tt_=tt_: e.tensor_tensor(
                    out=tt_[:, 0, :], in0=pa[:, :], in1=cst[:, 0, :], op=ALU.mult))
                k.op("dve", [pbb, csb], [ttb], lambda e, pb=pb, tt_=tt_: e.tensor_tensor(
                    out=tt_[:, 1, :], in0=pb[:, :], in1=cst[:, 1, :], op=ALU.mult))
                qo, qob = qko.next()
                k.op("pool", [ttb], [qob], lambda e, qo=qo, tt_=tt_: e.tensor_tensor(
                    out=qo[:, :], in0=tt_[:, 0, :], in1=tt_[:, 1, :], op=ALU.add))
                k.dma("sp", [qob], [], lambda e, qo=qo, fc=fc: e.dma_start(out=QKT[fc, :, t0:t0 + 512], in_=qo[:, :]))
            vt, vb = vo.next()
            for j in range(4):
                pc, pcb = psC.next()

                def mmV(e, pc=pc, j=j):
                    for dk in range(DK):
                        ins = e.matmul(pc[:, :], xTt[:, dk, j * 128:(j + 1) * 128], wall[:, dk, 2048:2560],
                                       start=(dk == 0), stop=(dk == DK - 1))
                    return ins
                k.op("pe", [Bw, xTb], [pcb], mmV)
                k.op("act", [pcb], [vb], lambda e, pc=pc, j=j: e.copy(out=vt[:, j, :], in_=pc[:, :]))
            k.dma("sp", [vb], [], lambda e: e.dma_start(
                out=VV[t0:t0 + 512, :].rearrange("(j p) f -> p j f", p=128), in_=vt[:, :, :]))
            for c in range(4):
                pc, pcb = psC.next()

                def mmU(e, pc=pc, c=c):
                    for dk in range(DK):
                        ins = e.matmul(pc[:, :], wall[:, dk, 2560 + c * 128:2560 + (c + 1) * 128], xTt[:, dk, :],
                                       start=(dk == 0), stop=(dk == DK - 1))
                    return ins
                k.op("pe", [Bw, xTb], [pcb], mmU)
                ut, ub = uo.next()
                k.op("act", [pcb], [ub], lambda e, pc=pc, ut=ut: e.copy(out=ut[:, :], in_=pc[:, :]))
                k.dma("sp", [ub], [], lambda e, ut=ut, c=c: e.dma_start(out=UT[c, :, t0:t0 + 512], in_=ut[:, :]))
        k.barrier()
    k.stack = prev


def core_inputs(cfg, P, x_flat, inp):
    m = {}
    m["x"] = np.ascontiguousarray(x_flat, dtype=np.float32)
    m["w_in"] = inp["w_in"][0]
    m["lamv"] = np.stack([inp["lam_q1"][0], inp["lam_k1"][0], inp["lam_q2"][0], inp["lam_k2"][0]], 0)
    m["subln_g"] = inp["subln_g"][0].reshape(128, 1)
    m["s5_a_re"] = inp["s5_a_re"][0]
    m["s5_a_im"] = inp["s5_a_im"][0]
    m["s5_log_dt"] = inp["s5_log_dt"][0].reshape(2, 32, 1)
    m["s5_b_re"] = inp["s5_b_re"][0]
    m["s5_b_im"] = inp["s5_b_im"][0]
    m["s5_c_re"] = inp["s5_c_re"][0].reshape(2, 512, 64)
    m["s5_c_im"] = inp["s5_c_im"][0].reshape(2, 512, 64)
    m["s5_d"] = inp["s5_d"][0].reshape(4, 128).T
    m["s5_glu_w"] = inp["s5_glu_w"][0]
    m["s5_glu_b"] = inp["s5_glu_b"][0].reshape(4, 128).T
    m["w_out_even"] = inp["w_out_even"][0]
    m["w_out_odd"] = inp["w_out_odd"][0]
    for n in ("ln_mix_g", "ln_mix_b", "w_router", "w_ff1", "w_ff3", "w_ff2", "ln_ffn_g", "ln_ffn_b"):
        m[n] = inp[n]
    for n, arr in P.tabs.items():
        m["t_" + n] = arr
    out = {}
    for n in P.in_names:
        out[n] = np.ascontiguousarray(m[n])
    return out


def phase_A2(P, k, L):
    cfg = P.cfg
    QKT, VV, ATT, T = L["QKT"], L["VV"], L["ATT"], L["T"]
    lamv, subln_g = L["lamv"], L["subln_g"]
    ones_b, root = L["ones_b"], L["root"]
    SL, NSEQ, NCH, QT, NQT = cfg.SL, cfg.NSEQ, cfg.NCH, cfg.QT, cfg.NQT
    lambda_init = 0.8 - 0.6 * math.exp(0.0)
    prev = k.stack
    with ExitStack() as st:
        k.stack = st
        lv = k.sb("a2_lv", [128, 4, 64], F32)
        Bs = Buf("a2_scal")
        k.dma("sp", [], [Bs], lambda e: e.dma_start(
            out=lv[:, :, :].rearrange("p a b -> p (a b)"),
            in_=lamv.ap().rearrange("a b -> (a b)").partition_broadcast(128)))
        pr = k.sb("a2_pr", [128, 2, 64], F32)
        sm = k.sb("a2_sm", [128, 4], F32)
        k.op("dve", [Bs], [Bs], lambda e: e.tensor_tensor(out=pr[:, 0, :], in0=lv[:, 0, :], in1=lv[:, 1, :], op=ALU.mult))
        k.op("dve", [Bs], [Bs], lambda e: e.tensor_tensor(out=pr[:, 1, :], in0=lv[:, 2, :], in1=lv[:, 3, :], op=ALU.mult))
        k.op("dve", [Bs], [Bs], lambda e: e.reduce_sum(out=sm[:, 0:1], in_=pr[:, 0, :], axis=AX.X))
        k.op("dve", [Bs], [Bs], lambda e: e.reduce_sum(out=sm[:, 1:2], in_=pr[:, 1, :], axis=AX.X))
        k.op("act", [Bs], [Bs], lambda e: e.activation(out=sm[:, 2:4], in_=sm[:, 0:2], func=AF.Exp))
        nlam = k.sb("a2_nlam", [128, 1], F32)
        k.op("dve", [Bs], [Bs], lambda e: e.tensor_tensor(out=nlam[:, :], in0=sm[:, 3:4], in1=sm[:, 2:3], op=ALU.subtract))
        k.op("dve", [Bs], [Bs], lambda e: e.tensor_scalar(out=nlam[:, :], in0=nlam[:, :], scalar1=-lambda_init, scalar2=None, op0=ALU.add))
        gsc = k.sb("a2_gsc", [128, 1], F32)
        k.dma("sp", [], [Bs], lambda e: e.dma_start(out=gsc[:, :], in_=subln_g[:, :]))
        k.op("dve", [Bs], [Bs], lambda e: e.tensor_scalar(out=gsc[:, :], in0=gsc[:, :], scalar1=1.0 - lambda_init, scalar2=None, op0=ALU.mult))
        mb = k.sb("a2_mb", [128, NCH * NQT], F32)
        k.dma("sp", [], [Bs], lambda e: e.dma_start(out=mb[:, :], in_=T["maskb"][:, :]))
        epsb = k.sb("a2_eps", [128, 1], F32)
        k.op("dve", [], [Bs], lambda e: e.memset(epsb[:, :], LN_EPS))

        qT = Ring(k, "a2_qT", [128, SL], BF16, 2)
        kT = Ring(k, "a2_kT", [128, SL], BF16, 2)
        vh = Ring(k, "a2_vh", [128, NCH, 128], BF16, 2)
        ps_s = Ring(k, "a2_pss", [128, 2, 512], F32, 2, psum=True)
        ps_o1 = k.ps("a2_o1", [128, 512]); Bo1 = Buf("o1")
        ps_o2 = k.ps("a2_o2", [128, 512]); Bo2 = Buf("o2")
        ps_z1 = k.ps("a2_z1", [128, 512]); Bz1 = Buf("z1")
        ps_z2 = k.ps("a2_z2", [128, 512]); Bz2 = Buf("z2")
        et = Ring(k, "a2_e", [128, 2, 512], BF16, 3)
        r12 = k.sb("a2_r12", [128, 2, 512], F32); Br = Buf("r12")
        ab = k.sb("a2_ab", [128, 2, 512], F32); Bab = Buf("ab")
        osb = k.sb("a2_o", [128, 512], F32); Bosb = Buf("osb")
        sq = k.sb("a2_sq", [128, 512], BF16); Bsq = Buf("sq")
        rstd = k.sb("a2_rstd", [128, 512], F32); Brs = Buf("rstd")
        ao = Ring(k, "a2_ao", [128, 512], BF16, 2)

        for s in range(NSEQ):
            for h in range(4):
                base = s * SL
                qt_, qb = qT.next()
                kt_, kb = kT.next()
                vt_, vb = vh.next()
                k.dma("sp", [], [qb], lambda e: e.dma_start(out=qt_[:, :], in_=QKT[h, :, base:base + SL]))
                k.dma("sp", [], [kb], lambda e: e.dma_start(out=kt_[:, :], in_=QKT[4 + h, :, base:base + SL]))
                nsp = 4 if NCH >= 16 else 1
                cpp = NCH // nsp
                for sp_ in range(nsp):
                    k.dma("sp", [], [vb], lambda e, sp_=sp_: e.dma_start(
                        out=vt_[:, sp_ * cpp:(sp_ + 1) * cpp, :],
                        in_=VV[base + sp_ * cpp * 128:base + (sp_ + 1) * cpp * 128, h * 128:(h + 1) * 128].rearrange("(c p) f -> p c f", p=128)))
                for qt in range(NQT):
                    q0 = qt * QT
                    pend = None
                    for kc in range(NCH + 1):
                        if kc < NCH:
                            pss, psb = ps_s.next()

                            def mmS(e, pss=pss, kc=kc):
                                e.matmul(pss[:, 0, 0:QT], kt_[0:64, kc * 128:(kc + 1) * 128], qt_[0:64, q0:q0 + QT],
                                         start=True, stop=True)
                                return e.matmul(pss[:, 1, 0:QT], kt_[64:128, kc * 128:(kc + 1) * 128],
                                                qt_[64:128, q0:q0 + QT], start=True, stop=True)
                            k.op("pe", [qb, kb], [psb], mmS)
                            ee, eb = et.next()
                            mcol = mb[:, kc * NQT + qt:kc * NQT + qt + 1]
                            k.op("act", [psb, Bs], [eb], lambda e, pss=pss, ee=ee, mcol=mcol: e.activation(
                                out=ee[:, :, 0:QT], in_=pss[:, :, 0:QT], func=AF.Exp, bias=mcol, scale=0.125))
                            cur = (ee, eb, kc)
                        else:
                            cur = None
                        if pend is not None:
                            pe_, peb, pkc = pend

                            def mmPV(e, pe_=pe_, pkc=pkc):
                                st_, sp_ = (pkc == 0), (pkc == NCH - 1)
                                e.matmul(ps_o1[:, 0:QT], vt_[:, pkc, :], pe_[:, 0, 0:QT], start=st_, stop=sp_)
                                e.matmul(ps_z1[:, 0:QT], ones_b[:, :], pe_[:, 0, 0:QT], start=st_, stop=sp_)
                                e.matmul(ps_o2[:, 0:QT], vt_[:, pkc, :], pe_[:, 1, 0:QT], start=st_, stop=sp_)
                                return e.matmul(ps_z2[:, 0:QT], ones_b[:, :], pe_[:, 1, 0:QT], start=st_, stop=sp_)
                            k.op("pe", [vb, peb], [Bo1, Bo2, Bz1, Bz2], mmPV)
                        pend = cur
                    k.op("dve", [Bz1], [Br], lambda e: e.reciprocal(out=r12[:, 0, 0:QT], in_=ps_z1[:, 0:QT]))
                    k.op("dve", [Bz2], [Br], lambda e: e.reciprocal(out=r12[:, 1, 0:QT], in_=ps_z2[:, 0:QT]))
                    k.op("dve", [Bo1, Br], [Bab], lambda e: e.tensor_tensor(out=ab[:, 0, 0:QT], in0=ps_o1[:, 0:QT], in1=r12[:, 0, 0:QT], op=ALU.mult))
                    k.op("dve", [Bo2, Br], [Bab], lambda e: e.tensor_tensor(out=ab[:, 1, 0:QT], in0=ps_o2[:, 0:QT], in1=r12[:, 1, 0:QT], op=ALU.mult))
                    k.op("dve", [Bab, Bs], [Bosb], lambda e: e.scalar_tensor_tensor(
                        out=osb[:, 0:QT], in0=ab[:, 1, 0:QT], scalar=nlam[:, 0:1], in1=ab[:, 0, 0:QT], op0=ALU.mult, op1=ALU.add))
                    k.op("pool", [Bosb], [Bsq], lambda e: e.tensor_tensor(out=sq[:, 0:QT], in0=osb[:, 0:QT], in1=osb[:, 0:QT], op=ALU.mult))
                    k.op("pe", [Bsq], [Bz1], lambda e: e.matmul(ps_z1[:, 0:QT], ones_b[:, :], sq[:, 0:QT], start=True, stop=True))
                    k.op("act", [Bz1, Bs], [Brs], lambda e: e.activation(out=rstd[:, 0:QT], in_=ps_z1[:, 0:QT], func=AF.Sqrt,
                                                                         bias=epsb[:, 0:1], scale=1.0 / 128.0))
                    k.op("dve", [Brs], [Brs], lambda e: e.reciprocal(out=rstd[:, 0:QT], in_=rstd[:, 0:QT]))
                    aot, aob = ao.next()
                    k.op("dve", [Bosb, Brs, Bs], [aob], lambda e: e.scalar_tensor_tensor(
                        out=aot[:, 0:QT], in0=osb[:, 0:QT], scalar=gsc[:, 0:1], in1=rstd[:, 0:QT], op0=ALU.mult, op1=ALU.mult))
                    import os
                    dm = os.environ.get("A2DBG", "")
                    if dm == "a":
                        k.op("dve", [Bab], [aob], lambda e: e.tensor_copy(out=aot[:, 0:QT], in_=ab[:, 0, 0:QT]))
                    elif dm == "b":
                        k.op("dve", [Bab], [aob], lambda e: e.tensor_copy(out=aot[:, 0:QT], in_=ab[:, 1, 0:QT]))
                    elif dm == "o":
                        k.op("dve", [Bosb], [aob], lambda e: e.tensor_copy(out=aot[:, 0:QT], in_=osb[:, 0:QT]))
                    elif dm == "n":
                        k.op("dve", [Bosb, Bs], [aob], lambda e: e.tensor_scalar(out=aot[:, 0:QT], in0=osb[:, 0:QT], scalar1=0.0, scalar2=nlam[:, 0:1], op0=ALU.mult, op1=ALU.add))
                    elif dm == "s":
                        k.op("dve", [Bosb, Bs], [aob], lambda e: e.tensor_scalar(out=aot[:, 0:QT], in0=osb[:, 0:QT], scalar1=0.0, scalar2=sm[:, int(os.environ.get("SMI", "0")):int(os.environ.get("SMI", "0")) + 1], op0=ALU.mult, op1=ALU.add))
                    elif dm == "r":
                        k.op("dve", [Brs], [aob], lambda e: e.tensor_copy(out=aot[:, 0:QT], in_=rstd[:, 0:QT]))
                    k.dma("sp", [aob], [], lambda e: e.dma_start(out=ATT[h, :, base + q0:base + q0 + QT], in_=aot[:, 0:QT]))
        k.barrier()
    k.stack = prev


def _cmul_bcast(k, eng, Bt, outr, outi, ar, ai, sr, si, tmp, shape):
    t1, t2, t3, t4 = tmp
    k.op(eng, [Bt], [Bt], lambda e: e.tensor_tensor(out=t1, in0=ar, in1=sr, op=ALU.mult))
    k.op(eng, [Bt], [Bt], lambda e: e.tensor_tensor(out=t2, in0=ai, in1=si, op=ALU.mult))
    k.op(eng, [Bt], [Bt], lambda e: e.tensor_tensor(out=t3, in0=ar, in1=si, op=ALU.mult))
    k.op(eng, [Bt], [Bt], lambda e: e.tensor_tensor(out=t4, in0=ai, in1=sr, op=ALU.mult))
    k.op(eng, [Bt], [Bt], lambda e: e.tensor_tensor(out=outr, in0=t1, in1=t2, op=ALU.subtract))
    k.op(eng, [Bt], [Bt], lambda e: e.tensor_tensor(out=outi, in0=t3, in1=t4, op=ALU.add))


def phase_A3(P, k, L):
    cfg = P.cfg
    UT, SSM, YB, T = L["UT"], L["SSM"], L["YB"], L["T"]
    ident_f, ones_b, root = L["ident_f"], L["ones_b"], L["root"]
    SL, NSEQ, NCH = cfg.SL, cfg.NSEQ, cfg.NCH
    prev = k.stack
    with ExitStack() as st:
        k.stack = st
        Bt = Buf("a3_setup")
        pst = k.ps("a3_pst", [128, 128], F32); Bpst = Buf("a3_pst")
        Ppos = [[k.sb("a3_pp%d%d" % (d, r), [128, 16, 128], F32) for r in range(2)] for d in range(2)]
        Wneg = [[k.sb("a3_wn%d%d" % (d, r), [128, 16, 128], F32) for r in range(2)] for d in range(2)]
        Bblk = [[k.sb("a3_bb%d%d" % (d, c), [128, 512], BF16) for c in range(4)] for d in range(2)]
        Cpad = [k.sb("a3_cp%d" % d, [128, 16, 2, 128], BF16) for d in range(2)]
        dskip = k.sb("a3_dsk", [128, 4], F32)
        glub = k.sb("a3_glub", [128, 4], F32)
        gluw = k.sb("a3_gluw", [128, 4, 512], BF16)
        tri = [k.sb("a3_tri%d" % d, [128, 128], BF16) for d in range(2)]
        cm = k.sb("a3_cm", [128, 2 * NCH], F32)
        k.dma("sp", [], [Bt], lambda e: e.dma_start(out=dskip[:, :], in_=L["s5_d"][:, :]))
        k.dma("sp", [], [Bt], lambda e: e.dma_start(out=glub[:, :], in_=L["s5_glu_b"][:, :]))
        k.dma("pool", [], [Bt], lambda e: e.dma_start(out=gluw[:, :, :], in_=L["s5_glu_w"].ap().rearrange("(c p) o -> p c o", p=128)))
        k.dma("pool", [], [Bt], lambda e: e.dma_start(out=tri[0][:, :], in_=T["tri"][:, :]))
        k.dma("pool", [], [Bt], lambda e: e.dma_start(out=tri[1][:, :], in_=T["trib"][:, :]))
        k.dma("sp", [], [Bt], lambda e: e.dma_start(out=cm[:, :], in_=T["cmask"][:, :]))
        with ExitStack() as st2:
            k.stack = st2
            tmpA = k.sb("a3_tmpA", [128, 4, 16, 128], F32)
            Pneg = [[k.sb("a3_pn%d%d" % (d, r), [128, 16, 128], F32) for r in range(2)] for d in range(2)]
            for d in range(2):
                are = k.sb("a3_are%d" % d, [16, 128], F32)
                aim = k.sb("a3_aim%d" % d, [16, 128], F32)
                ldt = k.sb("a3_ldt%d" % d, [16, 2], F32)
                k.dma("sp", [], [Bt], lambda e: e.dma_start(out=are[:, :], in_=L["s5_a_re"][d].rearrange("(c g) p -> c (g p)", g=2)))
                k.dma("sp", [], [Bt], lambda e: e.dma_start(out=aim[:, :], in_=L["s5_a_im"][d].rearrange("(c g) p -> c (g p)", g=2)))
                k.dma("sp", [], [Bt], lambda e: e.dma_start(out=ldt[:, :], in_=L["s5_log_dt"][d].rearrange("(c g) o -> c (g o)", g=2)))
                dt = k.sb("a3_dt%d" % d, [16, 2], F32)
                k.op("act", [Bt], [Bt], lambda e: e.activation(out=dt[:, :], in_=ldt[:, :], func=AF.Exp))
                wk = k.sb("a3_wk%d" % d, [16, 12, 128], F32)
                dtb = dt[:, :].unsqueeze(2).to_broadcast([16, 2, 64])

                def v3(i):
                    return wk[:, i, :].rearrange("c (g p) -> c g p", g=2)
                k.op("dve", [Bt], [Bt], lambda e: e.tensor_tensor(out=v3(0), in0=are[:, :].rearrange("c (g p) -> c g p", g=2), in1=dtb, op=ALU.mult))
                k.op("dve", [Bt], [Bt], lambda e: e.tensor_tensor(out=v3(1), in0=aim[:, :].rearrange("c (g p) -> c g p", g=2), in1=dtb, op=ALU.mult))
                k.op("dve", [Bt], [Bt], lambda e: e.tensor_scalar(out=wk[:, 1, :], in0=wk[:, 1, :], scalar1=1.0 / 16.0, scalar2=None, op0=ALU.mult))
                hp = k.sb("a3_hp%d" % d, [16, 1], F32)
                k.op("dve", [], [Bt], lambda e: e.memset(hp[:, :], math.pi / 2.0))
                k.op("act", [Bt], [Bt], lambda e: e.activation(out=wk[:, 2, :], in_=wk[:, 0, :], func=AF.Exp))
                k.op("act", [Bt], [Bt], lambda e: e.activation(out=wk[:, 3, :], in_=wk[:, 1, :], func=AF.Sin))
                k.op("act", [Bt], [Bt], lambda e: e.activation(out=wk[:, 4, :], in_=wk[:, 1, :], func=AF.Sin, bias=hp[:, 0:1]))
                for _ in range(4):
                    k.op("dve", [Bt], [Bt], lambda e: e.tensor_tensor(out=wk[:, 5, :], in0=wk[:, 4, :], in1=wk[:, 4, :], op=ALU.mult))
                    k.op("dve", [Bt], [Bt], lambda e: e.tensor_tensor(out=wk[:, 6, :], in0=wk[:, 3, :], in1=wk[:, 3, :], op=ALU.mult))
                    k.op("dve", [Bt], [Bt], lambda e: e.tensor_tensor(out=wk[:, 7, :], in0=wk[:, 3, :], in1=wk[:, 4, :], op=ALU.mult))
                    k.op("dve", [Bt], [Bt], lambda e: e.tensor_tensor(out=wk[:, 4, :], in0=wk[:, 5, :], in1=wk[:, 6, :], op=ALU.subtract))
                    k.op("dve", [Bt], [Bt], lambda e: e.tensor_scalar(out=wk[:, 3, :], in0=wk[:, 7, :], scalar1=2.0, scalar2=None, op0=ALU.mult))
                k.op("dve", [Bt], [Bt], lambda e: e.tensor_tensor(out=wk[:, 5, :], in0=wk[:, 2, :], in1=wk[:, 4, :], op=ALU.mult))
                k.op("dve", [Bt], [Bt], lambda e: e.tensor_tensor(out=wk[:, 6, :], in0=wk[:, 2, :], in1=wk[:, 3, :], op=ALU.mult))
                k.op("dve", [Bt], [Bt], lambda e: e.tensor_scalar(out=wk[:, 7, :], in0=wk[:, 5, :], scalar1=-1.0, scalar2=None, op0=ALU.add))
                k.op("dve", [Bt], [Bt], lambda e: e.tensor_tensor(out=wk[:, 8, :], in0=are[:, :], in1=are[:, :], op=ALU.mult))
                k.op("dve", [Bt], [Bt], lambda e: e.tensor_tensor(out=wk[:, 9, :], in0=aim[:, :], in1=aim[:, :], op=ALU.mult))
                k.op("dve", [Bt], [Bt], lambda e: e.tensor_tensor(out=wk[:, 8, :], in0=wk[:, 8, :], in1=wk[:, 9, :], op=ALU.add))
                k.op("dve", [Bt], [Bt], lambda e: e.reciprocal(out=wk[:, 8, :], in_=wk[:, 8, :]))
                k.op("dve", [Bt], [Bt], lambda e: e.tensor_tensor(out=wk[:, 9, :], in0=wk[:, 7, :], in1=are[:, :], op=ALU.mult))
                k.op("dve", [Bt], [Bt], lambda e: e.tensor_tensor(out=wk[:, 10, :], in0=wk[:, 6, :], in1=aim[:, :], op=ALU.mult))
                k.op("dve", [Bt], [Bt], lambda e: e.tensor_tensor(out=wk[:, 9, :], in0=wk[:, 9, :], in1=wk[:, 10, :], op=ALU.add))
                k.op("dve", [Bt], [Bt], lambda e: e.tensor_tensor(out=wk[:, 9, :], in0=wk[:, 9, :], in1=wk[:, 8, :], op=ALU.mult))
                k.op("dve", [Bt], [Bt], lambda e: e.tensor_tensor(out=wk[:, 10, :], in0=wk[:, 6, :], in1=are[:, :], op=ALU.mult))
                k.op("dve", [Bt], [Bt], lambda e: e.tensor_tensor(out=wk[:, 11, :], in0=wk[:, 7, :], in1=aim[:, :], op=ALU.mult))
                k.op("dve", [Bt], [Bt], lambda e: e.tensor_tensor(out=wk[:, 10, :], in0=wk[:, 10, :], in1=wk[:, 11, :], op=ALU.subtract))
                k.op("dve", [Bt], [Bt], lambda e: e.tensor_tensor(out=wk[:, 10, :], in0=wk[:, 10, :], in1=wk[:, 8, :], op=ALU.mult))
                k.op("dve", [Bt], [Bt], lambda e: e.tensor_tensor(out=wk[:, 0, :], in0=wk[:, 2, :], in1=wk[:, 2, :], op=ALU.mult))
                k.op("dve", [Bt], [Bt], lambda e: e.reciprocal(out=wk[:, 0, :], in_=wk[:, 0, :]))
                k.op("dve", [Bt], [Bt], lambda e: e.tensor_tensor(out=wk[:, 7, :], in0=wk[:, 5, :], in1=wk[:, 0, :], op=ALU.mult))
                k.op("dve", [Bt], [Bt], lambda e: e.tensor_tensor(out=wk[:, 11, :], in0=wk[:, 6, :], in1=wk[:, 0, :], op=ALU.mult))
                k.op("dve", [Bt], [Bt], lambda e: e.tensor_scalar(out=wk[:, 11, :], in0=wk[:, 11, :], scalar1=-1.0, scalar2=None, op0=ALU.mult))
                lamT = k.sb("a3_lamT%d" % d, [128, 6, 16], F32)
                for oi, wi in enumerate((5, 6, 7, 11, 9, 10)):
                    k.op("pe", [Bt], [Bpst], lambda e, wi=wi: e.transpose(out=pst[:, 0:16], in_=wk[:, wi, :], identity=ident_f[0:16, 0:16]))
                    k.op("act", [Bpst], [Bt], lambda e, oi=oi: e.copy(out=lamT[:, oi, :], in_=pst[:, 0:16]))
                for tabs_, ri0 in ((Ppos[d], 0), (Pneg[d], 2)):
                    pr_, pi_ = tabs_
                    first = 0 if d == 0 else 127
                    k.op("dve", [Bt], [Bt], lambda e: e.tensor_copy(out=pr_[:, :, first], in_=lamT[:, ri0, :]))
                    k.op("dve", [Bt], [Bt], lambda e: e.tensor_copy(out=pi_[:, :, first], in_=lamT[:, ri0 + 1, :]))
                    for m in range(7):
                        n = 1 << m
                        if d == 0:
                            src = slice(0, n); dst = slice(n, 2 * n); sc = n - 1
                        else:
                            src = slice(128 - n, 128); dst = slice(128 - 2 * n, 128 - n); sc = 128 - n
                        sr = pr_[:, :, sc:sc + 1].to_broadcast([128, 16, n])
                        si = pi_[:, :, sc:sc + 1].to_broadcast([128, 16, n])
                        tmp = [tmpA[:, i, :, 0:n] for i in range(4)]
                        _cmul_bcast(k, "dve", Bt, pr_[:, :, dst], pi_[:, :, dst], pr_[:, :, src], pi_[:, :, src], sr, si, tmp, None)
                for r in range(2):
                    for ct in range(16):
                        k.op("pe", [Bt], [Bpst], lambda e, r=r, ct=ct: e.transpose(out=pst[:, :], in_=Pneg[d][r][:, ct, :], identity=ident_f[:, :]))
                        k.op("act", [Bpst], [Bt], lambda e, r=r, ct=ct: e.copy(out=Wneg[d][r][:, ct, :], in_=pst[:, :]))
                bre = k.sb("a3_bre%d" % d, [128, 16, 16], F32)
                bim = k.sb("a3_bim%d" % d, [128, 16, 16], F32)
                for gi in range(2):
                    k.dma("sp", [], [Bt], lambda e, gi=gi: e.dma_start(
                        out=bre[gi * 64:(gi + 1) * 64, :, :], in_=L["s5_b_re"][d].rearrange("(c g) p h -> g p c h", g=2)[gi]))
                    k.dma("sp", [], [Bt], lambda e, gi=gi: e.dma_start(
                        out=bim[gi * 64:(gi + 1) * 64, :, :], in_=L["s5_b_im"][d].rearrange("(c g) p h -> g p c h", g=2)[gi]))
                bbr = k.sb("a3_bbr%d" % d, [128, 16, 16], F32)
                bbi = k.sb("a3_bbi%d" % d, [128, 16, 16], F32)
                crb = lamT[:, 4, :].unsqueeze(2).to_broadcast([128, 16, 16])
                cib = lamT[:, 5, :].unsqueeze(2).to_broadcast([128, 16, 16])
                tmp = [tmpA[:, i, :, 0:16] for i in range(4)]
                _cmul_bcast(k, "dve", Bt, bbr[:, :, :], bbi[:, :, :], bre[:, :, :], bim[:, :, :], crb, cib, tmp, None)
                in2 = k.sb("a3_in2%d" % d, [128, 128], F32)
                for c in range(4):
                    k.op("dve", [Bt], [Bt], lambda e, c=c: e.memset(Bblk[d][c][:, :], 0.0))
                    for r, bb in enumerate((bbr, bbi)):
                        for ctl in range(2):
                            k.op("dve", [Bt, Bpst], [Bt], lambda e: e.memset(in2[:, :], 0.0))
                            for gi in range(2):
                                for hf in range(2):
                                    gl = 2 * ctl + gi
                                    ct = 4 * c + 2 * hf + ctl
                                    col = hf * 64 + gl * 16
                                    k.op("dve", [Bt], [Bt], lambda e, gi=gi, ct=ct, col=col, bb=bb: e.tensor_copy(
                                        out=in2[gi * 64:(gi + 1) * 64, col:col + 16], in_=bb[gi * 64:(gi + 1) * 64, ct, :]))
                            k.op("pe", [Bt], [Bpst], lambda e: e.transpose(out=pst[:, :], in_=in2[:, :], identity=ident_f[:, :]))
                            k.op("act", [Bpst], [Bt], lambda e, c=c, r=r, ctl=ctl: e.copy(
                                out=Bblk[d][c][:, (r * 2 + ctl) * 128:(r * 2 + ctl + 1) * 128], in_=pst[:, :]))
                k.op("dve", [Bt], [Bt], lambda e: e.memset(Cpad[d][:, :, :, :], 0.0))
                csb = k.sb("a3_csb%d" % d, [128, 2, 64], F32)
                for r, cten in enumerate((L["s5_c_re"], L["s5_c_im"])):
                    for yt in range(4):
                        for dup in range(2):
                            k.dma("sp", [Bpst], [Bt], lambda e, dup=dup, yt=yt, cten=cten: e.dma_start(
                                out=csb[:, dup, :], in_=cten[d, yt * 128:(yt + 1) * 128, :]))
                        if r == 1:
                            k.op("dve", [Bt], [Bt], lambda e: e.tensor_scalar(out=csb[:, :, :], in0=csb[:, :, :], scalar1=-1.0, scalar2=None, op0=ALU.mult))
                        k.op("pe", [Bt], [Bpst], lambda e: e.transpose(out=pst[:, :], in_=csb[:, :, :].rearrange("a b c -> a (b c)"), identity=ident_f[:, :]))
                        for ctp in range(4):
                            for gi in range(2):
                                k.op("act", [Bpst], [Bt], lambda e, ctp=ctp, gi=gi, yt=yt, r=r: e.copy(
                                    out=Cpad[d][gi * 64:(gi + 1) * 64, 4 * yt + ctp, r, ctp * 32 + gi * 16:ctp * 32 + gi * 16 + 16],
                                    in_=pst[gi * 64:(gi + 1) * 64, (2 * ctp + gi) * 16:(2 * ctp + gi) * 16 + 16]))
            k.barrier()
        k.stack = st
        u4r = Ring(k, "a3_u4", [128, 4, 128], BF16, 3)
        ybr = Ring(k, "a3_yb", [128, 4, 128], F32, 2)
        ps_bu = Ring(k, "a3_psbu", [128, 512], F32, 2, psum=True)
        ps_g = Ring(k, "a3_psg", [128, 2, 128], F32, 2, psum=True)
        ps_y = Ring(k, "a3_psy", [128, 128], F32, 2, psum=True)
        ps_gl = Ring(k, "a3_psgl", [128, 128], F32, 1, psum=True)
        tq = Ring(k, "a3_tq", [128, 4, 256], F32, 2)
        bp = Ring(k, "a3_bp", [128, 2, 16, 128], BF16, 2)
        t4 = Ring(k, "a3_t4", [128, 4, 128], F32, 2)
        hf_ = Ring(k, "a3_hf", [128, 2, 128], F32, 2)
        hb = Ring(k, "a3_hb", [128, 2, 128], BF16, 10)
        ybs = Ring(k, "a3_ybs", [128, 128], F32, 3)
        carry = [k.sb("a3_carry%d" % d, [128, 16, 2], F32) for d in range(2)]
        Bcar = [[Buf("car%d_%d" % (d, ct)) for ct in range(16)] for d in range(2)]
        ysum = k.sb("a3_ysum", [128, 4, 128], F32); Bys = Buf("ysum")
        gtm = k.sb("a3_gtm", [128, 4, 128], F32); Bgt = Buf("gtm")
        ygf = k.sb("a3_ygf", [128, 4, 128], F32); Bygf = Buf("ygf")
        ygb = k.sb("a3_ygb", [128, 4, 128], BF16); Bygb = Buf("ygb")
        gate = Ring(k, "a3_gate", [128, 128], F32, 2)
        sso = Ring(k, "a3_sso", [128, 4, 128], BF16, 2)

        for s in range(NSEQ):
            for d in (1, 0):
                for ct in range(16):
                    k.op("pool", [], [Bcar[d][ct]], lambda e, ct=ct: e.memset(carry[d][:, ct, :], 0.0))
                order = range(NCH - 1, -1, -1) if d == 1 else range(NCH)
                jl = 0 if d == 1 else 127
                for c in order:
                    tb = s * SL + c * 128
                    u4, u4b = u4r.next()
                    k.dma("sp", [], [u4b], lambda e: e.dma_start(out=u4[:, :, :], in_=UT[:, :, tb:tb + 128].rearrange("c p t -> p c t")))
                    if d == 0:
                        ybt, ybb = ybr.next()
                        k.dma("sp", [], [ybb], lambda e: e.dma_start(out=ybt[:, :, :], in_=YB[:, :, tb:tb + 128].rearrange("c p t -> p c t")))
                    bpt, bpb = bp.next()
                    for uc in range(4):
                        for hf in range(2):
                            pbu, pbub = ps_bu.next()
                            k.op("pe", [u4b], [pbub], lambda e, pbu=pbu, uc=uc, hf=hf: e.matmul(
                                pbu[:, :], u4[hf * 64:(hf + 1) * 64, uc, :], Bblk[d][uc][hf * 64:(hf + 1) * 64, :], start=True, stop=True))
                            ct0 = 4 * uc + 2 * hf
                            tqt, tqb = tq.next()
                            wr = Wneg[d][0][:, ct0:ct0 + 2, :].rearrange("p a b -> p (a b)")
                            wi = Wneg[d][1][:, ct0:ct0 + 2, :].rearrange("p a b -> p (a b)")
                            k.op("dve", [pbub], [tqb], lambda e, pbu=pbu, tqt=tqt, wr=wr: e.tensor_tensor(out=tqt[:, 0, :], in0=pbu[:, 0:256], in1=wr, op=ALU.mult))
                            k.op("dve", [pbub], [tqb], lambda e, pbu=pbu, tqt=tqt, wi=wi: e.tensor_tensor(out=tqt[:, 1, :], in0=pbu[:, 256:512], in1=wi, op=ALU.mult))
                            k.op("dve", [pbub], [tqb], lambda e, pbu=pbu, tqt=tqt, wi=wi: e.tensor_tensor(out=tqt[:, 2, :], in0=pbu[:, 0:256], in1=wi, op=ALU.mult))
                            k.op("dve", [pbub], [tqb], lambda e, pbu=pbu, tqt=tqt, wr=wr: e.tensor_tensor(out=tqt[:, 3, :], in0=pbu[:, 256:512], in1=wr, op=ALU.mult))
                            k.op("pool", [tqb], [bpb], lambda e, tqt=tqt, ct0=ct0: e.tensor_tensor(
                                out=bpt[:, 0, ct0:ct0 + 2, :].rearrange("p a b -> p (a b)"), in0=tqt[:, 0, :], in1=tqt[:, 1, :], op=ALU.subtract))
                            k.op("pool", [tqb], [bpb], lambda e, tqt=tqt, ct0=ct0: e.tensor_tensor(
                                out=bpt[:, 1, ct0:ct0 + 2, :].rearrange("p a b -> p (a b)"), in0=tqt[:, 2, :], in1=tqt[:, 3, :], op=ALU.add))
                    hbs = []
                    for ct in range(16):
                        pg, pgb = ps_g.next()

                        def mmG(e, pg=pg, ct=ct):
                            e.matmul(pg[:, 0, :], bpt[:, 0, ct, :], tri[d][:, :], start=True, stop=True)
                            return e.matmul(pg[:, 1, :], bpt[:, 1, ct, :], tri[d][:, :], start=True, stop=True)
                        k.op("pe", [bpb], [pgb], mmG)
                        t4t, t4b = t4.next()
                        cr_ = carry[d][:, ct, 0:1]
                        ci_ = carry[d][:, ct, 1:2]
                        Wr = Ppos[d][0][:, ct, :]
                        Wi = Ppos[d][1][:, ct, :]
                        cb_ = Bcar[d][ct]
                        k.op("dve", [pgb, cb_], [t4b], lambda e, pg=pg, t4t=t4t, cr_=cr_, Wr=Wr: e.scalar_tensor_tensor(out=t4t[:, 0, :], in0=pg[:, 0, :], scalar=cr_, in1=Wr, op0=ALU.add, op1=ALU.mult))
                        k.op("dve", [pgb, cb_], [t4b], lambda e, pg=pg, t4t=t4t, ci_=ci_, Wi=Wi: e.scalar_tensor_tensor(out=t4t[:, 1, :], in0=pg[:, 1, :], scalar=ci_, in1=Wi, op0=ALU.add, op1=ALU.mult))
                        k.op("dve", [pgb, cb_], [t4b], lambda e, pg=pg, t4t=t4t, cr_=cr_, Wi=Wi: e.scalar_tensor_tensor(out=t4t[:, 2, :], in0=pg[:, 0, :], scalar=cr_, in1=Wi, op0=ALU.add, op1=ALU.mult))
                        k.op("dve", [pgb, cb_], [t4b], lambda e, pg=pg, t4t=t4t, ci_=ci_, Wr=Wr: e.scalar_tensor_tensor(out=t4t[:, 3, :], in0=pg[:, 1, :], scalar=ci_, in1=Wr, op0=ALU.add, op1=ALU.mult))
                        hft, hfb = hf_.next()
                        k.op("pool", [t4b], [hfb], lambda e, hft=hft, t4t=t4t: e.tensor_tensor(out=hft[:, 0, :], in0=t4t[:, 0, :], in1=t4t[:, 1, :], op=ALU.subtract))
                        k.op("pool", [t4b], [hfb], lambda e, hft=hft, t4t=t4t: e.tensor_tensor(out=hft[:, 1, :], in0=t4t[:, 2, :], in1=t4t[:, 3, :], op=ALU.add))
                        cmc = cm[:, (NCH if d == 1 else 0) + c:(NCH if d == 1 else 0) + c + 1]
                        k.op("pool", [hfb, Bt], [cb_], lambda e, hft=hft, ct=ct, cmc=cmc: e.tensor_scalar(
                            out=carry[d][:, ct, :], in0=hft[:, :, jl], scalar1=cmc, scalar2=None, op0=ALU.mult))
                        hbt, hbb = hb.next()
                        k.op("act", [hfb], [hbb], lambda e, hbt=hbt, hft=hft: e.copy(out=hbt[:, :, :], in_=hft[:, :, :]))
                        hbs.append((hbt, hbb))
                        if ct % 4 == 3:
                            yt = ct // 4
                            py, pyb = ps_y.next()
                            grp = hbs[-4:]

                            def mmY(e, py=py, grp=grp, yt=yt):
                                n = 0
                                for ctp in range(4):
                                    for r in range(2):
                                        ins = e.matmul(py[:, :], Cpad[d][:, 4 * yt + ctp, r, :], grp[ctp][0][:, r, :], start=(n == 0), stop=(n == 7))
                                        n += 1
                                return ins
                            k.op("pe", [g_[1] for g_ in grp], [pyb], mmY)
                            if d == 1:
                                yo, yob = ybs.next()
                                k.op("act", [pyb], [yob], lambda e, py=py, yo=yo: e.copy(out=yo[:, :], in_=py[:, :]))
                                k.dma("sp", [yob], [], lambda e, yo=yo, yt=yt: e.dma_start(out=YB[yt, :, tb:tb + 128], in_=yo[:, :]))
                            else:
                                k.op("dve", [pyb, ybb], [Bys], lambda e, py=py, yt=yt: e.tensor_tensor(out=ysum[:, yt, :], in0=py[:, :], in1=ybt[:, yt, :], op=ALU.add))
                                k.op("dve", [Bys, u4b, Bt], [Bys], lambda e, yt=yt: e.scalar_tensor_tensor(
                                    out=ysum[:, yt, :], in0=u4[:, yt, :], scalar=dskip[:, yt:yt + 1], in1=ysum[:, yt, :], op0=ALU.mult, op1=ALU.add))
                    if d == 0:
                        k.op("pool", [Bys], [Bgt], lambda e: e.tensor_tensor(out=gtm[:, :, :], in0=ysum[:, :, :], in1=ysum[:, :, :], op=ALU.mult))
                        k.op("pool", [Bgt], [Bgt], lambda e: e.tensor_scalar(out=gtm[:, :, :], in0=gtm[:, :, :], scalar1=0.044715, scalar2=1.0, op0=ALU.mult, op1=ALU.add))
                        k.op("pool", [Bgt, Bys], [Bgt], lambda e: e.tensor_tensor(out=gtm[:, :, :], in0=gtm[:, :, :], in1=ysum[:, :, :], op=ALU.mult))
                        k.op("act", [Bgt], [Bgt], lambda e: e.activation(out=gtm[:, :, :], in_=gtm[:, :, :], func=AF.Sigmoid, scale=1.5957691216057308))
                        k.op("dve", [Bgt, Bys], [Bygf], lambda e: e.tensor_tensor(out=ygf[:, :, :], in0=gtm[:, :, :], in1=ysum[:, :, :], op=ALU.mult))
                        k.op("act", [Bygf], [Bygb], lambda e: e.copy(out=ygb[:, :, :], in_=ygf[:, :, :]))
                        sot, sob = sso.next()
                        for ot in range(4):
                            pgl, pglb = ps_gl.next()

                            def mmGL(e, pgl=pgl, ot=ot):
                                for it in range(4):
                                    ins = e.matmul(pgl[:, :], gluw[:, it, ot * 128:(ot + 1) * 128], ygb[:, it, :], start=(it == 0), stop=(it == 3))
                                return ins
                            k.op("pe", [Bygb, Bt], [pglb], mmGL)
                            gt_, gtb = gate.next()
                            k.op("act", [pglb, Bt], [gtb], lambda e, pgl=pgl, gt_=gt_, ot=ot: e.activation(
                                out=gt_[:, :], in_=pgl[:, :], func=AF.Sigmoid, bias=glub[:, ot:ot + 1]))
                            k.op("dve", [gtb, Bygf], [sob], lambda e, gt_=gt_, ot=ot: e.tensor_tensor(out=sot[:, ot, :], in0=ygf[:, ot, :], in1=gt_[:, :], op=ALU.mult))
                        k.dma("sp", [sob], [], lambda e: e.dma_start(out=SSM[:, :, tb:tb + 128].rearrange("c p t -> p c t"), in_=sot[:, :, :]))
                k.barrier()
    k.stack = prev


class LNR:
    def __init__(self, P, k, L, gam_ap, bet_ap, wr_ap, pfx, nrows=128):
        self.k = k
        self.L = L
        self.nrows = nrows
        self.Bc = Buf(pfx + "const")
        self.gb = k.sb(pfx + "gb", [128, 2, D], F32)
        k.dma("sp", [], [self.Bc], lambda e: e.dma_start(out=self.gb[:, 0, :], in_=gam_ap.partition_broadcast(128)))
        k.dma("sp", [], [self.Bc], lambda e: e.dma_start(out=self.gb[:, 1, :], in_=bet_ap.partition_broadcast(128)))
        self.eps = k.sb(pfx + "eps", [128, 1], F32)
        k.op("dve", [], [self.Bc], lambda e: e.memset(self.eps[:, :], LN_EPS))
        self.z = Ring(k, pfx + "z", [128, D], F32, 2)
        self.st = Ring(k, pfx + "st", [128, 2, 6], F32, 2)
        self.mv = Ring(k, pfx + "mv", [128, 4], F32, 2)
        self.xl = Ring(k, pfx + "xl", [128, D], F32, 2)
        self.router = wr_ap is not None
        if self.router:
            self.wr = k.sb(pfx + "wr", [128, DK, NE], F32)
            k.dma("sp", [], [self.Bc], lambda e: e.dma_start(out=self.wr[:, :, :], in_=wr_ap.rearrange("(dk p) e -> p dk e", p=128)))
            self.xlT = Ring(k, pfx + "xlT", [128, DK, 128], F32, 2)
            self.pst = Ring(k, pfx + "pst", [128, 4, 128], F32, 2, psum=True)
            self.psl = Ring(k, pfx + "psl", [128, NE], F32, 1, psum=True)
            self.sm = Ring(k, pfx + "sm", [128, 4], F32, 2)
            self.ex = Ring(k, pfx + "ex", [128, NE], F32, 2)

    def ln(self, xt, xb_, m_src, m_bufs, m_is_two_bank=True):
        k = self.k
        n = self.nrows
        zt, zb = self.z.next()
        for h in range(2):
            k.op("dve", [xb_] + m_bufs, [zb], lambda e, h=h: e.scalar_tensor_tensor(
                out=zt[0:n, h * 512:(h + 1) * 512], in0=xt[0:n, h * 512:(h + 1) * 512], scalar=ALPHA, in1=m_src(h), op0=ALU.mult, op1=ALU.add))
        stt, stb = self.st.next()
        for h in range(2):
            k.op("dve", [zb], [stb], lambda e, h=h: e.bn_stats(out=stt[0:n, h, :], in_=zt[0:n, h * 512:(h + 1) * 512]))
        mvt, mvb = self.mv.next()
        k.op("dve", [stb], [mvb], lambda e: e.bn_aggr(out=mvt[0:n, 0:2], in_=stt[0:n, :, :].rearrange("p a b -> p (a b)")))
        k.op("act", [mvb, self.Bc], [mvb], lambda e: e.activation(out=mvt[0:n, 2:3], in_=mvt[0:n, 1:2], func=AF.Sqrt, bias=self.eps[0:n, 0:1]))
        k.op("dve", [mvb], [mvb], lambda e: e.reciprocal(out=mvt[0:n, 2:3], in_=mvt[0:n, 2:3]))
        k.op("dve", [mvb], [mvb], lambda e: e.scalar_tensor_tensor(out=mvt[0:n, 3:4], in0=mvt[0:n, 0:1], scalar=-1.0, in1=mvt[0:n, 2:3], op0=ALU.mult, op1=ALU.mult))
        xlt, xlb = self.xl.next()
        k.op("act", [zb, mvb], [xlb], lambda e: e.activation(out=xlt[0:n, :], in_=zt[0:n, :], func=AF.Identity, scale=mvt[0:n, 2:3], bias=mvt[0:n, 3:4]))
        k.op("pool", [xlb, self.Bc], [xlb], lambda e: e.tensor_tensor(out=xlt[0:n, :], in0=xlt[0:n, :], in1=self.gb[0:n, 0, :], op=ALU.mult))
        k.op("pool", [xlb, self.Bc], [xlb], lambda e: e.tensor_tensor(out=xlt[0:n, :], in0=xlt[0:n, :], in1=self.gb[0:n, 1, :], op=ALU.add))
        return xlt, xlb

    def route(self, xlt, xlb, aff_dst, aff_buf):
        k = self.k
        ident_f = self.L["ident_f"]
        xTt, xTb = self.xlT.next()
        for g in range(2):
            pt, ptb = self.pst.next()

            def tr(e, pt=pt, g=g):
                for j in range(4):
                    dk = g * 4 + j
                    ins = e.transpose(out=pt[:, j, :], in_=xlt[:, dk * 128:(dk + 1) * 128], identity=ident_f[:, :])
                return ins
            k.op("pe", [xlb], [ptb], tr)
            k.op("act", [ptb], [xTb], lambda e, pt=pt, g=g: e.copy(out=xTt[:, g * 4:(g + 1) * 4, :], in_=pt[:, :, :]))
        pl, plb = self.psl.next()

        def mm(e):
            for dk in range(DK):
                ins = e.matmul(pl[:, :], xTt[:, dk, :], self.wr[:, dk, :], start=(dk == 0), stop=(dk == DK - 1))
            return ins
        k.op("pe", [xTb, self.Bc], [plb], mm)
        smt, smb = self.sm.next()
        ext, exb = self.ex.next()
        k.op("dve", [plb], [smb], lambda e: e.reduce_max(out=smt[:, 0:1], in_=pl[:, :], axis=AX.X))
        k.op("dve", [smb], [smb], lambda e: e.tensor_scalar(out=smt[:, 1:2], in0=smt[:, 0:1], scalar1=-1.0, scalar2=None, op0=ALU.mult))
        k.op("act", [plb, smb], [exb, smb], lambda e: e.activation(out=ext[:, :], in_=pl[:, :], func=AF.Exp, bias=smt[:, 1:2], accum_out=smt[:, 2:3]))
        k.op("dve", [smb], [smb], lambda e: e.reciprocal(out=smt[:, 3:4], in_=smt[:, 2:3]))
        k.op("dve", [exb, smb], [aff_buf], lambda e: e.tensor_scalar(out=aff_dst, in0=ext[:, :], scalar1=smt[:, 3:4], scalar2=None, op0=ALU.mult))


def phase_A4(P, k, L):
    cfg = P.cfg
    ATT, SSM, XLN, XBF, x_in, root = L["ATT"], L["SSM"], L["XLN"], L["XBF"], L["x_in"], L["root"]
    aff_sb, Baff = L["aff_sb"], L["Baff"]
    prev = k.stack
    with ExitStack() as st:
        k.stack = st
        wo = k.sb("a4_wo", [128, DK, D], BF16); Bwo = Buf("a4_wo")
        k.dma("pool", [], [Bwo], lambda e: e.dma_start(out=wo[:, :, :], in_=L["w_out_even"].ap().rearrange("(dk p) n -> p dk n", p=128)))
        lnr = LNR(P, k, L, L["ln_mix_g"][0:1, :], L["ln_mix_b"][0:1, :], L["w_router"][0], "a4_")
        cat = Ring(k, "a4_cat", [128, 8, 128], BF16, 3)
        xr = Ring(k, "a4_x", [128, D], F32, 3)
        psm = Ring(k, "a4_psm", [128, 2, 512], F32, 2, psum=True)
        for c in range(cfg.NTILE):
            tb = c * 128
            ct, cb = cat.next()
            k.dma("sp", [], [cb], lambda e: e.dma_start(out=ct[:, 0:4, :], in_=ATT[:, :, tb:tb + 128].rearrange("c p t -> p c t")))
            k.dma("sp", [], [cb], lambda e: e.dma_start(out=ct[:, 4:8, :], in_=SSM[:, :, tb:tb + 128].rearrange("c p t -> p c t")))
            xt, xb_ = xr.next()
            k.dma("sp", [], [xb_], lambda e: e.dma_start(out=xt[:, :], in_=x_in[tb:tb + 128, :]))
            pm, pmb = psm.next()

            def mm(e):
                for h in range(2):
                    for kc in range(8):
                        ins = e.matmul(pm[:, h, :], ct[:, kc, :], wo[:, kc, h * 512:(h + 1) * 512], start=(kc == 0), stop=(kc == 7))
                return ins
            k.op("pe", [cb, Bwo], [pmb], mm)
            xlt, xlb = lnr.ln(xt, xb_, lambda h: pm[:, h, :], [pmb])
            k.dma("sp", [xlb], [], lambda e: e.dma_start(out=XLN[tb:tb + 128, :], in_=xlt[:, :]))
            k.dma("pool", [xlb], [], lambda e: e.dma_start(out=XBF[tb:tb + 128, :], in_=xlt[:, :]))
            lnr.route(xlt, xlb, aff_sb[:, c, :], Baff)
        if "AFFD" in P.dbg:
            k.dma("sp", [Baff], [], lambda e: e.dma_start(out=L["AFFD"][:, :], in_=aff_sb[:, :, :].rearrange("p c e -> p (c e)")))
        k.barrier()
    k.stack = prev


def phase_B(P, k, L, layer):
    cfg = P.cfg
    T, IDX, root = L["T"], L["IDX"], L["root"]
    aff_sb, Baff = L["aff_sb"], L["Baff"]
    ones_f = L["ones_f"]
    NTL, CAP = cfg.NTILE, cfg.CAP
    prev = k.stack
    with ExitStack() as st:
        k.stack = st
        Bb = Buf("b_small")
        cmp = k.sb("b_cmp", [128, NTL, NE], F32); Bcmp = Buf("b_cmp")
        cs = k.sb("b_cs", [128, NTL, NE], F32); Bcs = Buf("b_cs")
        sl_i = k.sb("b_sli", [128, NTL, NE], I32); Bsl = Buf("b_sli")
        zer = k.sb("b_zer", [128, NTL], F32)
        tok = k.sb("b_tok", [128, NTL], F32)
        ust = k.sb("b_ust", [128, 128], F32)
        k.op("dve", [], [Bb], lambda e: e.memset(zer[:, :], 0.0))
        k.dma("sp", [], [Bb], lambda e: e.dma_start(out=tok[:, :], in_=T["tok%d" % layer][:, :]))
        k.dma("sp", [], [Bb], lambda e: e.dma_start(out=ust[:, :], in_=T["ustrict"][:, :]))
        sm = k.sb("b_sm", [128, 8, NE], F32)
        pst = k.ps("b_ps", [128, NE], F32); Bps = Buf("b_ps")
        k.op("dve", [], [Bb], lambda e: e.memset(sm[:, 0, :], 0.0))
        k.op("dve", [], [Bb], lambda e: e.memset(sm[:, 1, :], 1.0))

        def compare(thr_row):
            k.op("dve", [Baff, Bb], [Bcmp], lambda e: e.tensor_tensor(
                out=cmp[:, :, :], in0=aff_sb[:, :, :], in1=sm[:, thr_row, :].unsqueeze(1).to_broadcast([128, NTL, NE]), op=ALU.is_gt))
        for it in range(30):
            k.op("dve", [Bb], [Bb], lambda e: e.tensor_tensor(out=sm[:, 2, :], in0=sm[:, 0, :], in1=sm[:, 1, :], op=ALU.add))
            k.op("dve", [Bb], [Bb], lambda e: e.tensor_scalar(out=sm[:, 2, :], in0=sm[:, 2, :], scalar1=0.5, scalar2=None, op0=ALU.mult))
            compare(2)
            k.op("dve", [Bcmp], [Bb], lambda e: e.tensor_reduce(out=sm[:, 3, :], in_=cmp[:, :, :].rearrange("p c e -> p e c"), axis=AX.X, op=ALU.add))
            k.op("pe", [Bb], [Bps], lambda e: e.matmul(pst[:, :], ones_f[:, :], sm[:, 3, :], start=True, stop=True))
            k.op("dve", [Bps], [Bb], lambda e: e.tensor_scalar(out=sm[:, 4, :], in0=pst[:, :], scalar1=float(CAP) - 0.5, scalar2=None, op0=ALU.is_ge))
            k.op("dve", [Bb], [Bb], lambda e: e.tensor_tensor(out=sm[:, 5, :], in0=sm[:, 2, :], in1=sm[:, 0, :], op=ALU.subtract))
            k.op("dve", [Bb], [Bb], lambda e: e.tensor_tensor(out=sm[:, 5, :], in0=sm[:, 5, :], in1=sm[:, 4, :], op=ALU.mult))
            k.op("dve", [Bb], [Bb], lambda e: e.tensor_tensor(out=sm[:, 0, :], in0=sm[:, 0, :], in1=sm[:, 5, :], op=ALU.add))
            k.op("dve", [Bb], [Bb], lambda e: e.tensor_tensor(out=sm[:, 5, :], in0=sm[:, 1, :], in1=sm[:, 2, :], op=ALU.subtract))
            k.op("dve", [Bb], [Bb], lambda e: e.tensor_tensor(out=sm[:, 5, :], in0=sm[:, 5, :], in1=sm[:, 4, :], op=ALU.mult))
            k.op("dve", [Bb], [Bb], lambda e: e.tensor_tensor(out=sm[:, 1, :], in0=sm[:, 2, :], in1=sm[:, 5, :], op=ALU.add))
        compare(0)
        for ex in range(NE):
            k.op("dve", [Bcmp, Bb], [Bcs], lambda e, ex=ex: e.tensor_tensor_scan(
                out=cs[:, :, ex], data0=cmp[:, :, ex], data1=zer[:, :], initial=0.0, op0=ALU.add, op1=ALU.add))
        k.op("pe", [Bcs, Bb], [Bps], lambda e: e.matmul(pst[:, :], ust[:, :], cs[:, NTL - 1, :], start=True, stop=True))
        k.op("dve", [Bps], [Bb], lambda e: e.tensor_scalar(out=sm[:, 6, :], in0=pst[:, :], scalar1=-1.0, scalar2=None, op0=ALU.add))
        k.op("dve", [Bcs, Bb], [Bcs], lambda e: e.tensor_tensor(
            out=cs[:, :, :], in0=cs[:, :, :], in1=sm[:, 6, :].unsqueeze(1).to_broadcast([128, NTL, NE]), op=ALU.add))
        BIG = float(1 << 20)
        k.op("dve", [Bcmp], [Bcmp], lambda e: e.tensor_scalar(out=cmp[:, :, :], in0=cmp[:, :, :], scalar1=-BIG, scalar2=BIG, op0=ALU.mult, op1=ALU.add))
        k.op("dve", [Bcs, Bcmp], [Bcs], lambda e: e.tensor_tensor(out=cs[:, :, :], in0=cs[:, :, :], in1=cmp[:, :, :], op=ALU.add))
        k.op("dve", [Bcs], [Bsl], lambda e: e.tensor_copy(out=sl_i[:, :, :], in_=cs[:, :, :]))
        src = Ring(k, "b_src", [128, NTL, 2], F32, 2)
        for ex in range(NE):
            st_, sb_ = src.next()
            k.op("act", [Bb], [sb_], lambda e: e.copy(out=st_[:, :, 0], in_=tok[:, :]))
            k.op("act", [Baff], [sb_], lambda e, ex=ex: e.copy(out=st_[:, :, 1], in_=aff_sb[:, :, ex]))
            for c in range(NTL):
                k.dma("pool", [Bsl, sb_], [], lambda e, c=c, ex=ex: e.indirect_dma_start(
                    out=IDX[ex][:, :], out_offset=bass.IndirectOffsetOnAxis(ap=sl_i[:, c, ex:ex + 1], axis=0),
                    in_=st_[:, c, :], in_offset=None, bounds_check=L["bnd_reg"], oob_is_err=False))
        k.barrier()
    k.stack = prev


def phase_C(P, k, L, layer):
    cfg = P.cfg
    IDX, XBF, YY, root = L["IDX"], L["XBF"], L["YY"], L["root"]
    ident_b = L["ident_b"]
    w1d, w3d, w2d = L["w_ff1"], L["w_ff3"], L["w_ff2"]
    CAP, TS, NT = cfg.CAP, cfg.TS, cfg.NT
    NSUB = TS // 128
    prev = k.stack
    with ExitStack() as st:
        k.stack = st
        zt = k.sb("c_zt", [128, 4 * D], F32); Bz = Buf("c_zt")
        k.op("dve", [], [Bz], lambda e: e.memset(zt[:, :], 0.0))
        for r0 in range(0, NT, 512):
            k.dma("sp", [Bz], [], lambda e, r0=r0: e.dma_start(out=YY[r0:r0 + 512, :].rearrange("(p a) d -> p (a d)", a=4), in_=zt[:, :]))
        k.barrier()
        w1 = k.sb("c_w1", [128, DK, FH], BF16)
        w3 = k.sb("c_w3", [128, DK, FH], BF16)
        w2 = k.sb("c_w2", [128, FK, D], BF16)
        Bw13 = Buf("c_w13"); Bw2 = Buf("c_w2")
        idf = Ring(k, "c_idf", [128, CAP // 128, 2], F32, 2)
        idi = Ring(k, "c_idi", [128, CAP // 128], I32, 2)
        xg = Ring(k, "c_xg", [128, NSUB, D], BF16, 2)
        xT = Ring(k, "c_xT", [128, DK, TS], BF16, 2)
        gT = Ring(k, "c_gT", [128, FK, TS], BF16, 2)
        sl = Ring(k, "c_sl", [128, TS], F32, 2)
        osb = Ring(k, "c_osb", [128, D], F32, 3)
        pst = Ring(k, "c_pst", [128, TS], BF16, 2, psum=True)
        ph1 = Ring(k, "c_ph1", [128, TS], F32, 2, psum=True)
        ph3 = Ring(k, "c_ph3", [128, TS], F32, 2, psum=True)
        pso = Ring(k, "c_pso", [128, 2, 512], F32, 1, psum=True)
        for ex in range(NE):
            idft, idfb = idf.next()
            ncol = CAP // 128
            nsp = 4 if ncol >= 16 else 1
            cpp = ncol // nsp
            for sp_ in range(nsp):
                k.dma("sp", [], [idfb], lambda e, sp_=sp_: e.dma_start(
                    out=idft[:, sp_ * cpp:(sp_ + 1) * cpp, :],
                    in_=IDX[ex][sp_ * cpp * 128:(sp_ + 1) * cpp * 128, :].rearrange("(c p) t -> p c t", p=128)))
            idit, idib = idi.next()
            k.op("dve", [idfb], [idib], lambda e: e.tensor_copy(out=idit[:, :], in_=idft[:, :, 0]))
            for hfi in range(2):
                f0 = hfi * FH
                k.dma("pool", [], [Bw13], lambda e: e.dma_start(out=w1[:, :, :], in_=w1d[layer, ex, :, f0:f0 + FH].rearrange("(dk p) f -> p dk f", p=128)))
                k.dma("pool", [], [Bw13], lambda e: e.dma_start(out=w3[:, :, :], in_=w3d[layer, ex, :, f0:f0 + FH].rearrange("(dk p) f -> p dk f", p=128)))
                k.dma("pool", [], [Bw2], lambda e: e.dma_start(out=w2[:, :, :], in_=w2d[layer, ex, f0:f0 + FH, :].rearrange("(fk p) d -> p fk d", p=128)))
                def gather(ti_):
                    xgt_, xgb_ = xg.next()
                    for j in range(NSUB):
                        col = ti_ * NSUB + j
                        k.dma("pool", [idib], [xgb_], lambda e, j=j, col=col: e.indirect_dma_start(
                            out=xgt_[:, j, :], out_offset=None, in_=XBF[:, :],
                            in_offset=bass.IndirectOffsetOnAxis(ap=idit[:, col:col + 1], axis=0)))
                    return xgt_, xgb_
                nxt = gather(0)
                for ti in range(CAP // TS):
                    xgt, xgb = nxt
                    if ti + 1 < CAP // TS:
                        nxt = gather(ti + 1)
                    xTt, xTb = xT.next()
                    for dk in range(DK):
                        pt, ptb = pst.next()

                        def tr(e, pt=pt, dk=dk):
                            for j in range(NSUB):
                                ins = e.transpose(out=pt[:, j * 128:(j + 1) * 128], in_=xgt[:, j, dk * 128:(dk + 1) * 128], identity=ident_b[:, :])
                            return ins
                        k.op("pe", [xgb], [ptb], tr)
                        if dk % 2 == 0:
                            k.op("act", [ptb], [xTb], lambda e, pt=pt, dk=dk: e.copy(out=xTt[:, dk, :], in_=pt[:, :]))
                        else:
                            k.op("dve", [ptb], [xTb], lambda e, pt=pt, dk=dk: e.tensor_copy(out=xTt[:, dk, :], in_=pt[:, :]))
                    gTt, gTb = gT.next()
                    for fk in range(FK):
                        p1, p1b = ph1.next()
                        p3, p3b = ph3.next()

                        def mm1(e, p1=p1, fk=fk):
                            for dk in range(DK):
                                ins = e.matmul(p1[:, :], w1[:, dk, fk * 128:(fk + 1) * 128], xTt[:, dk, :], start=(dk == 0), stop=(dk == DK - 1))
                            return ins

                        def mm3(e, p3=p3, fk=fk):
                            for dk in range(DK):
                                ins = e.matmul(p3[:, :], w3[:, dk, fk * 128:(fk + 1) * 128], xTt[:, dk, :], start=(dk == 0), stop=(dk == DK - 1))
                            return ins
                        k.op("pe", [Bw13, xTb], [p1b], mm1)
                        k.op("pe", [Bw13, xTb], [p3b], mm3)
                        slt, slb = sl.next()
                        k.op("act", [p1b], [slb], lambda e, p1=p1, slt=slt: e.activation(out=slt[:, :], in_=p1[:, :], func=AF.Silu))
                        k.op("dve", [slb, p3b], [gTb], lambda e, p3=p3, slt=slt, fk=fk: e.tensor_tensor(out=gTt[:, fk, :], in0=slt[:, :], in1=p3[:, :], op=ALU.mult))
                    for j in range(NSUB):
                        col = ti * NSUB + j
                        po, pob = pso.next()

                        def mmo(e, po=po, j=j):
                            for h in range(2):
                                for fk in range(FK):
                                    ins = e.matmul(po[:, h, :], gTt[:, fk, j * 128:(j + 1) * 128], w2[:, fk, h * 512:(h + 1) * 512], start=(fk == 0), stop=(fk == FK - 1))
                            return ins
                        k.op("pe", [Bw2, gTb], [pob], mmo)
                        ot, ob = osb.next()
                        k.op("act", [pob, idfb], [ob], lambda e, po=po, ot=ot, col=col: e.activation(
                            out=ot[:, :], in_=po[:, :, :].rearrange("p a b -> p (a b)"), func=AF.Copy, scale=idft[:, col, 1:2]))
                        k.dma("pool", [ob, idib], [], lambda e, ot=ot, col=col: e.indirect_dma_start(
                            out=YY[:, :], out_offset=bass.IndirectOffsetOnAxis(ap=idit[:, col:col + 1], axis=0),
                            in_=ot[:, :], in_offset=None, compute_op=ALU.add))
        k.barrier()
    k.stack = prev


def phase_D(P, k, L, layer, dst):
    cfg = P.cfg
    XLN, YY, root = L["XLN"], L["YY"], L["root"]
    prev = k.stack
    with ExitStack() as st:
        k.stack = st
        lnr = LNR(P, k, L, L["ln_ffn_g"][layer:layer + 1, :], L["ln_ffn_b"][layer:layer + 1, :], None, "d_")
        xr = Ring(k, "d_x", [128, D], F32, 3)
        yr = Ring(k, "d_y", [128, D], F32, 3)
        for c in range(cfg.NTILE):
            tb = c * 128
            xt, xb_ = xr.next()
            yt, yb_ = yr.next()
            k.dma("sp", [], [xb_], lambda e: e.dma_start(out=xt[:, :], in_=XLN[tb:tb + 128, :]))
            k.dma("sp", [], [yb_], lambda e: e.dma_start(out=yt[:, :], in_=YY[tb:tb + 128, :]))
            xlt, xlb = lnr.ln(xt, xb_, lambda h: yt[:, h * 512:(h + 1) * 512], [yb_])
            k.dma("sp", [xlb], [], lambda e: e.dma_start(out=dst[tb:tb + 128, :], in_=xlt[:, :]))
        k.barrier()
    k.stack = prev


def phase_B0(P, k, L):
    phase_B(P, k, L, 0)


def phase_C0(P, k, L):
    phase_C(P, k, L, 0)


def phase_D0(P, k, L):
    phase_D(P, k, L, 0, L["X1"])


def phase_F(P, k, L):
    cfg = P.cfg
    X1, XLN, XBF, AFS, T, root = L["X1"], L["XLN"], L["XBF"], L["AFS"], L["T"], L["root"]
    aff_sb, Baff, ident_b = L["aff_sb"], L["Baff"], L["ident_b"]
    SL, NSEQ, NCH, N2E, KPER = cfg.SL, cfg.NSEQ, cfg.NCH, cfg.N2E, cfg.KPER
    prev = k.stack
    with ExitStack() as st:
        k.stack = st
        Bt = Buf("f_setup")
        wcs = [k.sb("f_wc%d" % i, [128, DK, D], BF16) for i in range(2)]
        c1b = k.sb("f_c1", [128, 3, 128], BF16)
        c2b = k.sb("f_c2", [N2E, 2, KPER * 128], BF16)
        tw = k.sb("f_tw", [128, 2, N2E], F32)
        fidx = k.sb("f_idx", [128, NSEQ * N2E], I32)
        k.dma("pool", [], [Bt], lambda e: e.dma_start(out=c1b[:, 0, :], in_=T["c1"][:, :]))
        k.dma("pool", [], [Bt], lambda e: e.dma_start(out=c1b[:, 1, :], in_=T["s1"][:, :]))
        k.op("dve", [Bt], [Bt], lambda e: e.tensor_scalar(out=c1b[:, 2, :], in0=c1b[:, 1, :], scalar1=-1.0, scalar2=None, op0=ALU.mult))
        k.dma("pool", [], [Bt], lambda e: e.dma_start(out=c2b[:, 0, :], in_=T["c2p"][:, :]))
        k.dma("pool", [], [Bt], lambda e: e.dma_start(out=c2b[:, 1, :], in_=T["s2p"][:, :]))
        k.dma("sp", [], [Bt], lambda e: e.dma_start(out=tw[:, 0, :], in_=T["twr"][:, :]))
        k.dma("sp", [], [Bt], lambda e: e.dma_start(out=tw[:, 1, :], in_=T["twi"][:, :]))
        k.dma("sp", [], [Bt], lambda e: e.dma_start(out=fidx[:, :], in_=T["fidx"][:, :]))
        with ExitStack() as st2:
            k.stack = st2
            wob = k.sb("f_wob", [128, DK, D], BF16)
            ccb = k.sb("f_ccb", [128, 2, 512], BF16)
            k.dma("pool", [], [Bt], lambda e: e.dma_start(out=wob[:, :, :], in_=L["w_out_odd"].ap().rearrange("(dk p) n -> p dk n", p=128)))
            k.dma("pool", [], [Bt], lambda e: e.dma_start(out=ccb[:, 0, :], in_=T["cc2"][:, :]))
            k.dma("pool", [], [Bt], lambda e: e.dma_start(out=ccb[:, 1, :], in_=T["sc2"][:, :]))
            psw = Ring(k, "f_psw", [128, 2, 512], F32, 2, psum=True)
            for i in range(2):
                for mc in range(8):
                    g, mm = mc // 2, mc % 2
                    pw, pwb = psw.next()

                    def mmw(e, pw=pw, g=g, mm=mm, i=i):
                        for h in range(2):
                            for kk in range(2):
                                ins = e.matmul(pw[:, h, :], ccb[:, i, (kk * 2 + mm) * 128:(kk * 2 + mm + 1) * 128],
                                               wob[:, 2 * g + kk, h * 512:(h + 1) * 512], start=(kk == 0), stop=(kk == 1))
                        return ins
                    k.op("pe", [Bt], [pwb], mmw)
                    k.op("act", [pwb], [Bt], lambda e, pw=pw, i=i, mc=mc: e.copy(out=wcs[i][:, mc, :], in_=pw[:, :, :].rearrange("p a b -> p (a b)")))
            k.barrier()
        k.stack = st
        for s in range(NSEQ):
            base = s * SL
            with ExitStack() as sa:
                k.stack = sa
                xg = Ring(k, "f_xg", [128, D], F32, 2)
                xb = Ring(k, "f_xb", [128, D], BF16, 2)
                xT = Ring(k, "f_xT", [128, DK, 128], BF16, 2)
                ub = Ring(k, "f_ub", [128, 2, D], BF16, 2)
                tt = Ring(k, "f_tt", [128, 2, 512], F32, 2)
                apb = Ring(k, "f_apb", [128, 2, D], BF16, 2)
                pst = Ring(k, "f_pst", [128, 4, 128], BF16, 2, psum=True)
                psu = Ring(k, "f_psu", [128, 4, 512], F32, 1, psum=True)
                psa = Ring(k, "f_psa", [128, 2, 512], F32, 1, psum=True)
                for j in range(N2E):
                    xgt, xgb = xg.next()
                    col = s * N2E + j
                    k.dma("pool", [Bt], [xgb], lambda e: e.indirect_dma_start(
                        out=xgt[:, :], out_offset=None, in_=X1[:, :], in_offset=bass.IndirectOffsetOnAxis(ap=fidx[:, col:col + 1], axis=0)))
                    xbt, xbb = xb.next()
                    k.op("act", [xgb], [xbb], lambda e: e.copy(out=xbt[:, :], in_=xgt[:, :]))
                    xTt, xTb = xT.next()
                    for g in range(2):
                        pt, ptb = pst.next()

                        def tr(e, pt=pt, g=g):
                            for jj in range(4):
                                dk = g * 4 + jj
                                ins = e.transpose(out=pt[:, jj, :], in_=xbt[:, dk * 128:(dk + 1) * 128], identity=ident_b[:, :])
                            return ins
                        k.op("pe", [xbb], [ptb], tr)
                        k.op("dve", [ptb], [xTb], lambda e, pt=pt, g=g: e.tensor_copy(out=xTt[:, g * 4:(g + 1) * 4, :], in_=pt[:, :, :]))
                    pu, pub = psu.next()

                    def mmu(e):
                        for i in range(2):
                            for h in range(2):
                                for dk in range(DK):
                                    ins = e.matmul(pu[:, i * 2 + h, :], xTt[:, dk, :], wcs[i][:, dk, h * 512:(h + 1) * 512], start=(dk == 0), stop=(dk == DK - 1))
                        return ins
                    k.op("pe", [xTb, Bt], [pub], mmu)
                    ubt, ubb = ub.next()
                    k.op("act", [pub], [ubb], lambda e: e.copy(out=ubt[:, 0, :], in_=pu[:, 0:2, :].rearrange("p a b -> p (a b)")))
                    k.op("dve", [pub], [ubb], lambda e: e.tensor_copy(out=ubt[:, 1, :], in_=pu[:, 2:4, :].rearrange("p a b -> p (a b)")))
                    apt, apbb = apb.next()
                    for h in range(2):
                        pa, pab = psa.next()
                        hs = slice(h * 512, (h + 1) * 512)

                        def mma(e, pa=pa, hs=hs):
                            e.matmul(pa[:, 0, :], c1b[:, 0, :], ubt[:, 0, hs], start=True, stop=False)
                            e.matmul(pa[:, 0, :], c1b[:, 1, :], ubt[:, 1, hs], start=False, stop=True)
                            e.matmul(pa[:, 1, :], c1b[:, 0, :], ubt[:, 1, hs], start=True, stop=False)
                            return e.matmul(pa[:, 1, :], c1b[:, 2, :], ubt[:, 0, hs], start=False, stop=True)
                        k.op("pe", [ubb, Bt], [pab], mma)
                        ttt, ttb = tt.next()
                        k.op("act", [pab, Bt], [ttb], lambda e, pa=pa, ttt=ttt: e.activation(out=ttt[:, 0, :], in_=pa[:, 1, :], func=AF.Copy, scale=tw[:, 1, j:j + 1]))
                        k.op("act", [pab, Bt], [ttb], lambda e, pa=pa, ttt=ttt: e.activation(out=ttt[:, 1, :], in_=pa[:, 1, :], func=AF.Copy, scale=tw[:, 0, j:j + 1]))
                        k.op("dve", [pab, ttb, Bt], [apbb], lambda e, pa=pa, ttt=ttt, hs=hs: e.scalar_tensor_tensor(
                            out=apt[:, 0, hs], in0=pa[:, 0, :], scalar=tw[:, 0, j:j + 1], in1=ttt[:, 0, :], op0=ALU.mult, op1=ALU.subtract))
                        k.op("dve", [pab, ttb, Bt], [apbb], lambda e, pa=pa, ttt=ttt, hs=hs: e.scalar_tensor_tensor(
                            out=apt[:, 1, hs], in0=pa[:, 0, :], scalar=tw[:, 1, j:j + 1], in1=ttt[:, 1, :], op0=ALU.mult, op1=ALU.add))
                    k.dma("sp", [apbb], [], lambda e: e.dma_start(out=AFS[:, j, :, :], in_=apt[:, :, :]))
                k.barrier()
            with ExitStack() as sc:
                k.stack = sc
                lnr = LNR(P, k, L, L["ln_mix_g"][1:2, :], L["ln_mix_b"][1:2, :], L["w_router"][1], "fl_")
                a2 = Ring(k, "f_a2", [N2E, KPER, 2, D], BF16, 2 if KPER <= 4 else 1)
                xr = Ring(k, "f_xr", [128, D], F32, 2)
                psm = Ring(k, "f_psm", [128, 2, 512], F32, 2, psum=True)
                x1v = X1[base:base + SL, :].rearrange("(j k) d -> k j d", k=128)
                xlnv = XLN[base:base + SL, :].rearrange("(j k) d -> k j d", k=128)
                xbfv = XBF[base:base + SL, :].rearrange("(j k) d -> k j d", k=128)
                for q in range(NCH):
                    a2t, a2b = a2.next()
                    k.dma("sp", [], [a2b], lambda e: e.dma_start(out=a2t[:, :, :, :], in_=AFS[q * KPER:(q + 1) * KPER, :, :, :].rearrange("k j r c -> j k r c")))
                    xt, xb_ = xr.next()
                    for v in range(KPER):
                        k.dma("sp", [], [xb_], lambda e, v=v: e.dma_start(out=xt[v * N2E:(v + 1) * N2E, :], in_=x1v[q * KPER + v]))
                    pm, pmb = psm.next()

                    def mmm(e):
                        for h in range(2):
                            n = 0
                            for v in range(KPER):
                                for r in range(2):
                                    ins = e.matmul(pm[:, h, :], c2b[:, r, v * 128:(v + 1) * 128], a2t[:, v, r, h * 512:(h + 1) * 512],
                                                   start=(n == 0), stop=(n == 2 * KPER - 1))
                                    n += 1
                        return ins
                    k.op("pe", [a2b, Bt], [pmb], mmm)
                    xlt, xlb = lnr.ln(xt, xb_, lambda h: pm[:, h, :], [pmb])
                    for v in range(KPER):
                        k.dma("sp", [xlb], [], lambda e, v=v: e.dma_start(out=xlnv[q * KPER + v], in_=xlt[v * N2E:(v + 1) * N2E, :]))
                        k.dma("pool", [xlb], [], lambda e, v=v: e.dma_start(out=xbfv[q * KPER + v], in_=xlt[v * N2E:(v + 1) * N2E, :]))
                    lnr.route(xlt, xlb, aff_sb[:, s * NCH + q, :], Baff)
                k.barrier()
            k.stack = st
        if "AFFD" in P.dbg:
            k.dma("sp", [Baff], [], lambda e: e.dma_start(out=L["AFFD"][:, :], in_=aff_sb[:, :, :].rearrange("p c e -> p (c e)")))
        k.barrier()
    k.stack = prev


def phase_B1(P, k, L):
    phase_B(P, k, L, 1)


def phase_C1(P, k, L):
    phase_C(P, k, L, 1)


def phase_D1(P, k, L):
    phase_D(P, k, L, 1, L["y_out"])


_PROG = {}


def kernel(**inputs):
    xp = np.asarray(inputs["x_prompt"], dtype=np.float32)
    xs = np.asarray(inputs["x_sample"], dtype=np.float32)
    inp = {n: np.asarray(v) for n, v in inputs.items() if n not in ("x_prompt", "x_sample")}
    Bp, Sp, _ = xp.shape
    Bs, Ss, _ = xs.shape
    SL = max(Sp, Ss)
    assert Bp * Sp == Bs * Ss and SL % Sp == 0 and SL % Ss == 0
    nseq = Bp * Sp // SL
    cfg_p = Cfg(nseq, SL, Sp)
    cfg_s = Cfg(nseq, SL, Ss)
    key = (nseq, SL)
    if key not in _PROG:
        _PROG[key] = build(cfg_p)
    P = _PROG[key]
    maps = []
    for cfg, x in ((cfg_p, xp), (cfg_s, xs)):
        P.tabs = const_tables(cfg)
        maps.append(core_inputs(cfg, P, x.reshape(-1, D), inp))
    res = run_bass_kernel_spmd(P.nc, maps, core_ids=[0, 1])
    y_p = np.asarray(res.results[0]["y"], dtype=np.float32).reshape(Bp, Sp, D)
    y_s = np.asarray(res.results[1]["y"], dtype=np.float32).reshape(Bs, Ss, D)
    return (y_p, y_s)
```

```python
import math
import numpy as np
import ml_dtypes
import concourse.bass as bass
import concourse.mybir as mybir
from concourse.bass_utils import run_bass_kernel_spmd

F32 = mybir.dt.float32
BF16 = mybir.dt.bfloat16
I32 = mybir.dt.int32
ALU = mybir.AluOpType
AF = mybir.ActivationFunctionType
AX = mybir.AxisListType

D = 1024
DK = 8
NE = 16
DFF = 2816
FH = 1408
FK = 11
LN_EPS = 1e-5
ALPHA = 4.0 ** 0.25
NEG = -30000.0


class Buf:
    __slots__ = ("name", "w", "r")

    def __init__(self, name):
        self.name = name
        self.w = None
        self.r = []


class Eng:
    def __init__(self, k, name, e, sem):
        self.k = k
        self.name = name
        self.e = e
        self.sem = sem
        self.count = 0
        self.seen = {}


class K:
    def __init__(self, nc, stack):
        self.nc = nc
        self.stack = stack
        self.root = stack
        self.engs = {}
        for name, e in (("pe", nc.tensor), ("act", nc.scalar), ("dve", nc.vector),
                        ("pool", nc.gpsimd), ("sp", nc.sync)):
            sem = stack.enter_context(nc.semaphore("s_" + name))
            self.engs[name] = Eng(self, name, e, sem)
        self.ndma = 24
        self.dsems = [stack.enter_context(nc.semaphore("d%d" % i)) for i in range(self.ndma)]
        self.dcount = [0] * self.ndma
        self.dnext = 0
        self.pending = []
        self.same_engine_sync = False

    def _wait(self, eng, ev, raw=True):
        if ev is None:
            return
        sem, val, src = ev
        if src == eng.name and (src == "pe" or (not raw and src in ("act", "dve"))):
            return
        key = id(sem)
        if eng.seen.get(key, 0) >= val:
            return
        eng.e.wait_ge(sem, val)
        eng.seen[key] = val

    def deps(self, eng, reads, writes):
        for b in reads:
            self._wait(eng, b.w, raw=True)
        for b in writes:
            self._wait(eng, b.w, raw=False)
            for ev in b.r:
                self._wait(eng, ev, raw=False)

    def _record(self, ev, reads, writes):
        for b in reads:
            b.r.append(ev)
            if len(b.r) > 12:
                b.r = b.r[-12:]
        for b in writes:
            b.w = ev
            b.r = []

    def op(self, en, reads, writes, fn):
        eng = self.engs[en]
        self.deps(eng, reads, writes)
        ins = fn(eng.e)
        eng.count += 1
        ins.then_inc(eng.sem, 1)
        self._record((eng.sem, eng.count, en), reads, writes)
        return ins

    def dma(self, qn, reads, writes, fn):
        eng = self.engs[qn]
        self.deps(eng, reads, writes)
        i = self.dnext
        self.dnext = (self.dnext + 1) % self.ndma
        sem = self.dsems[i]
        if self.dcount[i] > 0:
            self._wait(eng, (sem, self.dcount[i], "dma"))
        ins = fn(eng.e)
        self.dcount[i] += 16
        ins.then_inc(sem, 16)
        ev = (sem, self.dcount[i], "dma")
        self._record(ev, reads, writes)
        self.pending.append(ev)
        if len(self.pending) > 4 * self.ndma:
            self.pending = self.pending[-self.ndma:]
        return ins

    def barrier(self):
        evs = [(e.sem, e.count, e.name) for e in self.engs.values() if e.count > 0]
        evs += [(self.dsems[i], self.dcount[i], "dma") for i in range(self.ndma) if self.dcount[i] > 0]
        for eng in self.engs.values():
            for ev in evs:
                if ev[2] == eng.name:
                    continue
                self._wait(eng, ev)
        self.pending = []

    _uid = 0

    def rotate(self):
        self.barrier()
        for eng in self.engs.values():
            if eng.count > 0:
                eng.sem = self.root.enter_context(self.nc.semaphore("s_%s_%d" % (eng.name, K._uid)))
                K._uid += 1
                eng.count = 0

    def sb(self, name, shape, dt):
        K._uid += 1
        return self.stack.enter_context(self.nc.sbuf_tensor("%s_%d" % (name, K._uid), shape, dt))

    def ps(self, name, shape, dt=F32):
        K._uid += 1
        return self.stack.enter_context(self.nc.psum_tensor("%s_%d" % (name, K._uid), shape, dt))


class Cfg:
    def __init__(self, nseq, sl, rs):
        self.NSEQ = nseq
        self.SL = sl
        self.RS = rs
        self.NT = nseq * sl
        self.NTILE = self.NT // 128
        self.CAP = max(1, 2 * self.NT // NE)
        self.NCH = sl // 128
        self.QT = min(512, sl)
        self.NQT = sl // self.QT
        self.TS = min(512, self.CAP)
        self.N2E = sl // 128
        self.N2 = rs // 128


def const_tables(cfg):
    SL, RS, NSEQ, NT = cfg.SL, cfg.RS, cfg.NSEQ, cfg.NT
    t = {}
    inv = (1.0 / (np.float32(10000.0) ** (np.arange(0, 64, 2, dtype=np.float32) / np.float32(64)))).astype(np.float32)
    pos = (np.arange(SL) % RS).astype(np.float32)
    ang = (pos[:, None] * inv[None, :]).astype(np.float32)
    ang = np.concatenate([ang, ang], -1)
    cosT = np.cos(ang).astype(np.float32).T
    sinT = np.sin(ang).astype(np.float32).T
    t["ropec"] = np.ascontiguousarray(np.concatenate([cosT, cosT], 0))
    t["ropes"] = np.ascontiguousarray(np.concatenate([sinT, sinT], 0))
    nkc, nqt = cfg.NCH, cfg.NQT
    mb = np.zeros((nkc, nqt), np.float32)
    for kc in range(nkc):
        for qt in range(nqt):
            if (kc * 128) // RS != (qt * cfg.QT) // RS:
                mb[kc, qt] = NEG
    t["maskb"] = np.ascontiguousarray(np.broadcast_to(mb.reshape(1, -1), (128, nkc * nqt))).astype(np.float32)
    cf = np.ones(nkc, np.float32)
    cb = np.ones(nkc, np.float32)
    for c in range(nkc):
        if ((c + 1) * 128) % RS == 0:
            cf[c] = 0.0
        if (c * 128) % RS == 0:
            cb[c] = 0.0
    t["cmask"] = np.ascontiguousarray(np.broadcast_to(np.concatenate([cf, cb]).reshape(1, -1), (128, 2 * nkc))).astype(np.float32)
    i = np.arange(128)
    t["tri"] = (i[:, None] <= i[None, :]).astype(np.float32)
    t["trib"] = (i[:, None] >= i[None, :]).astype(np.float32)
    t["ustrict"] = (i[:, None] < i[None, :]).astype(np.float32)
    t["ident"] = np.eye(128, dtype=np.float32)
    gm = np.zeros((128, 4), np.float32)
    for r in range(128):
        gm[r, (r % 64) // 16] = 1.0
    t["gmask"] = gm
    tok0 = (np.arange(cfg.NTILE)[None, :] * 128 + np.arange(128)[:, None]).astype(np.float32)
    t["tok0"] = tok0
    kper = 128 // cfg.N2E if cfg.N2E <= 128 else 1
    tok1 = np.zeros((128, cfg.NTILE), np.float32)
    ntile_seq = cfg.NCH
    for s in range(NSEQ):
        for q in range(ntile_seq):
            for p in range(128):
                k1 = q * kper + p // cfg.N2E
                jj = p % cfg.N2E
                tok1[p, s * ntile_seq + q] = s * SL + k1 + 128 * jj
    t["tok1"] = tok1
    cfg.KPER = kper
    N2, N2E = cfg.N2, cfg.N2E
    fidx = np.zeros((128, NSEQ * N2E), np.int32)
    twr = np.zeros((128, N2E), np.float32)
    twi = np.zeros((128, N2E), np.float32)
    for j in range(N2E):
        sh, t2 = j // N2, j % N2
        for s in range(NSEQ):
            fidx[:, s * N2E + j] = s * SL + sh * RS + N2 * np.arange(128) + t2
        a = 2.0 * np.pi * t2 * np.arange(128) / RS
        twr[:, j] = np.cos(a)
        twi[:, j] = -np.sin(a)
    t["fidx"] = fidx
    t["twr"] = twr
    t["twi"] = twi
    a1 = 2.0 * np.pi * np.outer(np.arange(128), np.arange(128)) / 128.0
    t["c1"] = (np.cos(a1) / np.sqrt(128.0)).astype(np.float32)
    t["s1"] = (np.sin(a1) / np.sqrt(128.0)).astype(np.float32)
    c2 = np.zeros((N2E, N2E), np.float64)
    s2 = np.zeros((N2E, N2E), np.float64)
    for j in range(N2E):
        for jp in range(N2E):
            if j // N2 == jp // N2:
                a = 2.0 * np.pi * (j % N2) * (jp % N2) / N2
                c2[j, jp] = np.cos(a) / np.sqrt(N2)
                s2[j, jp] = np.sin(a) / np.sqrt(N2)
    c2p = np.zeros((N2E, kper, 128), np.float32)
    s2p = np.zeros((N2E, kper, 128), np.float32)
    for v in range(kper):
        c2p[:, v, v * N2E:(v + 1) * N2E] = c2
        s2p[:, v, v * N2E:(v + 1) * N2E] = s2
    t["c2p"] = c2p.reshape(N2E, kper * 128)
    t["s2p"] = s2p.reshape(N2E, kper * 128)
    ac = 2.0 * np.pi * np.outer(np.arange(256), np.arange(256)) / 256.0
    cc = (np.cos(ac) / 16.0).astype(np.float32)
    sc = (-np.sin(ac) / 16.0).astype(np.float32)
    t["cc2"] = np.ascontiguousarray(cc.reshape(2, 128, 2, 128).transpose(1, 0, 2, 3)).reshape(128, 512)
    t["sc2"] = np.ascontiguousarray(sc.reshape(2, 128, 2, 128).transpose(1, 0, 2, 3)).reshape(128, 512)
    return t


from contextlib import ExitStack


PHASE_GROUPS = (("A1",), ("A2",), ("A3",), ("A4", "B0"), ("C0",), ("D0",), ("F", "B1"), ("C1",), ("D1",))


class Prog:
    def __init__(self, cfg, dbg=()):
        self.cfg = cfg
        self.dbg = set(dbg)
        self.nc = bass.Bass("TRN2", target_bir_lowering=False)
        self.in_names = []
        self.out_names = []

    def din(self, name, shape, dt=F32):
        self.in_names.append(name)
        return self.nc.dram_tensor(name, list(shape), dt, kind="ExternalInput")

    def dscr(self, name, shape, dt):
        if name in self.dbg:
            self.out_names.append(name)
            return self.nc.dram_tensor(name, list(shape), dt, kind="ExternalOutput")
        return self.nc.dram_tensor(name, list(shape), dt)

    def dout(self, name, shape, dt=F32):
        self.out_names.append(name)
        return self.nc.dram_tensor(name, list(shape), dt, kind="ExternalOutput")


def bcast_rows(ap2d, nparts):
    return ap2d.partition_broadcast(nparts)


def build(cfg, dbg=(), stop_after=None, skip=()):
    P = Prog(cfg, dbg)
    P.skip = set(skip)
    nc = P.nc
    NT, SL, NSEQ, NCH = cfg.NT, cfg.SL, cfg.NSEQ, cfg.NCH
    x_in = P.din("x", [NT, D])
    w_in = P.din("w_in", [D, 2048])
    lamv = P.din("lamv", [4, 64])
    subln_g = P.din("subln_g", [128, 1])
    s5_a_re = P.din("s5_a_re", [2, 32, 64])
    s5_a_im = P.din("s5_a_im", [2, 32, 64])
    s5_log_dt = P.din("s5_log_dt", [2, 32, 1])
    s5_b_re = P.din("s5_b_re", [2, 32, 64, 16])
    s5_b_im = P.din("s5_b_im", [2, 32, 64, 16])
    s5_c_re = P.din("s5_c_re", [2, 512, 64])
    s5_c_im = P.din("s5_c_im", [2, 512, 64])
    s5_d = P.din("s5_d", [128, 4])
    s5_glu_w = P.din("s5_glu_w", [512, 512])
    s5_glu_b = P.din("s5_glu_b", [128, 4])
    w_out_even = P.din("w_out_even", [D, D])
    w_out_odd = P.din("w_out_odd", [D, D])
    ln_mix_g = P.din("ln_mix_g", [2, D])
    ln_mix_b = P.din("ln_mix_b", [2, D])
    w_router = P.din("w_router", [2, D, NE])
    if stop_after in ("A1", "A2", "A3", "A4", "B0"):
        w_ff1 = w_ff3 = w_ff2 = None
    else:
        w_ff1 = P.din("w_ff1", [2, NE, D, DFF])
        w_ff3 = P.din("w_ff3", [2, NE, D, DFF])
        w_ff2 = P.din("w_ff2", [2, NE, DFF, D])
    ln_ffn_g = P.din("ln_ffn_g", [2, D])
    ln_ffn_b = P.din("ln_ffn_b", [2, D])
    T = {}
    tabs = const_tables(cfg)
    for name, arr in tabs.items():
        T[name] = P.din("t_" + name, arr.shape, I32 if arr.dtype == np.int32 else F32)
    P.tabs = tabs
    y_out = P.dout("y", [NT, D])
    QKT = P.dscr("QKT", [8, 128, NT], BF16)
    VV = P.dscr("VV", [NT, 512], BF16)
    UT = P.dscr("UT", [4, 128, NT], BF16)
    ATT = P.dscr("ATT", [4, 128, NT], BF16)
    SSM = P.dscr("SSM", [4, 128, NT], BF16)
    YB = P.dscr("YB", [4, 128, NT], F32)
    XLN = P.dscr("XLN", [NT, D], F32)
    XBF = P.dscr("XBF", [NT + 128, D], BF16)
    YY = P.dscr("YY", [NT, D], F32)
    X1 = P.dscr("X1", [NT, D], F32)
    IDX = [P.dscr("IDX%d" % i, [cfg.CAP + 128, 2], F32) for i in range(NE)]
    AFS = P.dscr("AFS", [128, cfg.N2E, 2, D], BF16)
    AFFD = P.dscr("AFFD", [128, cfg.NTILE * NE], F32)

    with ExitStack() as root:
        k = K(nc, root)
        e_ = k.engs

        ident_f = k.sb("ident_f", [128, 128], F32)
        ident_b = k.sb("ident_b", [128, 128], BF16)
        ones_b = k.sb("ones_b", [128, 128], BF16)
        ones_f = k.sb("ones_f", [128, 128], F32)
        B_const = Buf("const")
        k.dma("sp", [], [B_const], lambda e: e.dma_start(out=ident_f[:, :], in_=T["ident"][:, :]))
        k.op("dve", [B_const], [B_const], lambda e: e.tensor_copy(out=ident_b[:, :], in_=ident_f[:, :]))
        k.op("dve", [], [B_const], lambda e: e.memset(ones_b[:, :], 1.0))
        k.op("dve", [], [B_const], lambda e: e.memset(ones_f[:, :], 1.0))

        bnd_reg = nc.gpsimd.alloc_register("bnd")
        nc.gpsimd.reg_mov(bnd_reg, cfg.CAP - 1)
        L = dict(locals())
        for group in PHASE_GROUPS:
            with ExitStack() as gs:
                k.stack = gs
                if group[0] in ("A4", "F"):
                    L["aff_sb"] = k.sb("aff_sb", [128, cfg.NTILE, NE], F32)
                    L["Baff"] = Buf("aff")
                for name in group:
                    fn = globals().get("phase_" + name)
                    if name in P.skip or fn is None:
                        continue
                    fn(P, k, L)
                    k.rotate()
                    if stop_after == name:
                        k.barrier()
                        k.stack = root
                        return P
            k.stack = root
        k.barrier()
    return P


class Ring:
    def __init__(self, k, name, shape, dt, n, psum=False):
        self.t = []
        self.b = []
        for i in range(n):
            nm = "%s%d" % (name, i)
            self.t.append(k.ps(nm, shape, dt) if psum else k.sb(nm, shape, dt))
            self.b.append(Buf(nm))
        self.i = 0
        self.n = n

    def next(self):
        i = self.i
        self.i = (self.i + 1) % self.n
        return self.t[i], self.b[i]


def phase_A1(P, k, L):
    cfg = P.cfg
    x_in, w_in, QKT, VV, UT, T = L["x_in"], L["w_in"], L["QKT"], L["VV"], L["UT"], L["T"]
    ident_b, root = L["ident_b"], L["root"]
    NT, SL = cfg.NT, cfg.SL
    prev = k.stack
    with ExitStack() as st:
        k.stack = st
        wall = k.sb("wall", [128, DK, 3072], BF16)
        Bw = Buf("wall")
        w_v = w_in.ap().rearrange("(dk p) f -> p dk f", p=128)
        k.dma("pool", [], [Bw], lambda e: e.dma_start(out=wall[:, :, 0:1024], in_=w_v[:, :, 0:1024]))
        k.dma("pool", [], [Bw], lambda e: e.dma_start(out=wall[:, :, 2048:3072], in_=w_v[:, :, 1024:2048]))
        for dk in range(DK):
            dst = wall[:, dk, 1024:2048].rearrange("p (b h i) -> p b h i", h=2, i=32)
            src = w_v[:, dk, 0:1024].rearrange("p (b h i) -> p b h i", h=2, i=32)
            k.dma("pool", [], [Bw], lambda e, d=dst, s=src: e.dma_start(out=d[:, :, 0, :], in_=s[:, :, 1, :]))
            k.dma("pool", [], [Bw], lambda e, d=dst, s=src: e.dma_start(out=d[:, :, 1, :], in_=s[:, :, 0, :]))
        for dk in range(DK):
            v = wall[:, dk, 1024:2048].rearrange("p (b h i) -> p b h i", h=2, i=32)[:, :, 0, :]
            k.op("dve", [Bw], [Bw], lambda e, v=v: e.tensor_scalar(out=v, in0=v, scalar1=-1.0, scalar2=None, op0=ALU.mult))

        xb = Ring(k, "a1_xb", [128, 4, D], BF16, 2)
        xT = Ring(k, "a1_xT", [128, DK, 512], BF16, 2)
        cs = Ring(k, "a1_cs", [128, 2, 512], F32, 2)
        t12 = Ring(k, "a1_t12", [128, 2, 512], F32, 2)
        qko = Ring(k, "a1_qko", [128, 512], BF16, 3)
        vo = Ring(k, "a1_vo", [128, 4, 512], BF16, 2)
        uo = Ring(k, "a1_uo", [128, 512], BF16, 3)
        pst = Ring(k, "a1_pst", [128, 512], BF16, 2, psum=True)
        psA = Ring(k, "a1_psA", [128, 512], F32, 2, psum=True)
        psB = Ring(k, "a1_psB", [128, 512], F32, 2, psum=True)
        psC = Ring(k, "a1_psC", [128, 512], F32, 2, psum=True)

        ntile = NT // 512
        for tt in range(ntile):
            t0 = tt * 512
            p0 = t0 % SL
            xbt, xbb = xb.next()
            k.dma("pool", [], [xbb], lambda e: e.dma_start(
                out=xbt[:, :, :], in_=x_in[t0:t0 + 512, :].rearrange("(j p) d -> p j d", p=128)))
            cst, csb = cs.next()
            k.dma("sp", [], [csb], lambda e: e.dma_start(out=cst[:, 0, :], in_=T["ropec"][:, p0:p0 + 512]))
            k.dma("sp", [], [csb], lambda e: e.dma_start(out=cst[:, 1, :], in_=T["ropes"][:, p0:p0 + 512]))
            xTt, xTb = xT.next()
            for dk in range(DK):
                pt, ptb = pst.next()

                def tr(e, pt=pt, dk=dk):
                    for j in range(4):
                        ins = e.transpose(out=pt[:, j * 128:(j + 1) * 128], in_=xbt[:, j, dk * 128:(dk + 1) * 128],
                                          identity=ident_b[:, :])
                    return ins
                k.op("pe", [xbb], [ptb], tr)
                if dk % 2 == 0:
                    k.op("act", [ptb], [xTb], lambda e, pt=pt, dk=dk: e.copy(out=xTt[:, dk, :], in_=pt[:, :]))
                else:
                    k.op("dve", [ptb], [xTb], lambda e, pt=pt, dk=dk: e.tensor_copy(out=xTt[:, dk, :], in_=pt[:, :]))
            for fc in range(8):
                pa, pab = psA.next()
                pb, pbb = psB.next()

                def mmA(e, pa=pa, fc=fc):
                    for dk in range(DK):
                        ins = e.matmul(pa[:, :], wall[:, dk, fc * 128:(fc + 1) * 128], xTt[:, dk, :],
                                       start=(dk == 0), stop=(dk == DK - 1))
                    return ins

                def mmB(e, pb=pb, fc=fc):
                    for dk in range(DK):
                        ins = e.matmul(pb[:, :], wall[:, dk, 1024 + fc * 128:1024 + (fc + 1) * 128], xTt[:, dk, :],
                                       start=(dk == 0), stop=(dk == DK - 1))
                    return ins
                k.op("pe", [Bw, xTb], [pab], mmA)
                k.op("pe", [Bw, xTb], [pbb], mmB)
                tt_, ttb = t12.next()
                k.op("dve", [pab, csb], [ttb], lambda e, pa=pa, tt_=tt_: e.tensor_tensor(
                    out=tt_[:, 0, :], in0=pa[:, :], in1=cst[:, 0, :], op=ALU.mult))
                k.op("dve", [pbb, csb], [ttb], lambda e, pb=pb, tt_=tt_: e.tensor_tensor(
                    out=tt_[:, 1, :], in0=pb[:, :], in1=cst[:, 1, :], op=ALU.mult))
                qo, qob = qko.next()
                k.op("pool", [ttb], [qob], lambda e, qo=qo, tt_=tt_: e.tensor_tensor(
                    out=qo[:, :], in0=tt_[:, 0, :], in1=tt_[:, 1, :], op=ALU.add))
                k.dma("sp", [qob], [], lambda e, qo=qo, fc=fc: e.dma_start(out=QKT[fc, :, t0:t0 + 512], in_=qo[:, :]))
            vt, vb = vo.next()
            for j in range(4):
                pc, pcb = psC.next()

                def mmV(e, pc=pc, j=j):
                    for dk in range(DK):
                        ins = e.matmul(pc[:, :], xTt[:, dk, j * 128:(j + 1) * 128], wall[:, dk, 2048:2560],
                                       start=(dk == 0), stop=(dk == DK - 1))
                    return ins
                k.op("pe", [Bw, xTb], [pcb], mmV)
                k.op("act", [pcb], [vb], lambda e, pc=pc, j=j: e.copy(out=vt[:, j, :], in_=pc[:, :]))
            k.dma("sp", [vb], [], lambda e: e.dma_start(
                out=VV[t0:t0 + 512, :].rearrange("(j p) f -> p j f", p=128), in_=vt[:, :, :]))
            for c in range(4):
                pc, pcb = psC.next()

                def mmU(e, pc=pc, c=c):
                    for dk in range(DK):
                        ins = e.matmul(pc[:, :], wall[:, dk, 2560 + c * 128:2560 + (c + 1) * 128], xTt[:, dk, :],
                                       start=(dk == 0), stop=(dk == DK - 1))
                    return ins
                k.op("pe", [Bw, xTb], [pcb], mmU)
                ut, ub = uo.next()
                k.op("act", [pcb], [ub], lambda e, pc=pc, ut=ut: e.copy(out=ut[:, :], in_=pc[:, :]))
                k.dma("sp", [ub], [], lambda e, ut=ut, c=c: e.dma_start(out=UT[c, :, t0:t0 + 512], in_=ut[:, :]))
        k.barrier()
    k.stack = prev


def core_inputs(cfg, P, x_flat, inp):
    m = {}
    m["x"] = np.ascontiguousarray(x_flat, dtype=np.float32)
    m["w_in"] = inp["w_in"][0]
    m["lamv"] = np.stack([inp["lam_q1"][0], inp["lam_k1"][0], inp["lam_q2"][0], inp["lam_k2"][0]], 0)
    m["subln_g"] = inp["subln_g"][0].reshape(128, 1)
    m["s5_a_re"] = inp["s5_a_re"][0]
    m["s5_a_im"] = inp["s5_a_im"][0]
    m["s5_log_dt"] = inp["s5_log_dt"][0].reshape(2, 32, 1)
    m["s5_b_re"] = inp["s5_b_re"][0]
    m["s5_b_im"] = inp["s5_b_im"][0]
    m["s5_c_re"] = inp["s5_c_re"][0].reshape(2, 512, 64)
    m["s5_c_im"] = inp["s5_c_im"][0].reshape(2, 512, 64)
    m["s5_d"] = inp["s5_d"][0].reshape(4, 128).T
    m["s5_glu_w"] = inp["s5_glu_w"][0]
    m["s5_glu_b"] = inp["s5_glu_b"][0].reshape(4, 128).T
    m["w_out_even"] = inp["w_out_even"][0]
    m["w_out_odd"] = inp["w_out_odd"][0]
    for n in ("ln_mix_g", "ln_mix_b", "w_router", "w_ff1", "w_ff3", "w_ff2", "ln_ffn_g", "ln_ffn_b"):
        m[n] = inp[n]
    for n, arr in P.tabs.items():
        m["t_" + n] = arr
    out = {}
    for n in P.in_names:
        out[n] = np.ascontiguousarray(m[n])
    return out


def phase_A2(P, k, L):
    cfg = P.cfg
    QKT, VV, ATT, T = L["QKT"], L["VV"], L["ATT"], L["T"]
    lamv, subln_g = L["lamv"], L["subln_g"]
    ones_b, root = L["ones_b"], L["root"]
    SL, NSEQ, NCH, QT, NQT = cfg.SL, cfg.NSEQ, cfg.NCH, cfg.QT, cfg.NQT
    lambda_init = 0.8 - 0.6 * math.exp(0.0)
    prev = k.stack
    with ExitStack() as st:
        k.stack = st
        lv = k.sb("a2_lv", [128, 4, 64], F32)
        Bs = Buf("a2_scal")
        k.dma("sp", [], [Bs], lambda e: e.dma_start(
            out=lv[:, :, :].rearrange("p a b -> p (a b)"),
            in_=lamv.ap().rearrange("a b -> (a b)").partition_broadcast(128)))
        pr = k.sb("a2_pr", [128, 2, 64], F32)
        sm = k.sb("a2_sm", [128, 4], F32)
        k.op("dve", [Bs], [Bs], lambda e: e.tensor_tensor(out=pr[:, 0, :], in0=lv[:, 0, :], in1=lv[:, 1, :], op=ALU.mult))
        k.op("dve", [Bs], [Bs], lambda e: e.tensor_tensor(out=pr[:, 1, :], in0=lv[:, 2, :], in1=lv[:, 3, :], op=ALU.mult))
        k.op("dve", [Bs], [Bs], lambda e: e.reduce_sum(out=sm[:, 0:1], in_=pr[:, 0, :], axis=AX.X))
        k.op("dve", [Bs], [Bs], lambda e: e.reduce_sum(out=sm[:, 1:2], in_=pr[:, 1, :], axis=AX.X))
        k.op("act", [Bs], [Bs], lambda e: e.activation(out=sm[:, 2:4], in_=sm[:, 0:2], func=AF.Exp))
        nlam = k.sb("a2_nlam", [128, 1], F32)
        k.op("dve", [Bs], [Bs], lambda e: e.tensor_tensor(out=nlam[:, :], in0=sm[:, 3:4], in1=sm[:, 2:3], op=ALU.subtract))
        k.op("dve", [Bs], [Bs], lambda e: e.tensor_scalar(out=nlam[:, :], in0=nlam[:, :], scalar1=-lambda_init, scalar2=None, op0=ALU.add))
        gsc = k.sb("a2_gsc", [128, 1], F32)
        k.dma("sp", [], [Bs], lambda e: e.dma_start(out=gsc[:, :], in_=subln_g[:, :]))
        k.op("dve", [Bs], [Bs], lambda e: e.tensor_scalar(out=gsc[:, :], in0=gsc[:, :], scalar1=1.0 - lambda_init, scalar2=None, op0=ALU.mult))
        mb = k.sb("a2_mb", [128, NCH * NQT], F32)
        k.dma("sp", [], [Bs], lambda e: e.dma_start(out=mb[:, :], in_=T["maskb"][:, :]))
        epsb = k.sb("a2_eps", [128, 1], F32)
        k.op("dve", [], [Bs], lambda e: e.memset(epsb[:, :], LN_EPS))

        qT = Ring(k, "a2_qT", [128, SL], BF16, 2)
        kT = Ring(k, "a2_kT", [128, SL], BF16, 2)
        vh = Ring(k, "a2_vh", [128, NCH, 128], BF16, 2)
        ps_s = Ring(k, "a2_pss", [128, 2, 512], F32, 2, psum=True)
        ps_o1 = k.ps("a2_o1", [128, 512]); Bo1 = Buf("o1")
        ps_o2 = k.ps("a2_o2", [128, 512]); Bo2 = Buf("o2")
        ps_z1 = k.ps("a2_z1", [128, 512]); Bz1 = Buf("z1")
        ps_z2 = k.ps("a2_z2", [128, 512]); Bz2 = Buf("z2")
        et = Ring(k, "a2_e", [128, 2, 512], BF16, 3)
        r12 = k.sb("a2_r12", [128, 2, 512], F32); Br = Buf("r12")
        ab = k.sb("a2_ab", [128, 2, 512], F32); Bab = Buf("ab")
        osb = k.sb("a2_o", [128, 512], F32); Bosb = Buf("osb")
        sq = k.sb("a2_sq", [128, 512], BF16); Bsq = Buf("sq")
        rstd = k.sb("a2_rstd", [128, 512], F32); Brs = Buf("rstd")
        ao = Ring(k, "a2_ao", [128, 512], BF16, 2)

        for s in range(NSEQ):
            for h in range(4):
                base = s * SL
                qt_, qb = qT.next()
                kt_, kb = kT.next()
                vt_, vb = vh.next()
                k.dma("sp", [], [qb], lambda e: e.dma_start(out=qt_[:, :], in_=QKT[h, :, base:base + SL]))
                k.dma("sp", [], [kb], lambda e: e.dma_start(out=kt_[:, :], in_=QKT[4 + h, :, base:base + SL]))
                nsp = 4 if NCH >= 16 else 1
                cpp = NCH // nsp
                for sp_ in range(nsp):
                    k.dma("sp", [], [vb], lambda e, sp_=sp_: e.dma_start(
                        out=vt_[:, sp_ * cpp:(sp_ + 1) * cpp, :],
                        in_=VV[base + sp_ * cpp * 128:base + (sp_ + 1) * cpp * 128, h * 128:(h + 1) * 128].rearrange("(c p) f -> p c f", p=128)))
                for qt in range(NQT):
                    q0 = qt * QT
                    pend = None
                    for kc in range(NCH + 1):
                        if kc < NCH:
                            pss, psb = ps_s.next()

                            def mmS(e, pss=pss, kc=kc):
                                e.matmul(pss[:, 0, 0:QT], kt_[0:64, kc * 128:(kc + 1) * 128], qt_[0:64, q0:q0 + QT],
                                         start=True, stop=True)
                                return e.matmul(pss[:, 1, 0:QT], kt_[64:128, kc * 128:(kc + 1) * 128],
                                                qt_[64:128, q0:q0 + QT], start=True, stop=True)
                            k.op("pe", [qb, kb], [psb], mmS)
                            ee, eb = et.next()
                            mcol = mb[:, kc * NQT + qt:kc * NQT + qt + 1]
                            k.op("act", [psb, Bs], [eb], lambda e, pss=pss, ee=ee, mcol=mcol: e.activation(
                                out=ee[:, :, 0:QT], in_=pss[:, :, 0:QT], func=AF.Exp, bias=mcol, scale=0.125))
                            cur = (ee, eb, kc)
                        else:
                            cur = None
                        if pend is not None:
                            pe_, peb, pkc = pend

                            def mmPV(e, pe_=pe_, pkc=pkc):
                                st_, sp_ = (pkc == 0), (pkc == NCH - 1)
                                e.matmul(ps_o1[:, 0:QT], vt_[:, pkc, :], pe_[:, 0, 0:QT], start=st_, stop=sp_)
                                e.matmul(ps_z1[:, 0:QT], ones_b[:, :], pe_[:, 0, 0:QT], start=st_, stop=sp_)
                                e.matmul(ps_o2[:, 0:QT], vt_[:, pkc, :], pe_[:, 1, 0:QT], start=st_, stop=sp_)
                                return e.matmul(ps_z2[:, 0:QT], ones_b[:, :], pe_[:, 1, 0:QT], start=st_, stop=sp_)
                            k.op("pe", [vb, peb], [Bo1, Bo2, Bz1, Bz2], mmPV)
                        pend = cur
                    k.op("dve", [Bz1], [Br], lambda e: e.reciprocal(out=r12[:, 0, 0:QT], in_=ps_z1[:, 0:QT]))
                    k.op("dve", [Bz2], [Br], lambda e: e.reciprocal(out=r12[:, 1, 0:QT], in_=ps_z2[:, 0:QT]))
                    k.op("dve", [Bo1, Br], [Bab], lambda e: e.tensor_tensor(out=ab[:, 0, 0:QT], in0=ps_o1[:, 0:QT], in1=r12[:, 0, 0:QT], op=ALU.mult))
                    k.op("dve", [Bo2, Br], [Bab], lambda e: e.tensor_tensor(out=ab[:, 1, 0:QT], in0=ps_o2[:, 0:QT], in1=r12[:, 1, 0:QT], op=ALU.mult))
                    k.op("dve", [Bab, Bs], [Bosb], lambda e: e.scalar_tensor_tensor(
                        out=osb[:, 0:QT], in0=ab[:, 1, 0:QT], scalar=nlam[:, 0:1], in1=ab[:, 0, 0:QT], op0=ALU.mult, op1=ALU.add))
                    k.op("pool", [Bosb], [Bsq], lambda e: e.tensor_tensor(out=sq[:, 0:QT], in0=osb[:, 0:QT], in1=osb[:, 0:QT], op=ALU.mult))
                    k.op("pe", [Bsq], [Bz1], lambda e: e.matmul(ps_z1[:, 0:QT], ones_b[:, :], sq[:, 0:QT], start=True, stop=True))
                    k.op("act", [Bz1, Bs], [Brs], lambda e: e.activation(out=rstd[:, 0:QT], in_=ps_z1[:, 0:QT], func=AF.Sqrt,
                                                                         bias=epsb[:, 0:1], scale=1.0 / 128.0))
                    k.op("dve", [Brs], [Brs], lambda e: e.reciprocal(out=rstd[:, 0:QT], in_=rstd[:, 0:QT]))
                    aot, aob = ao.next()
                    k.op("dve", [Bosb, Brs, Bs], [aob], lambda e: e.scalar_tensor_tensor(
                        out=aot[:, 0:QT], in0=osb[:, 0:QT], scalar=gsc[:, 0:1], in1=rstd[:, 0:QT], op0=ALU.mult, op1=ALU.mult))
                    import os
                    dm = os.environ.get("A2DBG", "")
                    if dm == "a":
                        k.op("dve", [Bab], [aob], lambda e: e.tensor_copy(out=aot[:, 0:QT], in_=ab[:, 0, 0:QT]))
                    elif dm == "b":
                        k.op("dve", [Bab], [aob], lambda e: e.tensor_copy(out=aot[:, 0:QT], in_=ab[:, 1, 0:QT]))
                    elif dm == "o":
                        k.op("dve", [Bosb], [aob], lambda e: e.tensor_copy(out=aot[:, 0:QT], in_=osb[:, 0:QT]))
                    elif dm == "n":
                        k.op("dve", [Bosb, Bs], [aob], lambda e: e.tensor_scalar(out=aot[:, 0:QT], in0=osb[:, 0:QT], scalar1=0.0, scalar2=nlam[:, 0:1], op0=ALU.mult, op1=ALU.add))
                    elif dm == "s":
                        k.op("dve", [Bosb, Bs], [aob], lambda e: e.tensor_scalar(out=aot[:, 0:QT], in0=osb[:, 0:QT], scalar1=0.0, scalar2=sm[:, int(os.environ.get("SMI", "0")):int(os.environ.get("SMI", "0")) + 1], op0=ALU.mult, op1=ALU.add))
                    elif dm == "r":
                        k.op("dve", [Brs], [aob], lambda e: e.tensor_copy(out=aot[:, 0:QT], in_=rstd[:, 0:QT]))
                    k.dma("sp", [aob], [], lambda e: e.dma_start(out=ATT[h, :, base + q0:base + q0 + QT], in_=aot[:, 0:QT]))
        k.barrier()
    k.stack = prev


def _cmul_bcast(k, eng, Bt, outr, outi, ar, ai, sr, si, tmp, shape):
    t1, t2, t3, t4 = tmp
    k.op(eng, [Bt], [Bt], lambda e: e.tensor_tensor(out=t1, in0=ar, in1=sr, op=ALU.mult))
    k.op(eng, [Bt], [Bt], lambda e: e.tensor_tensor(out=t2, in0=ai, in1=si, op=ALU.mult))
    k.op(eng, [Bt], [Bt], lambda e: e.tensor_tensor(out=t3, in0=ar, in1=si, op=ALU.mult))
    k.op(eng, [Bt], [Bt], lambda e: e.tensor_tensor(out=t4, in0=ai, in1=sr, op=ALU.mult))
    k.op(eng, [Bt], [Bt], lambda e: e.tensor_tensor(out=outr, in0=t1, in1=t2, op=ALU.subtract))
    k.op(eng, [Bt], [Bt], lambda e: e.tensor_tensor(out=outi, in0=t3, in1=t4, op=ALU.add))


def phase_A3(P, k, L):
    cfg = P.cfg
    UT, SSM, YB, T = L["UT"], L["SSM"], L["YB"], L["T"]
    ident_f, ones_b, root = L["ident_f"], L["ones_b"], L["root"]
    SL, NSEQ, NCH = cfg.SL, cfg.NSEQ, cfg.NCH
    prev = k.stack
    with ExitStack() as st:
        k.stack = st
        Bt = Buf("a3_setup")
        pst = k.ps("a3_pst", [128, 128], F32); Bpst = Buf("a3_pst")
        Ppos = [[k.sb("a3_pp%d%d" % (d, r), [128, 16, 128], F32) for r in range(2)] for d in range(2)]
        Wneg = [[k.sb("a3_wn%d%d" % (d, r), [128, 16, 128], F32) for r in range(2)] for d in range(2)]
        Bblk = [[k.sb("a3_bb%d%d" % (d, c), [128, 512], BF16) for c in range(4)] for d in range(2)]
        Cpad = [k.sb("a3_cp%d" % d, [128, 16, 2, 128], BF16) for d in range(2)]
        dskip = k.sb("a3_dsk", [128, 4], F32)
        glub = k.sb("a3_glub", [128, 4], F32)
        gluw = k.sb("a3_gluw", [128, 4, 512], BF16)
        tri = [k.sb("a3_tri%d" % d, [128, 128], BF16) for d in range(2)]
        cm = k.sb("a3_cm", [128, 2 * NCH], F32)
        k.dma("sp", [], [Bt], lambda e: e.dma_start(out=dskip[:, :], in_=L["s5_d"][:, :]))
        k.dma("sp", [], [Bt], lambda e: e.dma_start(out=glub[:, :], in_=L["s5_glu_b"][:, :]))
        k.dma("pool", [], [Bt], lambda e: e.dma_start(out=gluw[:, :, :], in_=L["s5_glu_w"].ap().rearrange("(c p) o -> p c o", p=128)))
        k.dma("pool", [], [Bt], lambda e: e.dma_start(out=tri[0][:, :], in_=T["tri"][:, :]))
        k.dma("pool", [], [Bt], lambda e: e.dma_start(out=tri[1][:, :], in_=T["trib"][:, :]))
        k.dma("sp", [], [Bt], lambda e: e.dma_start(out=cm[:, :], in_=T["cmask"][:, :]))
        with ExitStack() as st2:
            k.stack = st2
            tmpA = k.sb("a3_tmpA", [128, 4, 16, 128], F32)
            Pneg = [[k.sb("a3_pn%d%d" % (d, r), [128, 16, 128], F32) for r in range(2)] for d in range(2)]
            for d in range(2):
                are = k.sb("a3_are%d" % d, [16, 128], F32)
                aim = k.sb("a3_aim%d" % d, [16, 128], F32)
                ldt = k.sb("a3_ldt%d" % d, [16, 2], F32)
                k.dma("sp", [], [Bt], lambda e: e.dma_start(out=are[:, :], in_=L["s5_a_re"][d].rearrange("(c g) p -> c (g p)", g=2)))
                k.dma("sp", [], [Bt], lambda e: e.dma_start(out=aim[:, :], in_=L["s5_a_im"][d].rearrange("(c g) p -> c (g p)", g=2)))
                k.dma("sp", [], [Bt], lambda e: e.dma_start(out=ldt[:, :], in_=L["s5_log_dt"][d].rearrange("(c g) o -> c (g o)", g=2)))
                dt = k.sb("a3_dt%d" % d, [16, 2], F32)
                k.op("act", [Bt], [Bt], lambda e: e.activation(out=dt[:, :], in_=ldt[:, :], func=AF.Exp))
                wk = k.sb("a3_wk%d" % d, [16, 12, 128], F32)
                dtb = dt[:, :].unsqueeze(2).to_broadcast([16, 2, 64])

                def v3(i):
                    return wk[:, i, :].rearrange("c (g p) -> c g p", g=2)
                k.op("dve", [Bt], [Bt], lambda e: e.tensor_tensor(out=v3(0), in0=are[:, :].rearrange("c (g p) -> c g p", g=2), in1=dtb, op=ALU.mult))
                k.op("dve", [Bt], [Bt], lambda e: e.tensor_tensor(out=v3(1), in0=aim[:, :].rearrange("c (g p) -> c g p", g=2), in1=dtb, op=ALU.mult))
                k.op("dve", [Bt], [Bt], lambda e: e.tensor_scalar(out=wk[:, 1, :], in0=wk[:, 1, :], scalar1=1.0 / 16.0, scalar2=None, op0=ALU.mult))
                hp = k.sb("a3_hp%d" % d, [16, 1], F32)
                k.op("dve", [], [Bt], lambda e: e.memset(hp[:, :], math.pi / 2.0))
                k.op("act", [Bt], [Bt], lambda e: e.activation(out=wk[:, 2, :], in_=wk[:, 0, :], func=AF.Exp))
                k.op("act", [Bt], [Bt], lambda e: e.activation(out=wk[:, 3, :], in_=wk[:, 1, :], func=AF.Sin))
                k.op("act", [Bt], [Bt], lambda e: e.activation(out=wk[:, 4, :], in_=wk[:, 1, :], func=AF.Sin, bias=hp[:, 0:1]))
                for _ in range(4):
                    k.op("dve", [Bt], [Bt], lambda e: e.tensor_tensor(out=wk[:, 5, :], in0=wk[:, 4, :], in1=wk[:, 4, :], op=ALU.mult))
                    k.op("dve", [Bt], [Bt], lambda e: e.tensor_tensor(out=wk[:, 6, :], in0=wk[:, 3, :], in1=wk[:, 3, :], op=ALU.mult))
                    k.op("dve", [Bt], [Bt], lambda e: e.tensor_tensor(out=wk[:, 7, :], in0=wk[:, 3, :], in1=wk[:, 4, :], op=ALU.mult))
                    k.op("dve", [Bt], [Bt], lambda e: e.tensor_tensor(out=wk[:, 4, :], in0=wk[:, 5, :], in1=wk[:, 6, :], op=ALU.subtract))
                    k.op("dve", [Bt], [Bt], lambda e: e.tensor_scalar(out=wk[:, 3, :], in0=wk[:, 7, :], scalar1=2.0, scalar2=None, op0=ALU.mult))
                k.op("dve", [Bt], [Bt], lambda e: e.tensor_tensor(out=wk[:, 5, :], in0=wk[:, 2, :], in1=wk[:, 4, :], op=ALU.mult))
                k.op("dve", [Bt], [Bt], lambda e: e.tensor_tensor(out=wk[:, 6, :], in0=wk[:, 2, :], in1=wk[:, 3, :], op=ALU.mult))
                k.op("dve", [Bt], [Bt], lambda e: e.tensor_scalar(out=wk[:, 7, :], in0=wk[:, 5, :], scalar1=-1.0, scalar2=None, op0=ALU.add))
                k.op("dve", [Bt], [Bt], lambda e: e.tensor_tensor(out=wk[:, 8, :], in0=are[:, :], in1=are[:, :], op=ALU.mult))
                k.op("dve", [Bt], [Bt], lambda e: e.tensor_tensor(out=wk[:, 9, :], in0=aim[:, :], in1=aim[:, :], op=ALU.mult))
                k.op("dve", [Bt], [Bt], lambda e: e.tensor_tensor(out=wk[:, 8, :], in0=wk[:, 8, :], in1=wk[:, 9, :], op=ALU.add))
                k.op("dve", [Bt], [Bt], lambda e: e.reciprocal(out=wk[:, 8, :], in_=wk[:, 8, :]))
                k.op("dve", [Bt], [Bt], lambda e: e.tensor_tensor(out=wk[:, 9, :], in0=wk[:, 7, :], in1=are[:, :], op=ALU.mult))
                k.op("dve", [Bt], [Bt], lambda e: e.tensor_tensor(out=wk[:, 10, :], in0=wk[:, 6, :], in1=aim[:, :], op=ALU.mult))
                k.op("dve", [Bt], [Bt], lambda e: e.tensor_tensor(out=wk[:, 9, :], in0=wk[:, 9, :], in1=wk[:, 10, :], op=ALU.add))
                k.op("dve", [Bt], [Bt], lambda e: e.tensor_tensor(out=wk[:, 9, :], in0=wk[:, 9, :], in1=wk[:, 8, :], op=ALU.mult))
                k.op("dve", [Bt], [Bt], lambda e: e.tensor_tensor(out=wk[:, 10, :], in0=wk[:, 6, :], in1=are[:, :], op=ALU.mult))
                k.op("dve", [Bt], [Bt], lambda e: e.tensor_tensor(out=wk[:, 11, :], in0=wk[:, 7, :], in1=aim[:, :], op=ALU.mult))
                k.op("dve", [Bt], [Bt], lambda e: e.tensor_tensor(out=wk[:, 10, :], in0=wk[:, 10, :], in1=wk[:, 11, :], op=ALU.subtract))
                k.op("dve", [Bt], [Bt], lambda e: e.tensor_tensor(out=wk[:, 10, :], in0=wk[:, 10, :], in1=wk[:, 8, :], op=ALU.mult))
                k.op("dve", [Bt], [Bt], lambda e: e.tensor_tensor(out=wk[:, 0, :], in0=wk[:, 2, :], in1=wk[:, 2, :], op=ALU.mult))
                k.op("dve", [Bt], [Bt], lambda e: e.reciprocal(out=wk[:, 0, :], in_=wk[:, 0, :]))
                k.op("dve", [Bt], [Bt], lambda e: e.tensor_tensor(out=wk[:, 7, :], in0=wk[:, 5, :], in1=wk[:, 0, :], op=ALU.mult))
                k.op("dve", [Bt], [Bt], lambda e: e.tensor_tensor(out=wk[:, 11, :], in0=wk[:, 6, :], in1=wk[:, 0, :], op=ALU.mult))
                k.op("dve", [Bt], [Bt], lambda e: e.tensor_scalar(out=wk[:, 11, :], in0=wk[:, 11, :], scalar1=-1.0, scalar2=None, op0=ALU.mult))
                lamT = k.sb("a3_lamT%d" % d, [128, 6, 16], F32)
                for oi, wi in enumerate((5, 6, 7, 11, 9, 10)):
                    k.op("pe", [Bt], [Bpst], lambda e, wi=wi: e.transpose(out=pst[:, 0:16], in_=wk[:, wi, :], identity=ident_f[0:16, 0:16]))
                    k.op("act", [Bpst], [Bt], lambda e, oi=oi: e.copy(out=lamT[:, oi, :], in_=pst[:, 0:16]))
                for tabs_, ri0 in ((Ppos[d], 0), (Pneg[d], 2)):
                    pr_, pi_ = tabs_
                    first = 0 if d == 0 else 127
                    k.op("dve", [Bt], [Bt], lambda e: e.tensor_copy(out=pr_[:, :, first], in_=lamT[:, ri0, :]))
                    k.op("dve", [Bt], [Bt], lambda e: e.tensor_copy(out=pi_[:, :, first], in_=lamT[:, ri0 + 1, :]))
                    for m in range(7):
                        n = 1 << m
                        if d == 0:
                            src = slice(0, n); dst = slice(n, 2 * n); sc = n - 1
                        else:
                            src = slice(128 - n, 128); dst = slice(128 - 2 * n, 128 - n); sc = 128 - n
                        sr = pr_[:, :, sc:sc + 1].to_broadcast([128, 16, n])
                        si = pi_[:, :, sc:sc + 1].to_broadcast([128, 16, n])
                        tmp = [tmpA[:, i, :, 0:n] for i in range(4)]
                        _cmul_bcast(k, "dve", Bt, pr_[:, :, dst], pi_[:, :, dst], pr_[:, :, src], pi_[:, :, src], sr, si, tmp, None)
                for r in range(2):
                    for ct in range(16):
                        k.op("pe", [Bt], [Bpst], lambda e, r=r, ct=ct: e.transpose(out=pst[:, :], in_=Pneg[d][r][:, ct, :], identity=ident_f[:, :]))
                        k.op("act", [Bpst], [Bt], lambda e, r=r, ct=ct: e.copy(out=Wneg[d][r][:, ct, :], in_=pst[:, :]))
                bre = k.sb("a3_bre%d" % d, [128, 16, 16], F32)
                bim = k.sb("a3_bim%d" % d, [128, 16, 16], F32)
                for gi in range(2):
                    k.dma("sp", [], [Bt], lambda e, gi=gi: e.dma_start(
                        out=bre[gi * 64:(gi + 1) * 64, :, :], in_=L["s5_b_re"][d].rearrange("(c g) p h -> g p c h", g=2)[gi]))
                    k.dma("sp", [], [Bt], lambda e, gi=gi: e.dma_start(
                        out=bim[gi * 64:(gi + 1) * 64, :, :], in_=L["s5_b_im"][d].rearrange("(c g) p h -> g p c h", g=2)[gi]))
                bbr = k.sb("a3_bbr%d" % d, [128, 16, 16], F32)
                bbi = k.sb("a3_bbi%d" % d, [128, 16, 16], F32)
                crb = lamT[:, 4, :].unsqueeze(2).to_broadcast([128, 16, 16])
                cib = lamT[:, 5, :].unsqueeze(2).to_broadcast([128, 16, 16])
                tmp = [tmpA[:, i, :, 0:16] for i in range(4)]
                _cmul_bcast(k, "dve", Bt, bbr[:, :, :], bbi[:, :, :], bre[:, :, :], bim[:, :, :], crb, cib, tmp, None)
                in2 = k.sb("a3_in2%d" % d, [128, 128], F32)
                for c in range(4):
                    k.op("dve", [Bt], [Bt], lambda e, c=c: e.memset(Bblk[d][c][:, :], 0.0))
                    for r, bb in enumerate((bbr, bbi)):
                        for ctl in range(2):
                            k.op("dve", [Bt, Bpst], [Bt], lambda e: e.memset(in2[:, :], 0.0))
                            for gi in range(2):
                                for hf in range(2):
                                    gl = 2 * ctl + gi
                                    ct = 4 * c + 2 * hf + ctl
                                    col = hf * 64 + gl * 16
                                    k.op("dve", [Bt], [Bt], lambda e, gi=gi, ct=ct, col=col, bb=bb: e.tensor_copy(
                                        out=in2[gi * 64:(gi + 1) * 64, col:col + 16], in_=bb[gi * 64:(gi + 1) * 64, ct, :]))
                            k.op("pe", [Bt], [Bpst], lambda e: e.transpose(out=pst[:, :], in_=in2[:, :], identity=ident_f[:, :]))
                            k.op("act", [Bpst], [Bt], lambda e, c=c, r=r, ctl=ctl: e.copy(
                                out=Bblk[d][c][:, (r * 2 + ctl) * 128:(r * 2 + ctl + 1) * 128], in_=pst[:, :]))
                k.op("dve", [Bt], [Bt], lambda e: e.memset(Cpad[d][:, :, :, :], 0.0))
                csb = k.sb("a3_csb%d" % d, [128, 2, 64], F32)
                for r, cten in enumerate((L["s5_c_re"], L["s5_c_im"])):
                    for yt in range(4):
                        for dup in range(2):
                            k.dma("sp", [Bpst], [Bt], lambda e, dup=dup, yt=yt, cten=cten: e.dma_start(
                                out=csb[:, dup, :], in_=cten[d, yt * 128:(yt + 1) * 128, :]))
                        if r == 1:
                            k.op("dve", [Bt], [Bt], lambda e: e.tensor_scalar(out=csb[:, :, :], in0=csb[:, :, :], scalar1=-1.0, scalar2=None, op0=ALU.mult))
                        k.op("pe", [Bt], [Bpst], lambda e: e.transpose(out=pst[:, :], in_=csb[:, :, :].rearrange("a b c -> a (b c)"), identity=ident_f[:, :]))
                        for ctp in range(4):
                            for gi in range(2):
                                k.op("act", [Bpst], [Bt], lambda e, ctp=ctp, gi=gi, yt=yt, r=r: e.copy(
                                    out=Cpad[d][gi * 64:(gi + 1) * 64, 4 * yt + ctp, r, ctp * 32 + gi * 16:ctp * 32 + gi * 16 + 16],
                                    in_=pst[gi * 64:(gi + 1) * 64, (2 * ctp + gi) * 16:(2 * ctp + gi) * 16 + 16]))
            k.barrier()
        k.stack = st
        u4r = Ring(k, "a3_u4", [128, 4, 128], BF16, 3)
        ybr = Ring(k, "a3_yb", [128, 4, 128], F32, 2)
        ps_bu = Ring(k, "a3_psbu", [128, 512], F32, 2, psum=True)
        ps_g = Ring(k, "a3_psg", [128, 2, 128], F32, 2, psum=True)
        ps_y = Ring(k, "a3_psy", [128, 128], F32, 2, psum=True)
        ps_gl = Ring(k, "a3_psgl", [128, 128], F32, 1, psum=True)
        tq = Ring(k, "a3_tq", [128, 4, 256], F32, 2)
        bp = Ring(k, "a3_bp", [128, 2, 16, 128], BF16, 2)
        t4 = Ring(k, "a3_t4", [128, 4, 128], F32, 2)
        hf_ = Ring(k, "a3_hf", [128, 2, 128], F32, 2)
        hb = Ring(k, "a3_hb", [128, 2, 128], BF16, 10)
        ybs = Ring(k, "a3_ybs", [128, 128], F32, 3)
        carry = [k.sb("a3_carry%d" % d, [128, 16, 2], F32) for d in range(2)]
        Bcar = [[Buf("car%d_%d" % (d, ct)) for ct in range(16)] for d in range(2)]
        ysum = k.sb("a3_ysum", [128, 4, 128], F32); Bys = Buf("ysum")
        gtm = k.sb("a3_gtm", [128, 4, 128], F32); Bgt = Buf("gtm")
        ygf = k.sb("a3_ygf", [128, 4, 128], F32); Bygf = Buf("ygf")
        ygb = k.sb("a3_ygb", [128, 4, 128], BF16); Bygb = Buf("ygb")
        gate = Ring(k, "a3_gate", [128, 128], F32, 2)
        sso = Ring(k, "a3_sso", [128, 4, 128], BF16, 2)

        for s in range(NSEQ):
            for d in (1, 0):
                for ct in range(16):
                    k.op("pool", [], [Bcar[d][ct]], lambda e, ct=ct: e.memset(carry[d][:, ct, :], 0.0))
                order = range(NCH - 1, -1, -1) if d == 1 else range(NCH)
                jl = 0 if d == 1 else 127
                for c in order:
                    tb = s * SL + c * 128
                    u4, u4b = u4r.next()
                    k.dma("sp", [], [u4b], lambda e: e.dma_start(out=u4[:, :, :], in_=UT[:, :, tb:tb + 128].rearrange("c p t -> p c t")))
                    if d == 0:
                        ybt, ybb = ybr.next()
                        k.dma("sp", [], [ybb], lambda e: e.dma_start(out=ybt[:, :, :], in_=YB[:, :, tb:tb + 128].rearrange("c p t -> p c t")))
                    bpt, bpb = bp.next()
                    for uc in range(4):
                        for hf in range(2):
                            pbu, pbub = ps_bu.next()
                            k.op("pe", [u4b], [pbub], lambda e, pbu=pbu, uc=uc, hf=hf: e.matmul(
                                pbu[:, :], u4[hf * 64:(hf + 1) * 64, uc, :], Bblk[d][uc][hf * 64:(hf + 1) * 64, :], start=True, stop=True))
                            ct0 = 4 * uc + 2 * hf
                            tqt, tqb = tq.next()
                            wr = Wneg[d][0][:, ct0:ct0 + 2, :].rearrange("p a b -> p (a b)")
                            wi = Wneg[d][1][:, ct0:ct0 + 2, :].rearrange("p a b -> p (a b)")
                            k.op("dve", [pbub], [tqb], lambda e, pbu=pbu, tqt=tqt, wr=wr: e.tensor_tensor(out=tqt[:, 0, :], in0=pbu[:, 0:256], in1=wr, op=ALU.mult))
                            k.op("dve", [pbub], [tqb], lambda e, pbu=pbu, tqt=tqt, wi=wi: e.tensor_tensor(out=tqt[:, 1, :], in0=pbu[:, 256:512], in1=wi, op=ALU.mult))
                            k.op("dve", [pbub], [tqb], lambda e, pbu=pbu, tqt=tqt, wi=wi: e.tensor_tensor(out=tqt[:, 2, :], in0=pbu[:, 0:256], in1=wi, op=ALU.mult))
                            k.op("dve", [pbub], [tqb], lambda e, pbu=pbu, tqt=tqt, wr=wr: e.tensor_tensor(out=tqt[:, 3, :], in0=pbu[:, 256:512], in1=wr, op=ALU.mult))
                            k.op("pool", [tqb], [bpb], lambda e, tqt=tqt, ct0=ct0: e.tensor_tensor(
                                out=bpt[:, 0, ct0:ct0 + 2, :].rearrange("p a b -> p (a b)"), in0=tqt[:, 0, :], in1=tqt[:, 1, :], op=ALU.subtract))
                            k.op("pool", [tqb], [bpb], lambda e, tqt=tqt, ct0=ct0: e.tensor_tensor(
                                out=bpt[:, 1, ct0:ct0 + 2, :].rearrange("p a b -> p (a b)"), in0=tqt[:, 2, :], in1=tqt[:, 3, :], op=ALU.add))
                    hbs = []
                    for ct in range(16):
                        pg, pgb = ps_g.next()

                        def mmG(e, pg=pg, ct=ct):
                            e.matmul(pg[:, 0, :], bpt[:, 0, ct, :], tri[d][:, :], start=True, stop=True)
                            return e.matmul(pg[:, 1, :], bpt[:, 1, ct, :], tri[d][:, :], start=True, stop=True)
                        k.op("pe", [bpb], [pgb], mmG)
                        t4t, t4b = t4.next()
                        cr_ = carry[d][:, ct, 0:1]
                        ci_ = carry[d][:, ct, 1:2]
                        Wr = Ppos[d][0][:, ct, :]
                        Wi = Ppos[d][1][:, ct, :]
                        cb_ = Bcar[d][ct]
                        k.op("dve", [pgb, cb_], [t4b], lambda e, pg=pg, t4t=t4t, cr_=cr_, Wr=Wr: e.scalar_tensor_tensor(out=t4t[:, 0, :], in0=pg[:, 0, :], scalar=cr_, in1=Wr, op0=ALU.add, op1=ALU.mult))
                        k.op("dve", [pgb, cb_], [t4b], lambda e, pg=pg, t4t=t4t, ci_=ci_, Wi=Wi: e.scalar_tensor_tensor(out=t4t[:, 1, :], in0=pg[:, 1, :], scalar=ci_, in1=Wi, op0=ALU.add, op1=ALU.mult))
                        k.op("dve", [pgb, cb_], [t4b], lambda e, pg=pg, t4t=t4t, cr_=cr_, Wi=Wi: e.scalar_tensor_tensor(out=t4t[:, 2, :], in0=pg[:, 0, :], scalar=cr_, in1=Wi, op0=ALU.add, op1=ALU.mult))
                        k.op("dve", [pgb, cb_], [t4b], lambda e, pg=pg, t4t=t4t, ci_=ci_, Wr=Wr: e.scalar_tensor_tensor(out=t4t[:, 3, :], in0=pg[:, 1, :], scalar=ci_, in1=Wr, op0=ALU.add, op1=ALU.mult))
                        hft, hfb = hf_.next()
                        k.op("pool", [t4b], [hfb], lambda e, hft=hft, t4t=t4t: e.tensor_tensor(out=hft[:, 0, :], in0=t4t[:, 0, :], in1=t4t[:, 1, :], op=ALU.subtract))
                        k.op("pool", [t4b], [hfb], lambda e, hft=hft, t4t=t4t: e.tensor_tensor(out=hft[:, 1, :], in0=t4t[:, 2, :], in1=t4t[:, 3, :], op=ALU.add))
                        cmc = cm[:, (NCH if d == 1 else 0) + c:(NCH if d == 1 else 0) + c + 1]
                        k.op("pool", [hfb, Bt], [cb_], lambda e, hft=hft, ct=ct, cmc=cmc: e.tensor_scalar(
                            out=carry[d][:, ct, :], in0=hft[:, :, jl], scalar1=cmc, scalar2=None, op0=ALU.mult))
                        hbt, hbb = hb.next()
                        k.op("act", [hfb], [hbb], lambda e, hbt=hbt, hft=hft: e.copy(out=hbt[:, :, :], in_=hft[:, :, :]))
                        hbs.append((hbt, hbb))
                        if ct % 4 == 3:
                            yt = ct // 4
                            py, pyb = ps_y.next()
                            grp = hbs[-4:]

                            def mmY(e, py=py, grp=grp, yt=yt):
                                n = 0
                                for ctp in range(4):
                                    for r in range(2):
                                        ins = e.matmul(py[:, :], Cpad[d][:, 4 * yt + ctp, r, :], grp[ctp][0][:, r, :], start=(n == 0), stop=(n == 7))
                                        n += 1
                                return ins
                            k.op("pe", [g_[1] for g_ in grp], [pyb], mmY)
                            if d == 1:
                                yo, yob = ybs.next()
                                k.op("act", [pyb], [yob], lambda e, py=py, yo=yo: e.copy(out=yo[:, :], in_=py[:, :]))
                                k.dma("sp", [yob], [], lambda e, yo=yo, yt=yt: e.dma_start(out=YB[yt, :, tb:tb + 128], in_=yo[:, :]))
                            else:
                                k.op("dve", [pyb, ybb], [Bys], lambda e, py=py, yt=yt: e.tensor_tensor(out=ysum[:, yt, :], in0=py[:, :], in1=ybt[:, yt, :], op=ALU.add))
                                k.op("dve", [Bys, u4b, Bt], [Bys], lambda e, yt=yt: e.scalar_tensor_tensor(
                                    out=ysum[:, yt, :], in0=u4[:, yt, :], scalar=dskip[:, yt:yt + 1], in1=ysum[:, yt, :], op0=ALU.mult, op1=ALU.add))
                    if d == 0:
                        k.op("pool", [Bys], [Bgt], lambda e: e.tensor_tensor(out=gtm[:, :, :], in0=ysum[:, :, :], in1=ysum[:, :, :], op=ALU.mult))
                        k.op("pool", [Bgt], [Bgt], lambda e: e.tensor_scalar(out=gtm[:, :, :], in0=gtm[:, :, :], scalar1=0.044715, scalar2=1.0, op0=ALU.mult, op1=ALU.add))
                        k.op("pool", [Bgt, Bys], [Bgt], lambda e: e.tensor_tensor(out=gtm[:, :, :], in0=gtm[:, :, :], in1=ysum[:, :, :], op=ALU.mult))
                        k.op("act", [Bgt], [Bgt], lambda e: e.activation(out=gtm[:, :, :], in_=gtm[:, :, :], func=AF.Sigmoid, scale=1.5957691216057308))
                        k.op("dve", [Bgt, Bys], [Bygf], lambda e: e.tensor_tensor(out=ygf[:, :, :], in0=gtm[:, :, :], in1=ysum[:, :, :], op=ALU.mult))
                        k.op("act", [Bygf], [Bygb], lambda e: e.copy(out=ygb[:, :, :], in_=ygf[:, :, :]))
                        sot, sob = sso.next()
                        for ot in range(4):
                            pgl, pglb = ps_gl.next()

                            def mmGL(e, pgl=pgl, ot=ot):
                                for it in range(4):
                                    ins = e.matmul(pgl[:, :], gluw[:, it, ot * 128:(ot + 1) * 128], ygb[:, it, :], start=(it == 0), stop=(it == 3))
                                return ins
                            k.op("pe", [Bygb, Bt], [pglb], mmGL)
                            gt_, gtb = gate.next()
                            k.op("act", [pglb, Bt], [gtb], lambda e, pgl=pgl, gt_=gt_, ot=ot: e.activation(
                                out=gt_[:, :], in_=pgl[:, :], func=AF.Sigmoid, bias=glub[:, ot:ot + 1]))
                            k.op("dve", [gtb, Bygf], [sob], lambda e, gt_=gt_, ot=ot: e.tensor_tensor(out=sot[:, ot, :], in0=ygf[:, ot, :], in1=gt_[:, :], op=ALU.mult))
                        k.dma("sp", [sob], [], lambda e: e.dma_start(out=SSM[:, :, tb:tb + 128].rearrange("c p t -> p c t"), in_=sot[:, :, :]))
                k.barrier()
    k.stack = prev


class LNR:
    def __init__(self, P, k, L, gam_ap, bet_ap, wr_ap, pfx, nrows=128):
        self.k = k
        self.L = L
        self.nrows = nrows
        self.Bc = Buf(pfx + "const")
        self.gb = k.sb(pfx + "gb", [128, 2, D], F32)
        k.dma("sp", [], [self.Bc], lambda e: e.dma_start(out=self.gb[:, 0, :], in_=gam_ap.partition_broadcast(128)))
        k.dma("sp", [], [self.Bc], lambda e: e.dma_start(out=self.gb[:, 1, :], in_=bet_ap.partition_broadcast(128)))
        self.eps = k.sb(pfx + "eps", [128, 1], F32)
        k.op("dve", [], [self.Bc], lambda e: e.memset(self.eps[:, :], LN_EPS))
        self.z = Ring(k, pfx + "z", [128, D], F32, 2)
        self.st = Ring(k, pfx + "st", [128, 2, 6], F32, 2)
        self.mv = Ring(k, pfx + "mv", [128, 4], F32, 2)
        self.xl = Ring(k, pfx + "xl", [128, D], F32, 2)
        self.router = wr_ap is not None
        if self.router:
            self.wr = k.sb(pfx + "wr", [128, DK, NE], F32)
            k.dma("sp", [], [self.Bc], lambda e: e.dma_start(out=self.wr[:, :, :], in_=wr_ap.rearrange("(dk p) e -> p dk e", p=128)))
            self.xlT = Ring(k, pfx + "xlT", [128, DK, 128], F32, 2)
            self.pst = Ring(k, pfx + "pst", [128, 4, 128], F32, 2, psum=True)
            self.psl = Ring(k, pfx + "psl", [128, NE], F32, 1, psum=True)
            self.sm = Ring(k, pfx + "sm", [128, 4], F32, 2)
            self.ex = Ring(k, pfx + "ex", [128, NE], F32, 2)

    def ln(self, xt, xb_, m_src, m_bufs, m_is_two_bank=True):
        k = self.k
        n = self.nrows
        zt, zb = self.z.next()
        for h in range(2):
            k.op("dve", [xb_] + m_bufs, [zb], lambda e, h=h: e.scalar_tensor_tensor(
                out=zt[0:n, h * 512:(h + 1) * 512], in0=xt[0:n, h * 512:(h + 1) * 512], scalar=ALPHA, in1=m_src(h), op0=ALU.mult, op1=ALU.add))
        stt, stb = self.st.next()
        for h in range(2):
            k.op("dve", [zb], [stb], lambda e, h=h: e.bn_stats(out=stt[0:n, h, :], in_=zt[0:n, h * 512:(h + 1) * 512]))
        mvt, mvb = self.mv.next()
        k.op("dve", [stb], [mvb], lambda e: e.bn_aggr(out=mvt[0:n, 0:2], in_=stt[0:n, :, :].rearrange("p a b -> p (a b)")))
        k.op("act", [mvb, self.Bc], [mvb], lambda e: e.activation(out=mvt[0:n, 2:3], in_=mvt[0:n, 1:2], func=AF.Sqrt, bias=self.eps[0:n, 0:1]))
        k.op("dve", [mvb], [mvb], lambda e: e.reciprocal(out=mvt[0:n, 2:3], in_=mvt[0:n, 2:3]))
        k.op("dve", [mvb], [mvb], lambda e: e.scalar_tensor_tensor(out=mvt[0:n, 3:4], in0=mvt[0:n, 0:1], scalar=-1.0, in1=mvt[0:n, 2:3], op0=ALU.mult, op1=ALU.mult))
        xlt, xlb = self.xl.next()
        k.op("act", [zb, mvb], [xlb], lambda e: e.activation(out=xlt[0:n, :], in_=zt[0:n, :], func=AF.Identity, scale=mvt[0:n, 2:3], bias=mvt[0:n, 3:4]))
        k.op("pool", [xlb, self.Bc], [xlb], lambda e: e.tensor_tensor(out=xlt[0:n, :], in0=xlt[0:n, :], in1=self.gb[0:n, 0, :], op=ALU.mult))
        k.op("pool", [xlb, self.Bc], [xlb], lambda e: e.tensor_tensor(out=xlt[0:n, :], in0=xlt[0:n, :], in1=self.gb[0:n, 1, :], op=ALU.add))
        return xlt, xlb

    def route(self, xlt, xlb, aff_dst, aff_buf):
        k = self.k
        ident_f = self.L["ident_f"]
        xTt, xTb = self.xlT.next()
        for g in range(2):
            pt, ptb = self.pst.next()

            def tr(e, pt=pt, g=g):
                for j in range(4):
                    dk = g * 4 + j
                    ins = e.transpose(out=pt[:, j, :], in_=xlt[:, dk * 128:(dk + 1) * 128], identity=ident_f[:, :])
                return ins
            k.op("pe", [xlb], [ptb], tr)
            k.op("act", [ptb], [xTb], lambda e, pt=pt, g=g: e.copy(out=xTt[:, g * 4:(g + 1) * 4, :], in_=pt[:, :, :]))
        pl, plb = self.psl.next()

        def mm(e):
            for dk in range(DK):
                ins = e.matmul(pl[:, :], xTt[:, dk, :], self.wr[:, dk, :], start=(dk == 0), stop=(dk == DK - 1))
            return ins
        k.op("pe", [xTb, self.Bc], [plb], mm)
        smt, smb = self.sm.next()
        ext, exb = self.ex.next()
        k.op("dve", [plb], [smb], lambda e: e.reduce_max(out=smt[:, 0:1], in_=pl[:, :], axis=AX.X))
        k.op("dve", [smb], [smb], lambda e: e.tensor_scalar(out=smt[:, 1:2], in0=smt[:, 0:1], scalar1=-1.0, scalar2=None, op0=ALU.mult))
        k.op("act", [plb, smb], [exb, smb], lambda e: e.activation(out=ext[:, :], in_=pl[:, :], func=AF.Exp, bias=smt[:, 1:2], accum_out=smt[:, 2:3]))
        k.op("dve", [smb], [smb], lambda e: e.reciprocal(out=smt[:, 3:4], in_=smt[:, 2:3]))
        k.op("dve", [exb, smb], [aff_buf], lambda e: e.tensor_scalar(out=aff_dst, in0=ext[:, :], scalar1=smt[:, 3:4], scalar2=None, op0=ALU.mult))


def phase_A4(P, k, L):
    cfg = P.cfg
    ATT, SSM, XLN, XBF, x_in, root = L["ATT"], L["SSM"], L["XLN"], L["XBF"], L["x_in"], L["root"]
    aff_sb, Baff = L["aff_sb"], L["Baff"]
    prev = k.stack
    with ExitStack() as st:
        k.stack = st
        wo = k.sb("a4_wo", [128, DK, D], BF16); Bwo = Buf("a4_wo")
        k.dma("pool", [], [Bwo], lambda e: e.dma_start(out=wo[:, :, :], in_=L["w_out_even"].ap().rearrange("(dk p) n -> p dk n", p=128)))
        lnr = LNR(P, k, L, L["ln_mix_g"][0:1, :], L["ln_mix_b"][0:1, :], L["w_router"][0], "a4_")
        cat = Ring(k, "a4_cat", [128, 8, 128], BF16, 3)
        xr = Ring(k, "a4_x", [128, D], F32, 3)
        psm = Ring(k, "a4_psm", [128, 2, 512], F32, 2, psum=True)
        for c in range(cfg.NTILE):
            tb = c * 128
            ct, cb = cat.next()
            k.dma("sp", [], [cb], lambda e: e.dma_start(out=ct[:, 0:4, :], in_=ATT[:, :, tb:tb + 128].rearrange("c p t -> p c t")))
            k.dma("sp", [], [cb], lambda e: e.dma_start(out=ct[:, 4:8, :], in_=SSM[:, :, tb:tb + 128].rearrange("c p t -> p c t")))
            xt, xb_ = xr.next()
            k.dma("sp", [], [xb_], lambda e: e.dma_start(out=xt[:, :], in_=x_in[tb:tb + 128, :]))
            pm, pmb = psm.next()

            def mm(e):
                for h in range(2):
                    for kc in range(8):
                        ins = e.matmul(pm[:, h, :], ct[:, kc, :], wo[:, kc, h * 512:(h + 1) * 512], start=(kc == 0), stop=(kc == 7))
                return ins
            k.op("pe", [cb, Bwo], [pmb], mm)
            xlt, xlb = lnr.ln(xt, xb_, lambda h: pm[:, h, :], [pmb])
            k.dma("sp", [xlb], [], lambda e: e.dma_start(out=XLN[tb:tb + 128, :], in_=xlt[:, :]))
            k.dma("pool", [xlb], [], lambda e: e.dma_start(out=XBF[tb:tb + 128, :], in_=xlt[:, :]))
            lnr.route(xlt, xlb, aff_sb[:, c, :], Baff)
        if "AFFD" in P.dbg:
            k.dma("sp", [Baff], [], lambda e: e.dma_start(out=L["AFFD"][:, :], in_=aff_sb[:, :, :].rearrange("p c e -> p (c e)")))
        k.barrier()
    k.stack = prev


def phase_B(P, k, L, layer):
    cfg = P.cfg
    T, IDX, root = L["T"], L["IDX"], L["root"]
    aff_sb, Baff = L["aff_sb"], L["Baff"]
    ones_f = L["ones_f"]
    NTL, CAP = cfg.NTILE, cfg.CAP
    prev = k.stack
    with ExitStack() as st:
        k.stack = st
        Bb = Buf("b_small")
        cmp = k.sb("b_cmp", [128, NTL, NE], F32); Bcmp = Buf("b_cmp")
        cs = k.sb("b_cs", [128, NTL, NE], F32); Bcs = Buf("b_cs")
        sl_i = k.sb("b_sli", [128, NTL, NE], I32); Bsl = Buf("b_sli")
        zer = k.sb("b_zer", [128, NTL], F32)
        tok = k.sb("b_tok", [128, NTL], F32)
        ust = k.sb("b_ust", [128, 128], F32)
        k.op("dve", [], [Bb], lambda e: e.memset(zer[:, :], 0.0))
        k.dma("sp", [], [Bb], lambda e: e.dma_start(out=tok[:, :], in_=T["tok%d" % layer][:, :]))
        k.dma("sp", [], [Bb], lambda e: e.dma_start(out=ust[:, :], in_=T["ustrict"][:, :]))
        sm = k.sb("b_sm", [128, 8, NE], F32)
        pst = k.ps("b_ps", [128, NE], F32); Bps = Buf("b_ps")
        k.op("dve", [], [Bb], lambda e: e.memset(sm[:, 0, :], 0.0))
        k.op("dve", [], [Bb], lambda e: e.memset(sm[:, 1, :], 1.0))

        def compare(thr_row):
            k.op("dve", [Baff, Bb], [Bcmp], lambda e: e.tensor_tensor(
                out=cmp[:, :, :], in0=aff_sb[:, :, :], in1=sm[:, thr_row, :].unsqueeze(1).to_broadcast([128, NTL, NE]), op=ALU.is_gt))
        for it in range(30):
            k.op("dve", [Bb], [Bb], lambda e: e.tensor_tensor(out=sm[:, 2, :], in0=sm[:, 0, :], in1=sm[:, 1, :], op=ALU.add))
            k.op("dve", [Bb], [Bb], lambda e: e.tensor_scalar(out=sm[:, 2, :], in0=sm[:, 2, :], scalar1=0.5, scalar2=None, op0=ALU.mult))
            compare(2)
            k.op("dve", [Bcmp], [Bb], lambda e: e.tensor_reduce(out=sm[:, 3, :], in_=cmp[:, :, :].rearrange("p c e -> p e c"), axis=AX.X, op=ALU.add))
            k.op("pe", [Bb], [Bps], lambda e: e.matmul(pst[:, :], ones_f[:, :], sm[:, 3, :], start=True, stop=True))
            k.op("dve", [Bps], [Bb], lambda e: e.tensor_scalar(out=sm[:, 4, :], in0=pst[:, :], scalar1=float(CAP) - 0.5, scalar2=None, op0=ALU.is_ge))
            k.op("dve", [Bb], [Bb], lambda e: e.tensor_tensor(out=sm[:, 5, :], in0=sm[:, 2, :], in1=sm[:, 0, :], op=ALU.subtract))
            k.op("dve", [Bb], [Bb], lambda e: e.tensor_tensor(out=sm[:, 5, :], in0=sm[:, 5, :], in1=sm[:, 4, :], op=ALU.mult))
            k.op("dve", [Bb], [Bb], lambda e: e.tensor_tensor(out=sm[:, 0, :], in0=sm[:, 0, :], in1=sm[:, 5, :], op=ALU.add))
            k.op("dve", [Bb], [Bb], lambda e: e.tensor_tensor(out=sm[:, 5, :], in0=sm[:, 1, :], in1=sm[:, 2, :], op=ALU.subtract))
            k.op("dve", [Bb], [Bb], lambda e: e.tensor_tensor(out=sm[:, 5, :], in0=sm[:, 5, :], in1=sm[:, 4, :], op=ALU.mult))
            k.op("dve", [Bb], [Bb], lambda e: e.tensor_tensor(out=sm[:, 1, :], in0=sm[:, 2, :], in1=sm[:, 5, :], op=ALU.add))
        compare(0)
        for ex in range(NE):
            k.op("dve", [Bcmp, Bb], [Bcs], lambda e, ex=ex: e.tensor_tensor_scan(
                out=cs[:, :, ex], data0=cmp[:, :, ex], data1=zer[:, :], initial=0.0, op0=ALU.add, op1=ALU.add))
        k.op("pe", [Bcs, Bb], [Bps], lambda e: e.matmul(pst[:, :], ust[:, :], cs[:, NTL - 1, :], start=True, stop=True))
        k.op("dve", [Bps], [Bb], lambda e: e.tensor_scalar(out=sm[:, 6, :], in0=pst[:, :], scalar1=-1.0, scalar2=None, op0=ALU.add))
        k.op("dve", [Bcs, Bb], [Bcs], lambda e: e.tensor_tensor(
            out=cs[:, :, :], in0=cs[:, :, :], in1=sm[:, 6, :].unsqueeze(1).to_broadcast([128, NTL, NE]), op=ALU.add))
        BIG = float(1 << 20)
        k.op("dve", [Bcmp], [Bcmp], lambda e: e.tensor_scalar(out=cmp[:, :, :], in0=cmp[:, :, :], scalar1=-BIG, scalar2=BIG, op0=ALU.mult, op1=ALU.add))
        k.op("dve", [Bcs, Bcmp], [Bcs], lambda e: e.tensor_tensor(out=cs[:, :, :], in0=cs[:, :, :], in1=cmp[:, :, :], op=ALU.add))
        k.op("dve", [Bcs], [Bsl], lambda e: e.tensor_copy(out=sl_i[:, :, :], in_=cs[:, :, :]))
        src = Ring(k, "b_src", [128, NTL, 2], F32, 2)
        for ex in range(NE):
            st_, sb_ = src.next()
            k.op("act", [Bb], [sb_], lambda e: e.copy(out=st_[:, :, 0], in_=tok[:, :]))
            k.op("act", [Baff], [sb_], lambda e, ex=ex: e.copy(out=st_[:, :, 1], in_=aff_sb[:, :, ex]))
            for c in range(NTL):
                k.dma("pool", [Bsl, sb_], [], lambda e, c=c, ex=ex: e.indirect_dma_start(
                    out=IDX[ex][:, :], out_offset=bass.IndirectOffsetOnAxis(ap=sl_i[:, c, ex:ex + 1], axis=0),
                    in_=st_[:, c, :], in_offset=None, bounds_check=L["bnd_reg"], oob_is_err=False))
        k.barrier()
    k.stack = prev


def phase_C(P, k, L, layer):
    cfg = P.cfg
    IDX, XBF, YY, root = L["IDX"], L["XBF"], L["YY"], L["root"]
    ident_b = L["ident_b"]
    w1d, w3d, w2d = L["w_ff1"], L["w_ff3"], L["w_ff2"]
    CAP, TS, NT = cfg.CAP, cfg.TS, cfg.NT
    NSUB = TS // 128
    prev = k.stack
    with ExitStack() as st:
        k.stack = st
        zt = k.sb("c_zt", [128, 4 * D], F32); Bz = Buf("c_zt")
        k.op("dve", [], [Bz], lambda e: e.memset(zt[:, :], 0.0))
        for r0 in range(0, NT, 512):
            k.dma("sp", [Bz], [], lambda e, r0=r0: e.dma_start(out=YY[r0:r0 + 512, :].rearrange("(p a) d -> p (a d)", a=4), in_=zt[:, :]))
        k.barrier()
        w1 = k.sb("c_w1", [128, DK, FH], BF16)
        w3 = k.sb("c_w3", [128, DK, FH], BF16)
        w2 = k.sb("c_w2", [128, FK, D], BF16)
        Bw13 = Buf("c_w13"); Bw2 = Buf("c_w2")
        idf = Ring(k, "c_idf", [128, CAP // 128, 2], F32, 2)
        idi = Ring(k, "c_idi", [128, CAP // 128], I32, 2)
        xg = Ring(k, "c_xg", [128, NSUB, D], BF16, 2)
        xT = Ring(k, "c_xT", [128, DK, TS], BF16, 2)
        gT = Ring(k, "c_gT", [128, FK, TS], BF16, 2)
        sl = Ring(k, "c_sl", [128, TS], F32, 2)
        osb = Ring(k, "c_osb", [128, D], F32, 3)
        pst = Ring(k, "c_pst", [128, TS], BF16, 2, psum=True)
        ph1 = Ring(k, "c_ph1", [128, TS], F32, 2, psum=True)
        ph3 = Ring(k, "c_ph3", [128, TS], F32, 2, psum=True)
        pso = Ring(k, "c_pso", [128, 2, 512], F32, 1, psum=True)
        for ex in range(NE):
            idft, idfb = idf.next()
            ncol = CAP // 128
            nsp = 4 if ncol >= 16 else 1
            cpp = ncol // nsp
            for sp_ in range(nsp):
                k.dma("sp", [], [idfb], lambda e, sp_=sp_: e.dma_start(
                    out=idft[:, sp_ * cpp:(sp_ + 1) * cpp, :],
                    in_=IDX[ex][sp_ * cpp * 128:(sp_ + 1) * cpp * 128, :].rearrange("(c p) t -> p c t", p=128)))
            idit, idib = idi.next()
            k.op("dve", [idfb], [idib], lambda e: e.tensor_copy(out=idit[:, :], in_=idft[:, :, 0]))
            for hfi in range(2):
                f0 = hfi * FH
                k.dma("pool", [], [Bw13], lambda e: e.dma_start(out=w1[:, :, :], in_=w1d[layer, ex, :, f0:f0 + FH].rearrange("(dk p) f -> p dk f", p=128)))
                k.dma("pool", [], [Bw13], lambda e: e.dma_start(out=w3[:, :, :], in_=w3d[layer, ex, :, f0:f0 + FH].rearrange("(dk p) f -> p dk f", p=128)))
                k.dma("pool", [], [Bw2], lambda e: e.dma_start(out=w2[:, :, :], in_=w2d[layer, ex, f0:f0 + FH, :].rearrange("(fk p) d -> p fk d", p=128)))
                def gather(ti_):
                    xgt_, xgb_ = xg.next()
                    for j in range(NSUB):
                        col = ti_ * NSUB + j
                        k.dma("pool", [idib], [xgb_], lambda e, j=j, col=col: e.indirect_dma_start(
                            out=xgt_[:, j, :], out_offset=None, in_=XBF[:, :],
                            in_offset=bass.IndirectOffsetOnAxis(ap=idit[:, col:col + 1], axis=0)))
                    return xgt_, xgb_
                nxt = gather(0)
                for ti in range(CAP // TS):
                    xgt, xgb = nxt
                    if ti + 1 < CAP // TS:
                        nxt = gather(ti + 1)
                    xTt, xTb = xT.next()
                    for dk in range(DK):
                        pt, ptb = pst.next()

                        def tr(e, pt=pt, dk=dk):
                            for j in range(NSUB):
                                ins = e.transpose(out=pt[:, j * 128:(j + 1) * 128], in_=xgt[:, j, dk * 128:(dk + 1) * 128], identity=ident_b[:, :])
                            return ins
                        k.op("pe", [xgb], [ptb], tr)
                        if dk % 2 == 0:
                            k.op("act", [ptb], [xTb], lambda e, pt=pt, dk=dk: e.copy(out=xTt[:, dk, :], in_=pt[:, :]))
                        else:
                            k.op("dve", [ptb], [xTb], lambda e, pt=pt, dk=dk: e.tensor_copy(out=xTt[:, dk, :], in_=pt[:, :]))
                    gTt, gTb = gT.next()
                    for fk in range(FK):
                        p1, p1b = ph1.next()
                        p3, p3b = ph3.next()

                        def mm1(e, p1=p1, fk=fk):
                            for dk in range(DK):
                                ins = e.matmul(p1[:, :], w1[:, dk, fk * 128:(fk + 1) * 128], xTt[:, dk, :], start=(dk == 0), stop=(dk == DK - 1))
                            return ins

                        def mm3(e, p3=p3, fk=fk):
                            for dk in range(DK):
                                ins = e.matmul(p3[:, :], w3[:, dk, fk * 128:(fk + 1) * 128], xTt[:, dk, :], start=(dk == 0), stop=(dk == DK - 1))
                            return ins
                        k.op("pe", [Bw13, xTb], [p1b], mm1)
                        k.op("pe", [Bw13, xTb], [p3b], mm3)
                        slt, slb = sl.next()
                        k.op("act", [p1b], [slb], lambda e, p1=p1, slt=slt: e.activation(out=slt[:, :], in_=p1[:, :], func=AF.Silu))
                        k.op("dve", [slb, p3b], [gTb], lambda e, p3=p3, slt=slt, fk=fk: e.tensor_tensor(out=gTt[:, fk, :], in0=slt[:, :], in1=p3[:, :], op=ALU.mult))
                    for j in range(NSUB):
                        col = ti * NSUB + j
                        po, pob = pso.next()

                        def mmo(e, po=po, j=j):
                            for h in range(2):
                                for fk in range(FK):
                                    ins = e.matmul(po[:, h, :], gTt[:, fk, j * 128:(j + 1) * 128], w2[:, fk, h * 512:(h + 1) * 512], start=(fk == 0), stop=(fk == FK - 1))
                            return ins
                        k.op("pe", [Bw2, gTb], [pob], mmo)
                        ot, ob = osb.next()
                        k.op("act", [pob, idfb], [ob], lambda e, po=po, ot=ot, col=col: e.activation(
                            out=ot[:, :], in_=po[:, :, :].rearrange("p a b -> p (a b)"), func=AF.Copy, scale=idft[:, col, 1:2]))
                        k.dma("pool", [ob, idib], [], lambda e, ot=ot, col=col: e.indirect_dma_start(
                            out=YY[:, :], out_offset=bass.IndirectOffsetOnAxis(ap=idit[:, col:col + 1], axis=0),
                            in_=ot[:, :], in_offset=None, compute_op=ALU.add))
        k.barrier()
    k.stack = prev


def phase_D(P, k, L, layer, dst):
    cfg = P.cfg
    XLN, YY, root = L["XLN"], L["YY"], L["root"]
    prev = k.stack
    with ExitStack() as st:
        k.stack = st
        lnr = LNR(P, k, L, L["ln_ffn_g"][layer:layer + 1, :], L["ln_ffn_b"][layer:layer + 1, :], None, "d_")
        xr = Ring(k, "d_x", [128, D], F32, 3)
        yr = Ring(k, "d_y", [128, D], F32, 3)
        for c in range(cfg.NTILE):
            tb = c * 128
            xt, xb_ = xr.next()
            yt, yb_ = yr.next()
            k.dma("sp", [], [xb_], lambda e: e.dma_start(out=xt[:, :], in_=XLN[tb:tb + 128, :]))
            k.dma("sp", [], [yb_], lambda e: e.dma_start(out=yt[:, :], in_=YY[tb:tb + 128, :]))
            xlt, xlb = lnr.ln(xt, xb_, lambda h: yt[:, h * 512:(h + 1) * 512], [yb_])
            k.dma("sp", [xlb], [], lambda e: e.dma_start(out=dst[tb:tb + 128, :], in_=xlt[:, :]))
        k.barrier()
    k.stack = prev


def phase_B0(P, k, L):
    phase_B(P, k, L, 0)


def phase_C0(P, k, L):
    phase_C(P, k, L, 0)


def phase_D0(P, k, L):
    phase_D(P, k, L, 0, L["X1"])


def phase_F(P, k, L):
    cfg = P.cfg
    X1, XLN, XBF, AFS, T, root = L["X1"], L["XLN"], L["XBF"], L["AFS"], L["T"], L["root"]
    aff_sb, Baff, ident_b = L["aff_sb"], L["Baff"], L["ident_b"]
    SL, NSEQ, NCH, N2E, KPER = cfg.SL, cfg.NSEQ, cfg.NCH, cfg.N2E, cfg.KPER
    prev = k.stack
    with ExitStack() as st:
        k.stack = st
        Bt = Buf("f_setup")
        wcs = [k.sb("f_wc%d" % i, [128, DK, D], BF16) for i in range(2)]
        c1b = k.sb("f_c1", [128, 3, 128], BF16)
        c2b = k.sb("f_c2", [N2E, 2, KPER * 128], BF16)
        tw = k.sb("f_tw", [128, 2, N2E], F32)
        fidx = k.sb("f_idx", [128, NSEQ * N2E], I32)
        k.dma("pool", [], [Bt], lambda e: e.dma_start(out=c1b[:, 0, :], in_=T["c1"][:, :]))
        k.dma("pool", [], [Bt], lambda e: e.dma_start(out=c1b[:, 1, :], in_=T["s1"][:, :]))
        k.op("dve", [Bt], [Bt], lambda e: e.tensor_scalar(out=c1b[:, 2, :], in0=c1b[:, 1, :], scalar1=-1.0, scalar2=None, op0=ALU.mult))
        k.dma("pool", [], [Bt], lambda e: e.dma_start(out=c2b[:, 0, :], in_=T["c2p"][:, :]))
        k.dma("pool", [], [Bt], lambda e: e.dma_start(out=c2b[:, 1, :], in_=T["s2p"][:, :]))
        k.dma("sp", [], [Bt], lambda e: e.dma_start(out=tw[:, 0, :], in_=T["twr"][:, :]))
        k.dma("sp", [], [Bt], lambda e: e.dma_start(out=tw[:, 1, :], in_=T["twi"][:, :]))
        k.dma("sp", [], [Bt], lambda e: e.dma_start(out=fidx[:, :], in_=T["fidx"][:, :]))
        with ExitStack() as st2:
            k.stack = st2
            wob = k.sb("f_wob", [128, DK, D], BF16)
            ccb = k.sb("f_ccb", [128, 2, 512], BF16)
            k.dma("pool", [], [Bt], lambda e: e.dma_start(out=wob[:, :, :], in_=L["w_out_odd"].ap().rearrange("(dk p) n -> p dk n", p=128)))
            k.dma("pool", [], [Bt], lambda e: e.dma_start(out=ccb[:, 0, :], in_=T["cc2"][:, :]))
            k.dma("pool", [], [Bt], lambda e: e.dma_start(out=ccb[:, 1, :], in_=T["sc2"][:, :]))
            psw = Ring(k, "f_psw", [128, 2, 512], F32, 2, psum=True)
            for i in range(2):
                for mc in range(8):
                    g, mm = mc // 2, mc % 2
                    pw, pwb = psw.next()

                    def mmw(e, pw=pw, g=g, mm=mm, i=i):
                        for h in range(2):
                            for kk in range(2):
                                ins = e.matmul(pw[:, h, :], ccb[:, i, (kk * 2 + mm) * 128:(kk * 2 + mm + 1) * 128],
                                               wob[:, 2 * g + kk, h * 512:(h + 1) * 512], start=(kk == 0), stop=(kk == 1))
                        return ins
                    k.op("pe", [Bt], [pwb], mmw)
                    k.op("act", [pwb], [Bt], lambda e, pw=pw, i=i, mc=mc: e.copy(out=wcs[i][:, mc, :], in_=pw[:, :, :].rearrange("p a b -> p (a b)")))
            k.barrier()
        k.stack = st
        for s in range(NSEQ):
            base = s * SL
            with ExitStack() as sa:
                k.stack = sa
                xg = Ring(k, "f_xg", [128, D], F32, 2)
                xb = Ring(k, "f_xb", [128, D], BF16, 2)
                xT = Ring(k, "f_xT", [128, DK, 128], BF16, 2)
                ub = Ring(k, "f_ub", [128, 2, D], BF16, 2)
                tt = Ring(k, "f_tt", [128, 2, 512], F32, 2)
                apb = Ring(k, "f_apb", [128, 2, D], BF16, 2)
                pst = Ring(k, "f_pst", [128, 4, 128], BF16, 2, psum=True)
                psu = Ring(k, "f_psu", [128, 4, 512], F32, 1, psum=True)
                psa = Ring(k, "f_psa", [128, 2, 512], F32, 1, psum=True)
                for j in range(N2E):
                    xgt, xgb = xg.next()
                    col = s * N2E + j
                    k.dma("pool", [Bt], [xgb], lambda e: e.indirect_dma_start(
                        out=xgt[:, :], out_offset=None, in_=X1[:, :], in_offset=bass.IndirectOffsetOnAxis(ap=fidx[:, col:col + 1], axis=0)))
                    xbt, xbb = xb.next()
                    k.op("act", [xgb], [xbb], lambda e: e.copy(out=xbt[:, :], in_=xgt[:, :]))
                    xTt, xTb = xT.next()
                    for g in range(2):
                        pt, ptb = pst.next()

                        def tr(e, pt=pt, g=g):
                            for jj in range(4):
                                dk = g * 4 + jj
                                ins = e.transpose(out=pt[:, jj, :], in_=xbt[:, dk * 128:(dk + 1) * 128], identity=ident_b[:, :])
                            return ins
                        k.op("pe", [xbb], [ptb], tr)
                        k.op("dve", [ptb], [xTb], lambda e, pt=pt, g=g: e.tensor_copy(out=xTt[:, g * 4:(g + 1) * 4, :], in_=pt[:, :, :]))
                    pu, pub = psu.next()

                    def mmu(e):
                        for i in range(2):
                            for h in range(2):
                                for dk in range(DK):
                                    ins = e.matmul(pu[:, i * 2 + h, :], xTt[:, dk, :], wcs[i][:, dk, h * 512:(h + 1) * 512], start=(dk == 0), stop=(dk == DK - 1))
                        return ins
                    k.op("pe", [xTb, Bt], [pub], mmu)
                    ubt, ubb = ub.next()
                    k.op("act", [pub], [ubb], lambda e: e.copy(out=ubt[:, 0, :], in_=pu[:, 0:2, :].rearrange("p a b -> p (a b)")))
                    k.op("dve", [pub], [ubb], lambda e: e.tensor_copy(out=ubt[:, 1, :], in_=pu[:, 2:4, :].rearrange("p a b -> p (a b)")))
                    apt, apbb = apb.next()
                    for h in range(2):
                        pa, pab = psa.next()
                        hs = slice(h * 512, (h + 1) * 512)

                        def mma(e, pa=pa, hs=hs):
                            e.matmul(pa[:, 0, :], c1b[:, 0, :], ubt[:, 0, hs], start=True, stop=False)
                            e.matmul(pa[:, 0, :], c1b[:, 1, :], ubt[:, 1, hs], start=False, stop=True)
                            e.matmul(pa[:, 1, :], c1b[:, 0, :], ubt[:, 1, hs], start=True, stop=False)
                            return e.matmul(pa[:, 1, :], c1b[:, 2, :], ubt[:, 0, hs], start=False, stop=True)
                        k.op("pe", [ubb, Bt], [pab], mma)
                        ttt, ttb = tt.next()
                        k.op("act", [pab, Bt], [ttb], lambda e, pa=pa, ttt=ttt: e.activation(out=ttt[:, 0, :], in_=pa[:, 1, :], func=AF.Copy, scale=tw[:, 1, j:j + 1]))
                        k.op("act", [pab, Bt], [ttb], lambda e, pa=pa, ttt=ttt: e.activation(out=ttt[:, 1, :], in_=pa[:, 1, :], func=AF.Copy, scale=tw[:, 0, j:j + 1]))
                        k.op("dve", [pab, ttb, Bt], [apbb], lambda e, pa=pa, ttt=ttt, hs=hs: e.scalar_tensor_tensor(
                            out=apt[:, 0, hs], in0=pa[:, 0, :], scalar=tw[:, 0, j:j + 1], in1=ttt[:, 0, :], op0=ALU.mult, op1=ALU.subtract))
                        k.op("dve", [pab, ttb, Bt], [apbb], lambda e, pa=pa, ttt=ttt, hs=hs: e.scalar_tensor_tensor(
                            out=apt[:, 1, hs], in0=pa[:, 0, :], scalar=tw[:, 1, j:j + 1], in1=ttt[:, 1, :], op0=ALU.mult, op1=ALU.add))
                    k.dma("sp", [apbb], [], lambda e: e.dma_start(out=AFS[:, j, :, :], in_=apt[:, :, :]))
                k.barrier()
            with ExitStack() as sc:
                k.stack = sc
                lnr = LNR(P, k, L, L["ln_mix_g"][1:2, :], L["ln_mix_b"][1:2, :], L["w_router"][1], "fl_")
                a2 = Ring(k, "f_a2", [N2E, KPER, 2, D], BF16, 2 if KPER <= 4 else 1)
                xr = Ring(k, "f_xr", [128, D], F32, 2)
                psm = Ring(k, "f_psm", [128, 2, 512], F32, 2, psum=True)
                x1v = X1[base:base + SL, :].rearrange("(j k) d -> k j d", k=128)
                xlnv = XLN[base:base + SL, :].rearrange("(j k) d -> k j d", k=128)
                xbfv = XBF[base:base + SL, :].rearrange("(j k) d -> k j d", k=128)
                for q in range(NCH):
                    a2t, a2b = a2.next()
                    k.dma("sp", [], [a2b], lambda e: e.dma_start(out=a2t[:, :, :, :], in_=AFS[q * KPER:(q + 1) * KPER, :, :, :].rearrange("k j r c -> j k r c")))
                    xt, xb_ = xr.next()
                    for v in range(KPER):
                        k.dma("sp", [], [xb_], lambda e, v=v: e.dma_start(out=xt[v * N2E:(v + 1) * N2E, :], in_=x1v[q * KPER + v]))
                    pm, pmb = psm.next()

                    def mmm(e):
                        for h in range(2):
                            n = 0
                            for v in range(KPER):
                                for r in range(2):
                                    ins = e.matmul(pm[:, h, :], c2b[:, r, v * 128:(v + 1) * 128], a2t[:, v, r, h * 512:(h + 1) * 512],
                                                   start=(n == 0), stop=(n == 2 * KPER - 1))
                                    n += 1
                        return ins
                    k.op("pe", [a2b, Bt], [pmb], mmm)
                    xlt, xlb = lnr.ln(xt, xb_, lambda h: pm[:, h, :], [pmb])
                    for v in range(KPER):
                        k.dma("sp", [xlb], [], lambda e, v=v: e.dma_start(out=xlnv[q * KPER + v], in_=xlt[v * N2E:(v + 1) * N2E, :]))
                        k.dma("pool", [xlb], [], lambda e, v=v: e.dma_start(out=xbfv[q * KPER + v], in_=xlt[v * N2E:(v + 1) * N2E, :]))
                    lnr.route(xlt, xlb, aff_sb[:, s * NCH + q, :], Baff)
                k.barrier()
            k.stack = st
        if "AFFD" in P.dbg:
            k.dma("sp", [Baff], [], lambda e: e.dma_start(out=L["AFFD"][:, :], in_=aff_sb[:, :, :].rearrange("p c e -> p (c e)")))
        k.barrier()
    k.stack = prev


def phase_B1(P, k, L):
    phase_B(P, k, L, 1)


def phase_C1(P, k, L):
    phase_C(P, k, L, 1)


def phase_D1(P, k, L):
    phase_D(P, k, L, 1, L["y_out"])


_PROG = {}


def kernel(**inputs):
    xp = np.asarray(inputs["x_prompt"], dtype=np.float32)
    xs = np.asarray(inputs["x_sample"], dtype=np.float32)
    inp = {n: np.asarray(v) for n, v in inputs.items() if n not in ("x_prompt", "x_sample")}
    Bp, Sp, _ = xp.shape
    Bs, Ss, _ = xs.shape
    SL = max(Sp, Ss)
    assert Bp * Sp == Bs * Ss and SL % Sp == 0 and SL % Ss == 0
    nseq = Bp * Sp // SL
    cfg_p = Cfg(nseq, SL, Sp)
    cfg_s = Cfg(nseq, SL, Ss)
    key = (nseq, SL)
    if key not in _PROG:
        _PROG[key] = build(cfg_p)
    P = _PROG[key]
    maps = []
    for cfg, x in ((cfg_p, xp), (cfg_s, xs)):
        P.tabs = const_tables(cfg)
        maps.append(core_inputs(cfg, P, x.reshape(-1, D), inp))
    res = run_bass_kernel_spmd(P.nc, maps, core_ids=[0, 1])
    y_p = np.asarray(res.results[0]["y"], dtype=np.float32).reshape(Bp, Sp, D)
    y_s = np.asarray(res.results[1]["y"], dtype=np.float32).reshape(Bs, Ss, D)
    return (y_p, y_s)
```
